# Optimizing a Trainium2 kernel written in Bass

```python
import math
import jax, jax.numpy as jnp
from jax import lax
import numpy as np

D_MODEL = 1024
BATCH = 4
SEQ = 8192
DEPTH = 1

GRID_W = 64
HY_WIDTH = 512
HY_ORDER = 2
SHORT_K = 3
FILTER_EMB = 33
FILTER_HIDDEN = 64
HY_QUICK_DECAY_PCT = 0.3
HY_GRADUAL_DECAY_PCT = 1.5
HY_DECAY_TARGET = 1e-2
N_HEADS = 8
N_KV_HEADS = 2
HEAD_DIM = 64
Q_BLOCK = 128
ROPE_THETA = 10000.0
N_EXPERTS = 16
EC_FACTOR = 2
D_FF_EXPERT = 2048
EPS = 1e-6
N_BRANCHES = 2
ATT_WIDTH = N_HEADS * HEAD_DIM
KV_WIDTH = N_KV_HEADS * HEAD_DIM
HY_IN = 3 * HY_WIDTH
IN_WIDTH = HY_IN + ATT_WIDTH + 2 * KV_WIDTH + N_BRANCHES * D_MODEL

kernel_name = "hybrid_hyena_gqa_ec_moe_block"

F32 = jnp.float32


def rms_norm(x, g):
    xf = x.astype(F32)
    y = xf * lax.rsqrt(jnp.mean(xf * xf, axis=-1, keepdims=True) + EPS)
    return (y * g.astype(F32)).astype(x.dtype)


def short_conv_centred(u, w, b):
    L = u.shape[1]
    pad = SHORT_K // 2
    up = jnp.pad(u, ((0, 0), (pad, pad), (0, 0)))
    y = b
    for j in range(SHORT_K):
        y = y + up[:, j:j + L] * w[j]
    return y


def hyena_filters(L, w1, b1, w2, b2, w3, b3, w4, freq):
    t01 = jnp.linspace(0.0, 1.0, L, dtype=F32)[:, None]
    bands = (FILTER_EMB - 1) // 2
    f = jnp.linspace(1e-4, bands - 1, bands, dtype=F32)[None, :]
    w = (2.0 * math.pi) * jnp.arange(L, dtype=F32)[:, None] / L
    z = jnp.concatenate([t01, jnp.cos(f * w), -jnp.sin(f * w)], axis=-1)
    fr = freq.astype(F32)
    hdn = jnp.sin(fr[0] * (z @ w1.astype(F32) + b1.astype(F32)))
    hdn = jnp.sin(fr[1] * (hdn @ w2.astype(F32) + b2.astype(F32)))
    hdn = jnp.sin(fr[2] * (hdn @ w3.astype(F32) + b3.astype(F32)))
    h = (hdn @ w4.astype(F32)).reshape(L, HY_ORDER, 2, HY_WIDTH)
    max_decay = math.log(HY_DECAY_TARGET) / HY_QUICK_DECAY_PCT
    min_decay = math.log(HY_DECAY_TARGET) / HY_GRADUAL_DECAY_PCT
    deltas = jnp.linspace(min_decay, max_decay, HY_WIDTH, dtype=F32)
    h = h * jnp.exp(-t01[:, :, None, None] * jnp.abs(deltas))
    h = h / (jnp.sum(jnp.abs(h), axis=0, keepdims=True) + EPS)
    return h


def bidir_fftconv(u, h_fwd, h_bwd):
    L = u.shape[1]
    n = 2 * L
    y_f = jnp.fft.irfft(jnp.fft.rfft(u, n=n, axis=1) * jnp.fft.rfft(h_fwd, n=n, axis=0)[None], n=n, axis=1)[:, :L]
    ur = u[:, ::-1]
    y_b = jnp.fft.irfft(jnp.fft.rfft(ur, n=n, axis=1) * jnp.fft.rfft(h_bwd, n=n, axis=0)[None], n=n, axis=1)[:, :L][:, ::-1]
    return y_f + y_b


def hyena_mixer(z, short_w, short_b, w1, b1, w2, b2, w3, b3, w4, freq, filt_bias):
    L = z.shape[1]
    zc = short_conv_centred(z, short_w, short_b).astype(F32)
    v, x1, x2 = jnp.split(zc, 3, axis=-1)
    h = hyena_filters(L, w1, b1, w2, b2, w3, b3, w4, freq)
    fb = filt_bias.astype(F32)
    gates = (x1, x2)
    y = v
    for o in range(HY_ORDER):
        y = gates[o] * (bidir_fftconv(y, h[:, o, 0], h[:, o, 1]) + fb[o] * y)
    return y.astype(z.dtype)


def axial_rope_tables(L):
    rows = L // GRID_W
    row = jnp.repeat(jnp.arange(rows), GRID_W).astype(F32)
    col = jnp.tile(jnp.arange(GRID_W), rows).astype(F32)
    half = HEAD_DIM // 2
    inv = ROPE_THETA ** (-jnp.arange(0, half, 2, dtype=F32) / half)
    ang = jnp.concatenate([row[:, None] * inv, col[:, None] * inv], axis=-1)
    return jnp.cos(ang), jnp.sin(ang)


def apply_axial_rope(x, cos, sin):
    half = HEAD_DIM // 2
    quarter = half // 2

    def rot(u, c, s):
        u1, u2 = u[..., :quarter], u[..., quarter:]
        return jnp.concatenate([u1 * c - u2 * s, u1 * s + u2 * c], axis=-1)

    return jnp.concatenate([rot(x[..., :half], cos[:, :quarter], sin[:, :quarter]),
                            rot(x[..., half:], cos[:, quarter:], sin[:, quarter:])], axis=-1)


def gqa_attention(q, k, v, q_gain, k_gain):
    B, L, _ = q.shape
    G = N_HEADS // N_KV_HEADS
    q = q.reshape(B, L, N_KV_HEADS, G, HEAD_DIM).transpose(0, 2, 3, 1, 4)
    k = k.reshape(B, L, N_KV_HEADS, HEAD_DIM).transpose(0, 2, 1, 3)
    v = v.reshape(B, L, N_KV_HEADS, HEAD_DIM).transpose(0, 2, 1, 3)
    cos, sin = axial_rope_tables(L)
    q = (apply_axial_rope(rms_norm(q, q_gain).astype(F32), cos, sin) * (HEAD_DIM ** -0.5)).astype(v.dtype)
    k = apply_axial_rope(rms_norm(k, k_gain).astype(F32), cos, sin).astype(v.dtype)
    nb = L // Q_BLOCK
    qb = q.reshape(B, N_KV_HEADS, G, nb, Q_BLOCK, HEAD_DIM).transpose(3, 0, 1, 2, 4, 5)

    def block(qblk):
        s = jnp.einsum('bkgqd,bksd->bkgqs', qblk, k).astype(F32)
        p = jax.nn.softmax(s, axis=-1).astype(v.dtype)
        return jnp.einsum('bkgqs,bksd->bkgqd', p, v)

    o = lax.map(block, qb)
    return o.transpose(1, 0, 4, 2, 3, 5).reshape(B, L, ATT_WIDTH)


def expert_choice_ffn(h, w_router, w_gate, w_up, w_down):
    B, L, D = h.shape
    cap = EC_FACTOR * L // N_EXPERTS
    aff = jax.nn.softmax((h @ w_router).astype(F32), axis=-1)
    top_w, top_i = lax.top_k(jnp.swapaxes(aff, 1, 2), cap)
    xe = jax.vmap(lambda hb, ib: hb[ib])(h, top_i)
    a = jnp.einsum('becd,edf->becf', xe, w_gate)
    u = jnp.einsum('becd,edf->becf', xe, w_up)
    ye = jnp.einsum('becf,efd->becd', jax.nn.silu(a) * u, w_down)
    ye = ye * top_w[..., None].astype(ye.dtype)
    return jax.vmap(lambda ib, yb: jnp.zeros((L, D), yb.dtype).at[ib.reshape(-1)].add(yb.reshape(-1, D)))(top_i, ye)


def setup_inputs(seed: int = 0) -> dict:
    key = jax.random.key(seed)
    ks = jax.random.split(key, 32)
    D = D_MODEL

    def nrm(k, shape, scale):
        return jax.random.normal(k, shape, F32) * scale

    return {
        "x": nrm(ks[0], (BATCH, SEQ, D), 1.0),
        "c": nrm(ks[1], (BATCH, D), 1.0),
        "w_ada": nrm(ks[2], (DEPTH, D, 6 * D), 0.5 * D ** -0.5),
        "b_ada": nrm(ks[3], (DEPTH, 6 * D), 0.02),
        "g_mix": 1.0 + nrm(ks[4], (DEPTH, D), 0.02),
        "g_ffn": 1.0 + nrm(ks[5], (DEPTH, D), 0.02),
        "w_in": nrm(ks[6], (DEPTH, D, IN_WIDTH), D ** -0.5),
        "b_in": nrm(ks[7], (DEPTH, IN_WIDTH), 0.02),
        "short_w": nrm(ks[8], (DEPTH, SHORT_K, HY_IN), SHORT_K ** -0.5),
        "short_b": nrm(ks[9], (DEPTH, HY_IN), 0.02),
        "hy_w1": nrm(ks[10], (DEPTH, FILTER_EMB, FILTER_HIDDEN), FILTER_EMB ** -0.5),
        "hy_b1": nrm(ks[11], (DEPTH, FILTER_HIDDEN), 0.02),
        "hy_w2": nrm(ks[12], (DEPTH, FILTER_HIDDEN, FILTER_HIDDEN), FILTER_HIDDEN ** -0.5),
        "hy_b2": nrm(ks[13], (DEPTH, FILTER_HIDDEN), 0.02),
        "hy_w3": nrm(ks[14], (DEPTH, FILTER_HIDDEN, FILTER_HIDDEN), FILTER_HIDDEN ** -0.5),
        "hy_b3": nrm(ks[15], (DEPTH, FILTER_HIDDEN), 0.02),
        "hy_w4": nrm(ks[16], (DEPTH, FILTER_HIDDEN, HY_ORDER * 2 * HY_WIDTH), FILTER_HIDDEN ** -0.5),
        "hy_freq": 1.0 + nrm(ks[17], (DEPTH, 3, FILTER_HIDDEN), 0.02),
        "hy_bias": nrm(ks[18], (DEPTH, HY_ORDER, HY_WIDTH), 0.1),
        "q_gain": 1.0 + nrm(ks[19], (DEPTH, HEAD_DIM), 0.02),
        "k_gain": 1.0 + nrm(ks[20], (DEPTH, HEAD_DIM), 0.02),
        "w_hy_out": nrm(ks[21], (DEPTH, HY_WIDTH, D), HY_WIDTH ** -0.5),
        "w_att_out": nrm(ks[22], (DEPTH, ATT_WIDTH, D), ATT_WIDTH ** -0.5),
        "w_out": nrm(ks[23], (DEPTH, D, D), D ** -0.5),
        "w_router": nrm(ks[24], (DEPTH, D, N_EXPERTS), D ** -0.5),
        "w_gate": nrm(ks[25], (DEPTH, N_EXPERTS, D, D_FF_EXPERT), D ** -0.5),
        "w_up": nrm(ks[26], (DEPTH, N_EXPERTS, D, D_FF_EXPERT), D ** -0.5),
        "w_down": nrm(ks[27], (DEPTH, N_EXPERTS, D_FF_EXPERT, D), D_FF_EXPERT ** -0.5),
    }


def reference(x, c, w_ada, b_ada, g_mix, g_ffn, w_in, b_in, short_w, short_b,
              hy_w1, hy_b1, hy_w2, hy_b2, hy_w3, hy_b3, hy_w4, hy_freq, hy_bias,
              q_gain, k_gain, w_hy_out, w_att_out, w_out,
              w_router, w_gate, w_up, w_down):
    split_at = [HY_IN, HY_IN + ATT_WIDTH, HY_IN + ATT_WIDTH + KV_WIDTH, HY_IN + ATT_WIDTH + 2 * KV_WIDTH]
    for l in range(DEPTH):
        mod = (jax.nn.silu(c) @ w_ada[l] + b_ada[l])[:, None, :]
        sh1, sc1, gt1, sh2, sc2, gt2 = jnp.split(mod, 6, axis=-1)

        h = rms_norm(x, g_mix[l]) * (1.0 + sc1) + sh1
        z = h @ w_in[l] + b_in[l]
        z_hy, q, k, v, z_g = jnp.split(z, split_at, axis=-1)
        y_hy = hyena_mixer(z_hy, short_w[l], short_b[l], hy_w1[l], hy_b1[l], hy_w2[l], hy_b2[l],
                           hy_w3[l], hy_b3[l], hy_w4[l], hy_freq[l], hy_bias[l]) @ w_hy_out[l]
        y_at = gqa_attention(q, k, v, q_gain[l], k_gain[l]) @ w_att_out[l]
        g_hy, g_at = jnp.split(jax.nn.sigmoid(z_g), N_BRANCHES, axis=-1)
        x = x + gt1 * ((g_hy * y_hy + g_at * y_at) @ w_out[l])

        h2 = rms_norm(x, g_ffn[l]) * (1.0 + sc2) + sh2
        x = x + gt2 * expert_choice_ffn(h2, w_router[l], w_gate[l], w_up[l], w_down[l])
    return x
```

```python
import math
import numpy as np
import concourse.bass as bass
import concourse.mybir as mybir
from concourse.bass_utils import run_bass_kernel_spmd

F32 = mybir.dt.float32
BF16 = mybir.dt.bfloat16
I32 = mybir.dt.int32
AF = mybir.ActivationFunctionType
ALU = mybir.AluOpType
AX = mybir.AxisListType

L = 8192
D = 1024
NT = 64
ENGS = ("pe", "act", "dve", "pool", "sp")


class _Op:
    __slots__ = ("eng", "fn", "deps", "needs_inc", "dma", "inc_idx", "dma_val", "epoch")

    def __init__(self, eng, fn, dma):
        self.eng = eng
        self.fn = fn
        self.deps = []
        self.needs_inc = False
        self.dma = dma
        self.inc_idx = 0
        self.dma_val = 0
        self.epoch = 0


class Sched:
    def __init__(self, nc):
        self.nc = nc
        self.ops = {e: [] for e in ENGS}
        self.last_w = {}
        self.readers = {}
        self.dma_cnt = {}
        self.dma_last = {}
        self.barrier_deps = []
        self.epoch = 0

    def add(self, eng, fn, reads=(), writes=(), dma=None):
        op = _Op(eng, fn, dma)
        op.epoch = self.epoch
        deps = []
        for k in reads:
            w = self.last_w.get(k)
            if w is not None:
                deps.append(w)
        for k in writes:
            w = self.last_w.get(k)
            if w is not None:
                deps.append(w)
            deps.extend(self.readers.get(k, {}).values())
        deps.extend(self.barrier_deps)
        if dma is not None:
            p = self.dma_last.get(dma)
            if p is not None:
                deps.append(p)
            self.dma_cnt[dma] = self.dma_cnt.get(dma, 0) + 16
            op.dma_val = self.dma_cnt[dma]
            self.dma_last[dma] = op
        seen = set()
        for d in deps:
            if id(d) in seen:
                continue
            seen.add(id(d))
            if d.dma is None and d.eng == "pe" and eng == "pe" and dma is None:
                continue
            op.deps.append(d)
            if d.dma is None:
                d.needs_inc = True
        rk = ("dma", dma) if dma is not None else ("eng", eng)
        for k in reads:
            self.readers.setdefault(k, {})[rk] = op
        for k in writes:
            self.last_w[k] = op
            self.readers[k] = {}
        self.ops[eng].append(op)
        return op

    def barrier(self):
        deps = []
        for e in ENGS:
            for op in reversed(self.ops[e]):
                if op.dma is None:
                    deps.append(op)
                    break
        deps.extend(self.dma_last.values())
        self.barrier_deps = deps
        self.epoch += 1

    def emit(self):
        nc = self.nc
        self.barrier()
        fin = _Op("sp", None, None)
        for d in self.barrier_deps:
            fin.deps.append(d)
            if d.dma is None:
                d.needs_inc = True
        self.ops["sp"].append(fin)
        esem = {}
        for e in ENGS:
            c = {}
            for op in self.ops[e]:
                if op.dma is None and op.needs_inc:
                    c[op.epoch] = c.get(op.epoch, 0) + 1
                    op.inc_idx = c[op.epoch]
                    if (e, op.epoch) not in esem:
                        esem[(e, op.epoch)] = nc.alloc_semaphore(name=f"s_{e}_{op.epoch}")
        dsem = {s: nc.alloc_semaphore(name=f"d_{s}") for s in self.dma_cnt}
        ops = self.ops

        def run(eng_name, eng):
            waited = {}
            for op in ops[eng_name]:
                for d in op.deps:
                    if d.dma is not None:
                        key, sem, val = ("d", d.dma), dsem[d.dma], d.dma_val
                    else:
                        key, sem, val = ("e", d.eng, d.epoch), esem[(d.eng, d.epoch)], d.inc_idx
                    if waited.get(key, 0) >= val:
                        continue
                    waited[key] = val
                    eng.wait_ge(sem, val)
                if op.fn is None:
                    continue
                ins = op.fn(eng)
                if op.dma is not None:
                    ins.then_inc(dsem[op.dma], 16)
                elif op.needs_inc:
                    ins.then_inc(esem[(eng_name, op.epoch)], 1)

        block = nc.Block()
        with block:
            @block.tensor
            def _(e):
                run("pe", e)

            @block.scalar
            def _(e):
                run("act", e)

            @block.vector
            def _(e):
                run("dve", e)

            @block.gpsimd
            def _(e):
                run("pool", e)

            @block.sync
            def _(e):
                run("sp", e)


class Arena:
    def __init__(self, big, lo, hi):
        self.big, self.lo, self.hi = big, lo, hi
        self.off = lo

    def reset(self):
        self.off = self.lo

    def alloc(self, shape, dt):
        n = int(np.prod(shape[1:]))
        esz = 4 if dt in (F32, I32) else 2
        nb = (n * esz + 31) // 32 * 32
        assert self.off + nb <= self.hi, ("SBUF arena overflow", shape, self.off, nb, self.hi)
        a = self.big[0:shape[0], self.off // 4:(self.off + nb) // 4]
        self.off += nb
        if dt != F32:
            a = a.bitcast(dt)
        a = a[:, 0:n]
        if len(shape) > 2:
            names = " ".join(f"d{i}" for i in range(1, len(shape)))
            a = a.rearrange(f"p ({names}) -> p {names}", **{f"d{i}": shape[i] for i in range(1, len(shape))})
        return a


def build(stop_after="F", dbg=()):
    nc = bass.Bass("TRN2", target_bir_lowering=False)
    S = Sched(nc)
    dbg_out = {}

    def din(name, shape, dt=F32):
        return nc.dram_tensor(name, list(shape), dt, kind="ExternalInput")

    def dscr(name, shape, dt):
        if name in dbg:
            t = nc.dram_tensor(name, list(shape), dt, kind="ExternalOutput")
            dbg_out[name] = t
            return t
        return nc.dram_tensor(name, list(shape), dt)

    x_d = din("x", [L, D])
    ccol_d = din("ccol", [128, 8])
    wada_d = din("wada", [128, 8, 6144])
    bada_d = din("bada", [1, 6144])
    gmix_d = din("gmixcol", [128, 8])
    gffn_d = din("gffnrow", [1, D])
    win_d = din("win", [128, 8, 4352])
    binh_d = din("binh", [1, 1536])
    bino_d = din("bino", [1, 2816])
    sw_d = din("sw", [3, 1536])
    sb_d = din("sb", [1, 1536])
    ind_d = din("ind", [4, 5, 128])
    ident_d = din("ident", [128, 256])
    zT_d = din("zT", [33, 2, L])
    t01_d = din("t01", [2, L])
    delta_d = din("deltacol", [128, 4])
    hw1_d = din("hw1", [33, 64])
    hw2_d = din("hw2", [64, 64])
    hw3_d = din("hw3", [64, 64])
    hw4_d = din("hw4", [64, 2048])
    hb_d = din("hbcol", [64, 3])
    hfr_d = din("hfrcol", [64, 3])
    fb_d = din("fbcol", [128, 8])
    rope_d = din("rope", [128, NT, 64])
    qg_d = din("qgain", [1, 64])
    kg_d = din("kgain", [1, 64])
    why_d = din("why", [128, 4, D])
    wat_d = din("wat", [128, 4, D])
    wout_d = din("wout", [128, 8, D])
    wr_d = din("wr", [128, 8, 16])
    wg_d = din("wg", [16, 128, 8, 2048])
    wu_d = din("wu", [16, 128, 8, 2048])
    wd_d = din("wd", [16, 128, 16, D])
    iota_d = din("iota", [128, 1024])
    tok_d = din("tokid", [128, NT])
    tri_d = din("tri", [128, 128])
    out_d = nc.dram_tensor("out", [L, D], F32, kind="ExternalOutput")

    mod_s = dscr("mod_s", [1, 6144], F32)
    v_s = dscr("v_s", [NT, 128, 512], BF16)
    x2_s = dscr("x2_s", [NT, 128, 512], BF16)
    x1_s = dscr("x1_s", [NT, 128, 512], BF16)
    q_s = dscr("q_s", [NT, 128, 512], BF16)
    kv_s = dscr("kv_s", [NT, 128, 256], BF16)
    gt_s = dscr("gt_s", [NT, 128, 2048], BF16)
    a_s = dscr("a_s", [2, 512, 16384], BF16)
    hy_s = dscr("hy_s", [NT, 128, 512], BF16)
    att_s = dscr("att_s", [NT, 128, 512], BF16)
    h2_s = dscr("h2_s", [L, D], BF16)

    NB = nc.sbuf_bytes_remaining
    big = nc.alloc_sbuf_tensor("big", [128, (NB - 128) // 4], F32)
    TOT = (NB - 128) // 4 * 4
    PERS = 14848
    P = Arena(big, 0, PERS)
    A = Arena(big, PERS, TOT)
    ps = [nc.alloc_psum_tensor(f"ps{i}", [128, 512], F32) for i in range(8)]

    def psb(i):
        return ps[i][:].bitcast(BF16)

    def dma(q, out, in_, reads=(), writes=(), stream=None, slow=False):
        if slow:
            return S.add(q, lambda e: e.dma_start(out=out, in_=in_, allow_slow_non_contiguous=True), reads=reads, writes=writes, dma=stream)
        return S.add(q, lambda e: e.dma_start(out=out, in_=in_), reads=reads, writes=writes, dma=stream)

    identf = P.alloc([128, 256], F32)
    identb = P.alloc([128, 256], BF16)
    modcol = P.alloc([128, 48], F32)
    gt1b = P.alloc([128, D], F32)
    gt2b = P.alloc([128, D], F32)
    A1col = P.alloc([128, 8], F32)
    gmixc = P.alloc([128, 8], F32)
    ones_b = P.alloc([128, 128], BF16)
    ones_f = P.alloc([128, 128], F32)
    aff_sb = P.alloc([128, NT, 16], F32)
    dma("sp", identf, ident_d.ap(), writes=["identf"], stream="c0")
    S.add("dve", lambda e: e.tensor_copy(out=identb, in_=identf), reads=["identf"], writes=["identb"])
    S.add("dve", lambda e: e.memset(ones_b, 1.0), writes=["ones_b"])
    S.add("dve", lambda e: e.memset(ones_f, 1.0), writes=["ones_f"])
    I_b = identb[:, 0:128]
    J_b = identb[:, 128:256]

    ccol = A.alloc([128, 8], F32)
    sil = A.alloc([128, 8], F32)
    modrow = A.alloc([1, 6144], F32)
    badar = A.alloc([1, 6144], F32)
    wa = A.alloc([128, 8, 2048], F32)
    dma("sp", ccol, ccol_d.ap(), writes=["ccol"], stream="c0")
    dma("sp", badar, bada_d.ap(), writes=["badar"], stream="c0")
    dma("sp", gmixc, gmix_d.ap(), writes=["gmixc"], stream="c0")
    S.add("act", lambda e: e.activation(out=sil, in_=ccol, func=AF.Silu), reads=["ccol"], writes=["sil"])
    for ch in range(3):
        dma("sp", wa, wada_d.ap()[:, :, ch * 2048:(ch + 1) * 2048], writes=["wa"], stream="wa")
        for n in range(4):
            for kc in range(8):
                S.add("pe", lambda e, n=n, kc=kc: e.matmul(ps[n][0:1, :], lhsT=sil[:, kc:kc + 1], rhs=wa[:, kc, n * 512:(n + 1) * 512],
                                                         start=(kc == 0), stop=(kc == 7)),
                      reads=["sil", "wa"], writes=[f"ps{n}"])
            c0 = ch * 2048 + n * 512
            S.add("dve", lambda e, n=n, c0=c0: e.tensor_tensor(out=modrow[:, c0:c0 + 512], in0=ps[n][0:1, :], in1=badar[:, c0:c0 + 512], op=ALU.add),
                  reads=[f"ps{n}", "badar"], writes=["modrow"])
    dma("sp", mod_s.ap(), modrow, reads=["modrow"], writes=["mod_s"], stream="c0")
    dma("sp", modcol, mod_s.ap().rearrange("o (q p) -> p (o q)", p=128), reads=["mod_s"], writes=["modcol"], stream="c0", slow=True)
    dma("sp", gt1b, mod_s.ap()[:, 2048:3072].partition_broadcast(128), reads=["mod_s"], writes=["gt1b"], stream="c0")
    dma("sp", gt2b, mod_s.ap()[:, 5120:6144].partition_broadcast(128), reads=["mod_s"], writes=["gt2b"], stream="c0")
    S.add("dve", lambda e: e.scalar_tensor_tensor(out=A1col, in0=modcol[:, 8:16], scalar=1.0, in1=gmixc, op0=ALU.add, op1=ALU.mult),
          reads=["modcol", "gmixc"], writes=["A1col"])
    sh1col = modcol[:, 0:8]
    if "mod" in dbg:
        dbg_out["mod"] = nc.dram_tensor("dbg_mod", [1, 6144], F32, kind="ExternalOutput")
        dma("sp", dbg_out["mod"].ap(), modrow, reads=["modrow"], stream="c0")

    S.barrier()
    A.reset()
    wb = A.alloc([128, 8, 4352], BF16)
    wh0 = A.alloc([128, 8, 1536], BF16)
    wh2 = A.alloc([128, 8, 1536], BF16)
    swb_lo = A.off
    swb = A.alloc([128, 3, 1536], BF16)
    swb_hi = A.off
    swf = A.alloc([3, 1536], F32)
    bh3 = A.alloc([4, 1536], F32)
    brow4 = A.alloc([4, 1536], BF16)
    bof = A.alloc([1, 2816], F32)
    bob = A.alloc([1, 2816], BF16)
    indf = A.alloc([4, 5, 128], F32)
    indb = A.alloc([4, 5, 128], BF16)
    for kc in range(8):
        dma("pool", wb[:, kc, :], win_d.ap()[:, kc, :], writes=["wb"], stream=f"wb{kc % 2}")
    dma("pool", swb.rearrange("p a c -> p (a c)"), sw_d.ap().rearrange("a c -> (a c)").partition_broadcast(128), writes=["swb"], stream="wb0")
    dma("sp", swf, sw_d.ap(), writes=["swf"], stream="c0")
    for r in range(3):
        dma("sp", bh3[r:r + 1, :], binh_d.ap(), writes=["bh3"], stream="c0")
    dma("sp", bh3[3:4, :], sb_d.ap(), writes=["bh3"], stream="c0")
    dma("sp", bof, bino_d.ap(), writes=["bof"], stream="c0")
    dma("sp", indf, ind_d.ap(), writes=["indf"], stream="c0")
    S.add("dve", lambda e: e.tensor_tensor(out=bh3[0:3, :], in0=bh3[0:3, :], in1=swf, op=ALU.mult), reads=["bh3", "swf"], writes=["bh3"])
    S.add("dve", lambda e: e.tensor_copy(out=brow4, in_=bh3), reads=["bh3"], writes=["brow4"])
    S.add("dve", lambda e: e.tensor_copy(out=bob, in_=bof), reads=["bof"], writes=["bob"])
    S.add("dve", lambda e: e.tensor_copy(out=indb, in_=indf), reads=["indf"], writes=["indb"])
    for kc in range(8):
        S.add("dve", lambda e, kc=kc: e.tensor_tensor(out=wh0[:, kc, :], in0=wb[:, kc, 0:1536], in1=swb[:, 0, :], op=ALU.mult),
              reads=["wb", "swb"], writes=["wh0"])
        S.add("pool", lambda e, kc=kc: e.tensor_tensor(out=wh2[:, kc, :], in0=wb[:, kc, 0:1536], in1=swb[:, 2, :], op=ALU.mult),
              reads=["wb", "swb"], writes=["wh2"])
    for kc in range(8):
        S.add("dve", lambda e, kc=kc: e.tensor_tensor(out=wb[:, kc, 0:1536], in0=wb[:, kc, 0:1536], in1=swb[:, 1, :], op=ALU.mult),
              reads=["wb", "swb", "wh0", "wh2"], writes=["wb"])
    whs = [wh0, wb, wh2]

    NSL = 4
    S.barrier()
    A2_ = Arena(big, swb_lo, swb_hi)
    xt = [A.alloc([128, D], F32)] * 3
    junk = A.alloc([128, D], BF16)
    ssq = [A.alloc([128, 1], F32) for _ in range(2)] + [A2_.alloc([128, 1], F32)]
    xb = [A.alloc([128, D], BF16) for _ in range(2)] + [A2_.alloc([128, D], BF16)]
    hN = [A.alloc([128, 8, 130], BF16) for _ in range(3)] + [A2_.alloc([128, 8, 130], BF16)]
    hR = [A.alloc([128, 8, 130], BF16) for _ in range(3)] + [A2_.alloc([128, 8, 130], BF16)]
    stg = [A.alloc([128, 4352], BF16)] * 2
    for s in range(NSL):
        S.add("dve", lambda e, s=s: e.memset(hN[s], 0.0), writes=[f"hN{s}"])
        S.add("dve", lambda e, s=s: e.memset(hR[s], 0.0), writes=[f"hR{s}"])

    def stage1(j):
        s2, s3 = j % 3, j % NSL
        dma("sp", xt[s2], x_d.ap()[j * 128:(j + 1) * 128, :], writes=["xt"], stream="x0")
        S.add("act", lambda e: e.activation(out=junk, in_=xt[s2], func=AF.Square, accum_out=ssq[s2]), reads=["xt"], writes=["junk", f"ssq{s2}"])
        S.add("act", lambda e: e.activation(out=ssq[s2], in_=ssq[s2], func=AF.Sqrt, scale=1.0 / D, bias=1e-6), reads=[f"ssq{s2}"], writes=[f"ssq{s2}"])
        S.add("dve", lambda e: e.reciprocal(out=ssq[s2], in_=ssq[s2]), reads=[f"ssq{s2}"], writes=[f"ssq{s2}"])
        S.add("dve", lambda e: e.tensor_scalar(out=xb[s2], in0=xt[s2], scalar1=ssq[s2][:, 0:1], scalar2=None, op0=ALU.mult),
              reads=["xt", f"ssq{s2}"], writes=[f"xb{s2}"])
        for kc in range(8):
            S.add("pe", lambda e, kc=kc: e.transpose(out=psb(0)[:, kc * 128:(kc + 1) * 128], in_=xb[s2][:, kc * 128:(kc + 1) * 128], identity=I_b),
                  reads=[f"xb{s2}", "identb"], writes=["ps0"])
        for kc in range(8):
            S.add("pe", lambda e, kc=kc: e.transpose(out=psb(1)[:, kc * 128:(kc + 1) * 128], in_=xb[s2][:, kc * 128:(kc + 1) * 128], identity=J_b),
                  reads=[f"xb{s2}", "identb"], writes=["ps1"])
        for kc in range(8):
            S.add("act", lambda e, kc=kc: e.activation(out=hN[s3][:, kc, 1:129], in_=psb(0)[:, kc * 128:(kc + 1) * 128], func=AF.Identity,
                                                        scale=A1col[:, kc:kc + 1], bias=sh1col[:, kc:kc + 1]),
                  reads=["ps0", "A1col", "modcol"], writes=[f"hN{s3}"])
            S.add("act", lambda e, kc=kc: e.activation(out=hR[s3][:, kc, 1:129], in_=psb(1)[:, kc * 128:(kc + 1) * 128], func=AF.Identity,
                                                        scale=A1col[:, kc:kc + 1], bias=sh1col[:, kc:kc + 1]),
                  reads=["ps1", "A1col", "modcol"], writes=[f"hR{s3}"])
        if j > 0:
            sp = (j - 1) % NSL
            S.add("dve", lambda e: e.tensor_copy(out=hN[sp][:, :, 129:130], in_=hN[s3][:, :, 1:2]), reads=[f"hN{s3}"], writes=[f"hN{sp}"])
            S.add("dve", lambda e: e.tensor_copy(out=hN[s3][:, :, 0:1], in_=hN[sp][:, :, 128:129]), reads=[f"hN{sp}"], writes=[f"hN{s3}"])
            S.add("dve", lambda e: e.tensor_copy(out=hR[sp][:, :, 0:1], in_=hR[s3][:, :, 128:129]), reads=[f"hR{s3}"], writes=[f"hR{sp}"])
            S.add("dve", lambda e: e.tensor_copy(out=hR[s3][:, :, 129:130], in_=hR[sp][:, :, 1:2]), reads=[f"hR{sp}"], writes=[f"hR{s3}"])
        else:
            S.add("dve", lambda e: e.memset(hN[s3][:, :, 0:1], 0.0), writes=[f"hN{s3}"])
            S.add("dve", lambda e: e.memset(hR[s3][:, :, 129:130], 0.0), writes=[f"hR{s3}"])
        if j == NT - 1:
            S.add("dve", lambda e: e.memset(hN[s3][:, :, 129:130], 0.0), writes=[f"hN{s3}"])
            S.add("dve", lambda e: e.memset(hR[s3][:, :, 0:1], 0.0), writes=[f"hR{s3}"])

    bank_rr = [2]

    def nextbank():
        b = bank_rr[0]
        bank_rr[0] = 2 + (b - 2 + 1) % 6
        return b

    def inproj(j):
        s3, s2 = j % NSL, j % 2
        iN = 1 if j == 0 else (2 if j == NT - 1 else 0)
        iR = 3 if j == 0 else (4 if j == NT - 1 else 0)
        offR = [2, 1, 0]
        offN = [0, 1, 2]
        groups = []
        for h in range(2):
            groups.append(("R", h * 512, 512))
        groups.append(("N", 1024, 512))
        for c0, w in ((1536, 512), (2048, 256), (2304, 512), (2816, 512), (3328, 512), (3840, 512)):
            groups.append(("O", c0, w))
        for gi, (kind, c0, w) in enumerate(groups):
            b = nextbank()
            key = f"ps{b}"
            if kind in ("R", "N"):
                src, srck, offs, ii = (hR[s3], f"hR{s3}", offR, iR) if kind == "R" else (hN[s3], f"hN{s3}", offN, iN)
                first = True
                for sh in range(3):
                    for kc in range(8):
                        S.add("pe", lambda e, b=b, sh=sh, kc=kc, src=src, offs=offs, first=first, c0=c0, w=w:
                              e.matmul(ps[b][:, 0:w], lhsT=src[:, kc, offs[sh]:offs[sh] + 128], rhs=whs[sh][:, kc, c0:c0 + w], start=first, stop=False),
                              reads=[srck, "wb", "wh0", "wh2"], writes=[key])
                        first = False
                S.add("pe", lambda e, b=b, ii=ii, c0=c0, w=w: e.matmul(ps[b][:, 0:w], lhsT=indb[:, ii, :], rhs=brow4[:, c0:c0 + w], start=False, stop=True),
                      reads=["indb", "brow4"], writes=[key])
            else:
                for kc in range(8):
                    S.add("pe", lambda e, b=b, kc=kc, c0=c0, w=w: e.matmul(ps[b][:, 0:w], lhsT=hN[s3][:, kc, 1:129], rhs=wb[:, kc, c0:c0 + w], start=(kc == 0), stop=False),
                          reads=[f"hN{s3}", "wb"], writes=[key])
                S.add("pe", lambda e, b=b, c0=c0, w=w: e.matmul(ps[b][:, 0:w], lhsT=ones_b[0:1, :], rhs=bob[:, c0 - 1536:c0 - 1536 + w], start=False, stop=True),
                      reads=["ones_b", "bob"], writes=[key])
            if c0 >= 2304:
                S.add("act", lambda e, b=b, c0=c0, w=w: e.activation(out=stg[s2][:, c0:c0 + w], in_=ps[b][:, 0:w], func=AF.Sigmoid),
                      reads=[key], writes=["stg"])
            else:
                S.add("dve", lambda e, b=b, c0=c0, w=w: e.tensor_copy(out=stg[s2][:, c0:c0 + w], in_=ps[b][:, 0:w]),
                      reads=[key], writes=["stg"])
        st = stg[s2]
        for dst, c0, w in ((v_s, 0, 512), (x2_s, 512, 512), (x1_s, 1024, 512), (q_s, 1536, 512), (kv_s, 2048, 256), (gt_s, 2304, 2048)):
            dma("sp", dst.ap()[j], st[:, c0:c0 + w], reads=["stg"], writes=[dst.name], stream="st0")

    stage1(0)
    stage1(1)
    for j in range(NT):
        if j + 2 < NT:
            stage1(j + 2)
        inproj(j)

    if stop_after == "B":
        S.emit()
        return nc, dbg_out

    S.barrier()
    A.reset()
    TWO_PI = 2.0 * math.pi
    w1f = A.alloc([33, 64], F32)
    w2f = A.alloc([64, 64], F32)
    w3f = A.alloc([64, 64], F32)
    w4f = A.alloc([64, 2048], F32)
    hbc = A.alloc([64, 3], F32)
    hfc = A.alloc([64, 3], F32)
    hoff = A.alloc([64, 3], F32)
    fbc = A.alloc([128, 8], F32)
    dlc = A.alloc([128, 4], F32)
    l1 = A.alloc([128, 2], F32)
    cen = A.alloc([128, 2], F32)
    negpi = A.alloc([128, 1], F32)
    h3 = [A.alloc([64, L], F32) for _ in range(2)]
    zTs = A.alloc([33, L], F32)
    h1s = A.alloc([64, L], F32)
    h2s = A.alloc([64, L], F32)
    for t, d_ in ((w1f, hw1_d), (w2f, hw2_d), (w3f, hw3_d), (w4f, hw4_d), (hbc, hb_d), (hfc, hfr_d), (fbc, fb_d), (dlc, delta_d)):
        dma("sp", t, d_.ap(), writes=["fw"], stream="c0")
    S.add("dve", lambda e: e.memset(negpi, -math.pi), writes=["negpi"])
    hfs = A.alloc([64, 3], F32)
    S.add("dve", lambda e: e.tensor_scalar(out=hfs, in0=hfc, scalar1=1.0 / TWO_PI, scalar2=None, op0=ALU.mult), reads=["fw"], writes=["hfs"])
    S.add("dve", lambda e: e.tensor_tensor(out=hoff, in0=hbc, in1=hfs, op=ALU.mult), reads=["fw", "hfs"], writes=["hoff"])
    tmpc = [A.alloc([64, 512], F32) for _ in range(2)]
    tmpi = [A.alloc([64, 512], I32) for _ in range(2)]
    tmpf = [A.alloc([64, 512], F32) for _ in range(2)]
    for o2 in range(2):
        dma("sp", zTs, zT_d.ap()[:, o2, :], writes=["zTs"], stream="c0")
        layers = ((w1f, zTs, "zTs", h1s, "h1s", 33), (w2f, h1s, "h1s", h2s, "h2s", 64), (w3f, h2s, "h2s", h3[o2], f"h3{o2}", 64))
        for li, (wf, src, srck, dst, dstk, kk) in enumerate(layers):
            for ch in range(16):
                b = ch % 4
                tb = ch % 2
                sl = slice(ch * 512, (ch + 1) * 512)
                S.add("pe", lambda e, b=b, wf=wf, src=src, sl=sl, kk=kk: e.matmul(ps[b][0:64, :], lhsT=wf[0:kk, :], rhs=src[0:kk, sl], start=True, stop=True),
                      reads=["fw", srck], writes=[f"ps{b}"])
                S.add("dve", lambda e, b=b, tb=tb, li=li: e.tensor_scalar(out=tmpc[tb], in0=ps[b][0:64, :], scalar1=hfs[:, li:li + 1], scalar2=hoff[:, li:li + 1],
                                                                         op0=ALU.mult, op1=ALU.add),
                      reads=[f"ps{b}", "hfs", "hoff"], writes=[f"tmpc{tb}"])
                S.add("dve", lambda e, tb=tb: e.tensor_copy(out=tmpi[tb], in_=tmpc[tb]), reads=[f"tmpc{tb}"], writes=[f"tmpi{tb}"])
                S.add("pool", lambda e, tb=tb: e.tensor_copy(out=tmpf[tb], in_=tmpi[tb]), reads=[f"tmpi{tb}"], writes=[f"tmpf{tb}"])
                S.add("pool", lambda e, tb=tb: e.tensor_tensor(out=tmpf[tb], in0=tmpc[tb], in1=tmpf[tb], op=ALU.subtract), reads=[f"tmpc{tb}", f"tmpf{tb}"], writes=[f"tmpf{tb}"])
                S.add("act", lambda e, tb=tb, dst=dst, sl=sl: e.activation(out=dst[:, sl], in_=tmpf[tb], func=AF.Sin, scale=TWO_PI - 1e-6),
                      reads=[f"tmpf{tb}"], writes=[dstk])
    if "h3" in dbg:
        dbg_out["h3"] = nc.dram_tensor("dbg_h3", [2, 64, L], F32, kind="ExternalOutput")
        for o2 in range(2):
            dma("sp", dbg_out["h3"].ap()[o2], h3[o2], reads=[f"h3{o2}"], stream="c0")
    S.barrier()
    A.off -= (33 + 64 + 64) * 0
    A.off = A.off - 3 * L * 4 - 6 * 512 * 4 - 32
    wnd = A.alloc([128, L], F32)
    hfull = A.alloc([128, L], F32)
    Abuf = A.alloc([128, 16384], BF16)
    S.add("dve", lambda e: e.memset(Abuf[:, 16382:16384], 0.0), writes=["Abuf"])
    for o in range(2):
        for g in range(4):
            parts = ((1, 1 - o), (0, o))
            for pi_, (o2, dr) in enumerate(parts):
                col0 = o * 1024 + dr * 512 + g * 128
                dma("sp", wnd, t01_d.ap()[o2:o2 + 1, :].partition_broadcast(128), writes=["wnd"], stream="c0")
                S.add("act", lambda e, g=g: e.activation(out=wnd, in_=wnd, func=AF.Exp, scale=dlc[:, g:g + 1]), reads=["wnd", "fw"], writes=["wnd"])
                for ch in range(16):
                    b = ch % 4
                    sl = slice(ch * 512, (ch + 1) * 512)
                    S.add("pe", lambda e, b=b, col0=col0, o2=o2, sl=sl: e.matmul(ps[b][:, :], lhsT=w4f[:, col0:col0 + 128], rhs=h3[o2][:, sl], start=True, stop=True),
                          reads=["fw", f"h3{o2}"], writes=[f"ps{b}"])
                    S.add("dve", lambda e, b=b, sl=sl: e.tensor_tensor(out=hfull[:, sl], in0=ps[b][:, :], in1=wnd[:, sl], op=ALU.mult),
                          reads=[f"ps{b}", "wnd"], writes=["hfull"])
                S.add("act", lambda e, pi_=pi_: e.activation(out=wnd, in_=hfull, func=AF.Abs, accum_out=l1[:, pi_:pi_ + 1]), reads=["hfull"], writes=["wnd", "l1"])
                S.add("dve", lambda e, pi_=pi_: e.tensor_scalar(out=l1[:, pi_:pi_ + 1], in0=l1[:, pi_:pi_ + 1], scalar1=1e-6, scalar2=None, op0=ALU.add), reads=["l1"], writes=["l1"])
                S.add("dve", lambda e, pi_=pi_: e.reciprocal(out=l1[:, pi_:pi_ + 1], in_=l1[:, pi_:pi_ + 1]), reads=["l1"], writes=["l1"])
                if pi_ == 0:
                    S.add("dve", lambda e: e.tensor_scalar(out=Abuf[:, 0:8192], in0=hfull, scalar1=l1[:, 0:1], scalar2=None, op0=ALU.mult), reads=["hfull", "l1"], writes=["Abuf"])
                    S.add("dve", lambda e: e.tensor_scalar(out=cen[:, 0:1], in0=hfull[:, 8191:8192], scalar1=l1[:, 0:1], scalar2=None, op0=ALU.mult), reads=["hfull", "l1"], writes=["cen"])
                else:
                    S.add("dve", lambda e: e.tensor_scalar(out=Abuf[:, 8191:16383], in0=hfull, scalar1=l1[:, 1:2], scalar2=None, op0=ALU.mult), reads=["hfull", "l1"], writes=["Abuf"])
                    S.add("dve", lambda e: e.tensor_scalar(out=cen[:, 1:2], in0=hfull[:, 0:1], scalar1=l1[:, 1:2], scalar2=None, op0=ALU.mult), reads=["hfull", "l1"], writes=["cen"])
            fcol = o * 4 + g
            S.add("dve", lambda e: e.tensor_tensor(out=cen[:, 0:1], in0=cen[:, 0:1], in1=cen[:, 1:2], op=ALU.add), reads=["cen"], writes=["cen"])
            S.add("dve", lambda e, fcol=fcol: e.tensor_tensor(out=Abuf[:, 8191:8192], in0=cen[:, 0:1], in1=fbc[:, fcol:fcol + 1], op=ALU.add), reads=["cen", "fw", "Abuf"], writes=["Abuf"])
            dma("sp", a_s.ap()[o, g * 128:(g + 1) * 128, :], Abuf, reads=["Abuf"], writes=["a_s"], stream="c0")

    if stop_after == "A2":
        S.emit()
        return nc, dbg_out

    S.barrier()
    A.reset()
    NSTRIP = 3
    SW = 127 * 128
    vg = A.alloc([128, NT, 128], BF16)
    x1g = A.alloc([128, NT, 128], BF16)
    x2g = A.alloc([128, NT, 128], BF16)
    wgb = A.alloc([128, NT, 128], BF16)
    hyo = A.alloc([128, NT, 128], BF16)
    strips = [A.alloc([128, SW], BF16) for _ in range(NSTRIP)]
    sctr = [0]
    cb = [0]
    dlist = [0] + [d for d in range(-63, 64) if d != 0]

    def conv(o, g, src, srck, gate, gatek, dst, dstk):
        for c in range(128):
            si = sctr[0] % NSTRIP
            sctr[0] += 1
            ch = g * 128 + c
            base = a_s.ap()[o, ch:ch + 1, 0:1]
            hk = bass.AP(a_s, base.offset, [[1, 128], [1, SW]])
            dma("sp", strips[si], hk, reads=["a_s"], writes=[f"strip{si}"], stream=f"strip{si}")
            b = 4 + cb[0] % 4
            cb[0] += 1
            for n_, d in enumerate(dlist):
                i0, i1 = max(0, d), min(64, 64 + d)
                off = 128 * (d + 63) if o == 0 else 128 * (63 - d)
                S.add("pe", lambda e, b=b, si=si, off=off, i0=i0, i1=i1, d=d, c=c, n_=n_:
                      e.matmul(ps[b][:, i0:i1], lhsT=strips[si][:, off:off + 128], rhs=src[:, i0 - d:i1 - d, c], start=(n_ == 0), stop=(n_ == 126)),
                      reads=[f"strip{si}", srck], writes=[f"ps{b}"])
            S.add("dve", lambda e, b=b, c=c: e.tensor_tensor(out=dst[:, :, c], in0=ps[b][:, 0:64], in1=gate[:, :, c], op=ALU.mult),
                  reads=[f"ps{b}", gatek], writes=[dstk])

    for g in range(4):
        cs = slice(g * 128, (g + 1) * 128)
        dma("sp", vg, v_s.ap()[:, :, cs].rearrange("j p c -> p j c"), reads=["v_s"], writes=["vg"], stream="hl0")
        dma("sp", x1g, x1_s.ap()[:, :, cs].rearrange("j p c -> p j c"), reads=["x1_s"], writes=["x1g"], stream="hl1")
        dma("sp", x2g, x2_s.ap()[:, :, cs].rearrange("j p c -> p j c"), reads=["x2_s"], writes=["x2g"], stream="hl2")
        conv(0, g, vg, "vg", x1g, "x1g", wgb, "wgb")
        conv(1, g, wgb, "wgb", x2g, "x2g", hyo, "hyo")
        dma("sp", hy_s.ap()[:, :, cs].rearrange("j p c -> p j c"), hyo, reads=["hyo"], writes=["hy_s"], stream="hl3")

    if stop_after == "C":
        S.emit()
        return nc, dbg_out

    S.barrier()
    A.reset()
    qT = A.alloc([128, 4, L], BF16)
    kTA = A.alloc([128, L], BF16)
    kTB = A.alloc([128, L], BF16)
    Vx = A.alloc([128, NT, 2, 128], BF16)
    ropeT = A.alloc([128, NT, 64], F32)
    gq = A.alloc([128, 64], F32)
    gk = A.alloc([128, 64], F32)
    dma("sp", ropeT, rope_d.ap(), writes=["ropeT"], stream="c0")
    dma("sp", gq, qg_d.ap().partition_broadcast(128), writes=["gq"], stream="c0")
    dma("sp", gk, kg_d.ap().partition_broadcast(128), writes=["gk"], stream="c0")
    S.add("pool", lambda e: e.memset(Vx, 0.0), writes=["Vx"])
    S.add("pool", lambda e: e.memset(Vx[:, :, :, 64:65], 1.0), writes=["Vx"])
    S.add("dve", lambda e: e.memset(kTA[64:128, :], 0.0), writes=["kT"])
    S.add("dve", lambda e: e.memset(kTB[0:64, :], 0.0), writes=["kT"])
    qb = [A.alloc([128, 512], BF16) for _ in range(2)]
    kvb = [A.alloc([128, 256], BF16) for _ in range(2)]
    wf_ = A.alloc([128, 512], F32)
    wsq = A.alloc([128, 512], F32)
    wss = A.alloc([128, 8], F32)
    wn = A.alloc([128, 512], F32)
    wtA = A.alloc([128, 256], F32)
    wtB = A.alloc([128, 256], F32)
    qr = A.alloc([128, 512], BF16)
    kr = A.alloc([128, 128], BF16)

    def normrope(srcv, nh, gain, gaink, j, outv, tag):
        W = nh * 64
        f, sq_, ss_, n_ = wf_[:, 0:W], wsq[:, 0:W], wss[:, 0:nh], wn[:, 0:W]
        f3 = f.rearrange("p (h d) -> p h d", h=nh)
        n3 = n_.rearrange("p (h d) -> p h d", h=nh)
        S.add("dve", lambda e: e.tensor_copy(out=f, in_=srcv), reads=[tag], writes=["wf"])
        S.add("dve", lambda e: e.tensor_tensor(out=sq_, in0=f, in1=f, op=ALU.mult), reads=["wf"], writes=["wsq"])
        S.add("dve", lambda e: e.tensor_reduce(out=ss_, in_=sq_.rearrange("p (h d) -> p h d", h=nh), axis=AX.X, op=ALU.add), reads=["wsq"], writes=["wss"])
        S.add("act", lambda e: e.activation(out=ss_, in_=ss_, func=AF.Sqrt, scale=1.0 / 64, bias=1e-6), reads=["wss"], writes=["wss"])
        S.add("dve", lambda e: e.reciprocal(out=ss_, in_=ss_), reads=["wss"], writes=["wss"])
        S.add("dve", lambda e: e.tensor_tensor(out=n3, in0=f3, in1=ss_.unsqueeze(2).broadcast_to([128, nh, 64]), op=ALU.mult), reads=["wf", "wss"], writes=["wn"])
        S.add("dve", lambda e: e.tensor_tensor(out=n3, in0=n3, in1=gain.unsqueeze(1).broadcast_to([128, nh, 64]), op=ALU.mult), reads=["wn", gaink], writes=["wn"])
        o3 = outv.rearrange("p (h d) -> p h d", h=nh)
        for hf in range(2):
            nh4 = n3[:, :, hf * 32:(hf + 1) * 32].rearrange("p h (u d) -> p h u d", u=2)
            oh4 = o3[:, :, hf * 32:(hf + 1) * 32].rearrange("p h (u d) -> p h u d", u=2)
            cosb = ropeT[:, j, hf * 16:(hf + 1) * 16]
            sinb = ropeT[:, j, 32 + hf * 16:32 + (hf + 1) * 16]
            tA = wtA[:, 0:nh * 32].rearrange("p (h u d) -> p h u d", h=nh, u=2)
            tB = wtB[:, 0:nh * 32].rearrange("p (h u d) -> p h u d", h=nh, u=2)
            S.add("dve", lambda e, nh4=nh4, tA=tA, cosb=cosb: e.tensor_tensor(out=tA, in0=nh4, in1=cosb.unsqueeze(1).unsqueeze(1).broadcast_to([128, nh, 2, 16]), op=ALU.mult),
                  reads=["wn", "ropeT"], writes=["wtA"])
            S.add("dve", lambda e, nh4=nh4, tB=tB, sinb=sinb: e.tensor_tensor(out=tB[:, :, 0, :], in0=nh4[:, :, 1, :], in1=sinb.unsqueeze(1).broadcast_to([128, nh, 16]), op=ALU.mult),
                  reads=["wn", "ropeT"], writes=["wtB"])
            S.add("dve", lambda e, nh4=nh4, tB=tB, sinb=sinb: e.tensor_tensor(out=tB[:, :, 1, :], in0=nh4[:, :, 0, :], in1=sinb.unsqueeze(1).broadcast_to([128, nh, 16]), op=ALU.mult),
                  reads=["wn", "ropeT", "wtB"], writes=["wtB"])
            S.add("dve", lambda e, oh4=oh4, tA=tA, tB=tB: e.tensor_tensor(out=oh4[:, :, 0, :], in0=tA[:, :, 0, :], in1=tB[:, :, 0, :], op=ALU.subtract),
                  reads=["wtA", "wtB"], writes=[tag + "r"])
            S.add("dve", lambda e, oh4=oh4, tA=tA, tB=tB: e.tensor_tensor(out=oh4[:, :, 1, :], in0=tA[:, :, 1, :], in1=tB[:, :, 1, :], op=ALU.add),
                  reads=["wtA", "wtB", tag + "r"], writes=[tag + "r"])

    for j in range(NT):
        s2 = j % 2
        dma("sp", qb[s2], q_s.ap()[j], reads=["q_s"], writes=[f"qb{s2}"], stream=f"ql{s2}")
        dma("sp", kvb[s2], kv_s.ap()[j], reads=["kv_s"], writes=[f"kvb{s2}"], stream=f"kl{s2}")
        normrope(qb[s2], 8, gq, "gq", j, qr, f"qb{s2}")
        for g in range(4):
            S.add("pe", lambda e, g=g: e.transpose(out=psb(0)[:, g * 128:(g + 1) * 128], in_=qr[:, g * 128:(g + 1) * 128], identity=I_b),
                  reads=[f"qb{s2}r", "identb"], writes=["ps0"])
        S.add("act", lambda e, j=j: e.activation(out=qT[:, :, j * 128:(j + 1) * 128], in_=psb(0)[:, 0:512].rearrange("p (g t) -> p g t", g=4), func=AF.Copy),
              reads=["ps0"], writes=["qT"])
        normrope(kvb[s2][:, 0:128], 2, gk, "gk", j, kr, f"kvb{s2}")
        S.add("pe", lambda e: e.transpose(out=psb(1)[:, 0:128], in_=kr, identity=I_b), reads=[f"kvb{s2}r", "identb"], writes=["ps1"])
        S.add("act", lambda e, j=j: e.activation(out=kTA[0:64, j * 128:(j + 1) * 128], in_=psb(1)[0:64, 0:128], func=AF.Copy), reads=["ps1"], writes=["kT"])
        S.add("act", lambda e, j=j: e.activation(out=kTB[64:128, j * 128:(j + 1) * 128], in_=psb(1)[64:128, 0:128], func=AF.Copy), reads=["ps1"], writes=["kT"])
        S.add("pool", lambda e, j=j, s2=s2: e.tensor_copy(out=Vx[:, j, :, 0:64], in_=kvb[s2][:, 128:256].rearrange("p (h d) -> p h d", h=2)),
              reads=[f"kvb{s2}"], writes=["Vx"])

    PT = [A.alloc([128, 512], BF16) for _ in range(4)]
    rc = A.alloc([128, 8], F32)
    oT = [A.alloc([65, 512], F32) for _ in range(2)]
    attst = [A.alloc([128, 4, 128], BF16) for _ in range(2)]
    iters = [(g, qg, kt, hh) for g in range(4) for qg in range(16) for kt in range(NT) for hh in range(2)]
    NI = len(iters)
    LA = 2

    def emit_S(i):
        g, qg, kt, hh = iters[i]
        sb_ = i % 4
        kTx = kTA if hh == 0 else kTB
        S.add("pe", lambda e: e.matmul(ps[sb_][:, :], lhsT=kTx[:, kt * 128:(kt + 1) * 128], rhs=qT[:, g, qg * 512:(qg + 1) * 512], start=True, stop=True),
              reads=["kT", "qT"], writes=[f"ps{sb_}"])
        S.add("act", lambda e: e.activation(out=PT[sb_], in_=ps[sb_][:, :], func=AF.Exp, scale=0.125), reads=[f"ps{sb_}"], writes=[f"PT{sb_}"])

    def emit_PV(i):
        g, qg, kt, hh = iters[i]
        sb_ = i % 4
        it_ = g * 16 + qg
        ob = 4 + 2 * (it_ % 2)
        S.add("pe", lambda e: e.matmul(ps[ob + hh][:, :], lhsT=Vx[:, kt, hh, :], rhs=PT[sb_], start=(kt == 0), stop=(kt == NT - 1)),
              reads=[f"PT{sb_}", "Vx"], writes=[f"ps{ob + hh}"])
        if kt == NT - 1 and hh == 1:
            a2 = it_ % 2
            for h2_ in range(2):
                S.add("act", lambda e, h2_=h2_: e.activation(out=oT[h2_], in_=ps[ob + h2_][0:65, :], func=AF.Copy), reads=[f"ps{ob + h2_}"], writes=[f"oT{h2_}"])
                for qs in range(4):
                    S.add("pe", lambda e, h2_=h2_, qs=qs: e.transpose(out=ps[ob + h2_][:, qs * 65:(qs + 1) * 65], in_=oT[h2_][0:65, qs * 128:(qs + 1) * 128], identity=identf[0:65, 0:65]),
                          reads=[f"oT{h2_}", "identf"], writes=[f"ps{ob + h2_}"])
                o4 = ps[ob + h2_][:, 0:260].rearrange("p (q d) -> p q d", q=4)
                S.add("dve", lambda e, o4=o4, h2_=h2_: e.reciprocal(out=rc[:, h2_ * 4:(h2_ + 1) * 4].unsqueeze(2), in_=o4[:, :, 64:65]), reads=[f"ps{ob + h2_}"], writes=["rc"])
                S.add("dve", lambda e, o4=o4, h2_=h2_: e.tensor_tensor(out=attst[a2][:, :, h2_ * 64:(h2_ + 1) * 64], in0=o4[:, :, 0:64],
                                                                     in1=rc[:, h2_ * 4:(h2_ + 1) * 4].unsqueeze(2).broadcast_to([128, 4, 64]), op=ALU.mult),
                      reads=[f"ps{ob + h2_}", "rc"], writes=[f"attst{a2}"])
            dma("sp", att_s.ap()[qg * 4:(qg + 1) * 4, :, g * 128:(g + 1) * 128].rearrange("t p c -> p t c"), attst[a2], reads=[f"attst{a2}"], writes=["att_s"], stream=f"as{a2}")

    for i in range(NI + LA):
        if i < NI:
            emit_S(i)
        if i - LA >= 0:
            emit_PV(i - LA)

    if stop_after == "D":
        S.emit()
        return nc, dbg_out

    S.barrier()
    A.reset()
    why_sb = A.alloc([128, 4, D], BF16)
    wat_sb = A.alloc([128, 4, D], BF16)
    wout_sb = A.alloc([128, 8, D], BF16)
    wr_sb = A.alloc([128, 8, 16], F32)
    A2row = A.alloc([128, D], F32)
    sh2row = A.alloc([128, D], F32)
    gfb = A.alloc([128, D], F32)
    dma("pool", why_sb, why_d.ap(), writes=["why_sb"], stream="wb0")
    dma("pool", wat_sb, wat_d.ap(), writes=["wat_sb"], stream="wb1")
    dma("pool", wout_sb, wout_d.ap(), writes=["wout_sb"], stream="wb0")
    dma("sp", wr_sb, wr_d.ap(), writes=["wr_sb"], stream="c0")
    dma("sp", A2row, mod_s.ap()[:, 4096:5120].partition_broadcast(128), reads=["mod_s"], writes=["A2row"], stream="c0")
    dma("sp", sh2row, mod_s.ap()[:, 3072:4096].partition_broadcast(128), reads=["mod_s"], writes=["sh2row"], stream="c0")
    dma("sp", gfb, gffn_d.ap().partition_broadcast(128), writes=["gfb"], stream="c0")
    S.add("dve", lambda e: e.scalar_tensor_tensor(out=A2row, in0=A2row, scalar=1.0, in1=gfb, op0=ALU.add, op1=ALU.mult), reads=["A2row", "gfb"], writes=["A2row"])
    hyb = [A.alloc([128, 512], BF16) for _ in range(2)]
    atb = [A.alloc([128, 512], BF16) for _ in range(2)]
    gtb = [A.alloc([128, 2048], BF16) for _ in range(2)]
    xe_ = [A.alloc([128, D], F32) for _ in range(2)]
    hatT_ = [A.alloc([128, 8, 128], BF16) for _ in range(2)]
    t1_ = [A.alloc([128, D], F32) for _ in range(2)]
    t2_ = [A.alloc([128, D], F32) for _ in range(2)]
    Pb_ = [A.alloc([128, D], BF16) for _ in range(2)]
    PTt_ = [A.alloc([128, 8, 128], BF16) for _ in range(2)]
    x1t_ = [A.alloc([128, D], F32) for _ in range(2)]
    junk2 = A.alloc([128, D], BF16)
    ss2 = A.alloc([128, 1], F32)
    xs2_ = [A.alloc([128, D], F32) for _ in range(2)]
    h2b_ = [A.alloc([128, D], BF16) for _ in range(2)]
    h2T_ = [A.alloc([128, 8, 128], F32) for _ in range(2)]
    mx = A.alloc([128, 1], F32)
    sm = A.alloc([128, 1], F32)
    ex = A.alloc([128, 16], F32)
    def efirst(j):
        s2 = j % 2
        dma("sp", hyb[s2], hy_s.ap()[j], reads=["hy_s"], writes=[f"hyb{s2}"], stream=f"e0{s2}")
        dma("sp", atb[s2], att_s.ap()[j], reads=["att_s"], writes=[f"atb{s2}"], stream=f"e1{s2}")
        dma("sp", gtb[s2], gt_s.ap()[j], reads=["gt_s"], writes=[f"gtb{s2}"], stream=f"e2{s2}")
        dma("sp", xe_[s2], x_d.ap()[j * 128:(j + 1) * 128, :], writes=[f"xe{s2}"], stream=f"e3{s2}")
        for cc in range(4):
            S.add("pe", lambda e, cc=cc: e.transpose(out=psb(0)[:, cc * 128:(cc + 1) * 128], in_=hyb[s2][:, cc * 128:(cc + 1) * 128], identity=J_b),
                  reads=[f"hyb{s2}", "identb"], writes=["ps0"])
        for cc in range(4):
            S.add("pe", lambda e, cc=cc: e.transpose(out=psb(0)[:, 512 + cc * 128:512 + (cc + 1) * 128], in_=atb[s2][:, cc * 128:(cc + 1) * 128], identity=I_b),
                  reads=[f"atb{s2}", "identb"], writes=["ps0"])
        S.add("act", lambda e: e.activation(out=hatT_[s2].rearrange("p k t -> p (k t)"), in_=psb(0)[:, :], func=AF.Copy), reads=["ps0"], writes=[f"hatT{s2}"])
        for h in range(2):
            for cc in range(4):
                S.add("pe", lambda e, h=h, cc=cc: e.matmul(ps[2 + h][:, :], lhsT=hatT_[s2][:, cc, :], rhs=why_sb[:, cc, h * 512:(h + 1) * 512], start=(cc == 0), stop=(cc == 3)),
                      reads=[f"hatT{s2}", "why_sb"], writes=[f"ps{2 + h}"])
            for cc in range(4):
                S.add("pe", lambda e, h=h, cc=cc: e.matmul(ps[4 + h][:, :], lhsT=hatT_[s2][:, 4 + cc, :], rhs=wat_sb[:, cc, h * 512:(h + 1) * 512], start=(cc == 0), stop=(cc == 3)),
                      reads=[f"hatT{s2}", "wat_sb"], writes=[f"ps{4 + h}"])
        for h in range(2):
            hs = slice(h * 512, (h + 1) * 512)
            S.add("dve", lambda e, h=h, hs=hs: e.tensor_tensor(out=t1_[s2][:, hs], in0=ps[2 + h][:, :], in1=gtb[s2][:, hs], op=ALU.mult), reads=[f"ps{2 + h}", f"gtb{s2}"], writes=[f"t1{s2}"])
            S.add("dve", lambda e, h=h, hs=hs: e.tensor_tensor(out=t2_[s2][:, hs], in0=ps[4 + h][:, :], in1=gtb[s2][:, 1024 + h * 512:1024 + (h + 1) * 512], op=ALU.mult),
                  reads=[f"ps{4 + h}", f"gtb{s2}"], writes=[f"t2{s2}"])
        S.add("pool", lambda e: e.tensor_tensor(out=Pb_[s2], in0=t1_[s2], in1=t2_[s2], op=ALU.add), reads=[f"t1{s2}", f"t2{s2}"], writes=[f"Pb{s2}"])
    def esecond(j):
        s2 = j % 2
        for kc in range(8):
            S.add("pe", lambda e, kc=kc: e.transpose(out=psb(1)[:, kc * 128:(kc + 1) * 128], in_=Pb_[s2][:, kc * 128:(kc + 1) * 128], identity=I_b),
                  reads=[f"Pb{s2}", "identb"], writes=["ps1"])
        S.add("act", lambda e: e.activation(out=PTt_[s2].rearrange("p k t -> p (k t)"), in_=psb(1)[:, :], func=AF.Copy), reads=["ps1"], writes=[f"PTt{s2}"])
        for h in range(2):
            for kc in range(8):
                S.add("pe", lambda e, h=h, kc=kc: e.matmul(ps[6 + h][:, :], lhsT=PTt_[s2][:, kc, :], rhs=wout_sb[:, kc, h * 512:(h + 1) * 512], start=(kc == 0), stop=(kc == 7)),
                      reads=[f"PTt{s2}", "wout_sb"], writes=[f"ps{6 + h}"])
        for h in range(2):
            hs = slice(h * 512, (h + 1) * 512)
            S.add("dve", lambda e, h=h, hs=hs: e.tensor_tensor(out=t1_[s2][:, hs], in0=ps[6 + h][:, :], in1=gt1b[:, hs], op=ALU.mult), reads=[f"ps{6 + h}", "gt1b"], writes=[f"t1{s2}"])
        S.add("pool", lambda e: e.tensor_tensor(out=x1t_[s2], in0=t1_[s2], in1=xe_[s2], op=ALU.add), reads=[f"t1{s2}", f"xe{s2}"], writes=[f"x1t{s2}"])
        dma("sp", out_d.ap()[j * 128:(j + 1) * 128, :], x1t_[s2], reads=[f"x1t{s2}"], writes=["out_d"], stream="eo")
        S.add("act", lambda e: e.activation(out=junk2, in_=x1t_[s2], func=AF.Square, accum_out=ss2), reads=[f"x1t{s2}"], writes=["junk2", "ss2"])
        S.add("act", lambda e: e.activation(out=ss2, in_=ss2, func=AF.Sqrt, scale=1.0 / D, bias=1e-6), reads=["ss2"], writes=["ss2"])
        S.add("dve", lambda e: e.reciprocal(out=ss2, in_=ss2), reads=["ss2"], writes=["ss2"])
        S.add("dve", lambda e: e.scalar_tensor_tensor(out=xs2_[s2], in0=x1t_[s2], scalar=ss2[:, 0:1], in1=A2row, op0=ALU.mult, op1=ALU.mult), reads=[f"x1t{s2}", "ss2", "A2row"], writes=[f"xs2{s2}"])
        S.add("pool", lambda e: e.tensor_tensor(out=h2b_[s2], in0=xs2_[s2], in1=sh2row, op=ALU.add), reads=[f"xs2{s2}", "sh2row"], writes=[f"h2b{s2}"])
        dma("sp", h2_s.ap()[j * 128:(j + 1) * 128, :], h2b_[s2], reads=[f"h2b{s2}"], writes=["h2_s"], stream="eh")
        S.add("pool", lambda e: e.tensor_tensor(out=xs2_[s2], in0=xs2_[s2], in1=sh2row, op=ALU.add), reads=[f"xs2{s2}", "sh2row", f"h2b{s2}"], writes=[f"xs2{s2}"])
        for hh in range(2):
            for kc in range(4):
                S.add("pe", lambda e, kc=kc, hh=hh: e.transpose(out=ps[6 + hh][:, kc * 128:(kc + 1) * 128], in_=xs2_[s2][:, (hh * 4 + kc) * 128:(hh * 4 + kc + 1) * 128], identity=identf[:, 0:128]),
                      reads=[f"xs2{s2}", "identf"], writes=[f"ps{6 + hh}"])
            S.add("act", lambda e, hh=hh: e.activation(out=h2T_[s2][:, hh * 4:(hh + 1) * 4, :].rearrange("p k t -> p (k t)"), in_=ps[6 + hh][:, :], func=AF.Copy), reads=[f"ps{6 + hh}"], writes=[f"h2T{s2}"])
        for kc in range(8):
            S.add("pe", lambda e, kc=kc: e.matmul(ps[1][:, 0:16], lhsT=h2T_[s2][:, kc, :], rhs=wr_sb[:, kc, :], start=(kc == 0), stop=(kc == 7)),
                  reads=[f"h2T{s2}", "wr_sb"], writes=["ps1"])
        S.add("dve", lambda e: e.tensor_reduce(out=mx, in_=ps[1][:, 0:16], axis=AX.X, op=ALU.max), reads=["ps1"], writes=["mx"])
        S.add("dve", lambda e: e.tensor_scalar(out=mx, in0=mx, scalar1=-1.0, scalar2=None, op0=ALU.mult), reads=["mx"], writes=["mx"])
        S.add("act", lambda e: e.activation(out=ex, in_=ps[1][:, 0:16], func=AF.Exp, bias=mx[:, 0:1], accum_out=sm), reads=["ps1", "mx"], writes=["ex", "sm"])
        S.add("dve", lambda e: e.reciprocal(out=sm, in_=sm), reads=["sm"], writes=["sm"])
        S.add("dve", lambda e, j=j: e.tensor_scalar(out=aff_sb[:, j, :], in0=ex, scalar1=sm[:, 0:1], scalar2=None, op0=ALU.mult), reads=["ex", "sm"], writes=["aff_sb"])
    efirst(0)
    for j in range(NT):
        if j + 1 < NT:
            efirst(j + 1)
        esecond(j)
    if "aff" in dbg:
        dbg_out["aff"] = nc.dram_tensor("dbg_aff", [128, NT, 16], F32, kind="ExternalOutput")
        dma("sp", dbg_out["aff"].ap(), aff_sb, reads=["aff_sb"], stream="c0")

    if stop_after == "E":
        S.emit()
        return nc, dbg_out

    S.barrier()
    A.reset()
    lo = A.alloc([128, 16], F32)
    hi = A.alloc([128, 16], F32)
    mid = A.alloc([128, 16], F32)
    cnt = A.alloc([128, 16], F32)
    sel = A.alloc([128, 16], F32)
    dl = A.alloc([128, 16], F32)
    dh_ = A.alloc([128, 16], F32)
    cmpb = A.alloc([128, NT, 16], F32)
    maskb = A.alloc([128, NT, 16], F32)
    S.add("dve", lambda e: e.memset(lo, 0.0), writes=["lo"])
    S.add("dve", lambda e: e.memset(hi, 1.0), writes=["hi"])
    for _ in range(30):
        S.add("dve", lambda e: e.tensor_tensor(out=mid, in0=lo, in1=hi, op=ALU.add), reads=["lo", "hi"], writes=["mid"])
        S.add("dve", lambda e: e.tensor_scalar(out=mid, in0=mid, scalar1=0.5, scalar2=None, op0=ALU.mult), reads=["mid"], writes=["mid"])
        S.add("dve", lambda e: e.tensor_tensor(out=cmpb, in0=aff_sb, in1=mid.unsqueeze(1).broadcast_to([128, NT, 16]), op=ALU.is_gt), reads=["aff_sb", "mid"], writes=["cmpb"])
        S.add("dve", lambda e: e.tensor_reduce(out=cnt, in_=cmpb.rearrange("p j e -> p e j"), axis=AX.X, op=ALU.add), reads=["cmpb"], writes=["cnt"])
        S.add("pe", lambda e: e.matmul(ps[0][:, 0:16], lhsT=ones_f, rhs=cnt, start=True, stop=True), reads=["cnt", "ones_f"], writes=["ps0"])
        S.add("dve", lambda e: e.tensor_single_scalar(out=sel, in_=ps[0][:, 0:16], scalar=1023.5, op=ALU.is_ge), reads=["ps0"], writes=["sel"])
        S.add("dve", lambda e: e.tensor_tensor(out=dl, in0=mid, in1=lo, op=ALU.subtract), reads=["mid", "lo"], writes=["dl"])
        S.add("dve", lambda e: e.tensor_tensor(out=dl, in0=dl, in1=sel, op=ALU.mult), reads=["dl", "sel"], writes=["dl"])
        S.add("dve", lambda e: e.tensor_tensor(out=dh_, in0=hi, in1=mid, op=ALU.subtract), reads=["mid", "hi"], writes=["dh"])
        S.add("dve", lambda e: e.tensor_tensor(out=dh_, in0=dh_, in1=sel, op=ALU.mult), reads=["dh", "sel"], writes=["dh"])
        S.add("dve", lambda e: e.tensor_tensor(out=lo, in0=lo, in1=dl, op=ALU.add), reads=["lo", "dl"], writes=["lo"])
        S.add("dve", lambda e: e.tensor_tensor(out=hi, in0=mid, in1=dh_, op=ALU.add), reads=["mid", "dh"], writes=["hi"])
    S.add("dve", lambda e: e.tensor_tensor(out=maskb, in0=aff_sb, in1=lo.unsqueeze(1).broadcast_to([128, NT, 16]), op=ALU.is_gt), reads=["aff_sb", "lo"], writes=["maskb"])
    tri = A.alloc([128, 128], F32)
    iota_sb = A.alloc([128, 1024], F32)
    tokid = A.alloc([128, NT], F32)
    csb = A.alloc([128, NT, 16], F32)
    totb = A.alloc([128, NT, 16], F32)
    sca = A.alloc([128, NT, 16], F32)
    scb = A.alloc([128, NT, 16], F32)
    pos = A.alloc([128, NT, 16], F32)
    dma("sp", tri, tri_d.ap(), writes=["tri"], stream="c0")
    dma("sp", iota_sb, iota_d.ap(), writes=["iota_sb"], stream="c0")
    dma("sp", tokid, tok_d.ap(), writes=["tokid"], stream="c0")
    mflat = maskb.rearrange("p j e -> p (j e)")
    for h in range(2):
        S.add("pe", lambda e, h=h: e.matmul(ps[2 + h][:, :], lhsT=tri, rhs=mflat[:, h * 512:(h + 1) * 512], start=True, stop=True), reads=["tri", "maskb"], writes=[f"ps{2 + h}"])
        S.add("pe", lambda e, h=h: e.matmul(ps[4 + h][:, :], lhsT=ones_f, rhs=mflat[:, h * 512:(h + 1) * 512], start=True, stop=True), reads=["ones_f", "maskb"], writes=[f"ps{4 + h}"])
        S.add("dve", lambda e, h=h: e.tensor_copy(out=csb.rearrange("p j e -> p (j e)")[:, h * 512:(h + 1) * 512], in_=ps[2 + h][:, :]), reads=[f"ps{2 + h}"], writes=["csb"])
        S.add("dve", lambda e, h=h: e.tensor_copy(out=totb.rearrange("p j e -> p (j e)")[:, h * 512:(h + 1) * 512], in_=ps[4 + h][:, :]), reads=[f"ps{4 + h}"], writes=["totb"])
    cur, curk, oth, othk = totb, "totb", sca, "sca"
    for sft in (1, 2, 4, 8, 16, 32):
        S.add("dve", lambda e, cur=cur, oth=oth, sft=sft: e.tensor_tensor(out=oth[:, sft:, :], in0=cur[:, sft:, :], in1=cur[:, 0:NT - sft, :], op=ALU.add), reads=[curk], writes=[othk])
        S.add("dve", lambda e, cur=cur, oth=oth, sft=sft: e.tensor_copy(out=oth[:, 0:sft, :], in_=cur[:, 0:sft, :]), reads=[curk, othk], writes=[othk])
        if cur is totb:
            cur, curk, oth, othk = sca, "sca", scb, "scb"
        else:
            cur, curk, oth, othk = oth, othk, cur, curk
    S.add("dve", lambda e, cur=cur: e.tensor_tensor(out=pos, in0=cur, in1=totb, op=ALU.subtract), reads=[curk, "totb"], writes=["pos"])
    S.add("dve", lambda e: e.tensor_tensor(out=pos, in0=pos, in1=csb, op=ALU.add), reads=["pos", "csb"], writes=["pos"])
    S.add("dve", lambda e: e.tensor_scalar(out=pos, in0=pos, scalar1=-1.0, scalar2=None, op0=ALU.add), reads=["pos"], writes=["pos"])
    idx_all = A.alloc([128, 16, 8], I32)
    w_all = A.alloc([128, 16, 8], F32)
    vals5 = [A.alloc([128, NT, 5], BF16) for _ in range(2)]
    r1 = A.alloc([128, NT], F32)
    r2 = A.alloc([128, NT], F32)
    a1f = A.alloc([128, NT], F32)
    oh = [A.alloc([128, 1024], BF16) for _ in range(4)]
    rows5 = A.alloc([5, 1024], F32)
    tokf = A.alloc([128, 8], F32)
    pvs = A.alloc([128, 40], F32)
    lof = A.alloc([128, 1], F32)
    S.add("dve", lambda e: e.tensor_scalar(out=lof, in0=tokid[:, 0:1], scalar1=1.0, scalar2=None, op0=ALU.mult), reads=["tokid"], writes=["lof"])
    for v2 in range(2):
        S.add("dve", lambda e, v2=v2: e.tensor_scalar(out=r1, in0=tokid, scalar1=lof[:, 0:1], scalar2=1.0 / 128, op0=ALU.subtract, op1=ALU.mult), reads=["tokid", "lof"], writes=["r1"])
        S.add("dve", lambda e, v2=v2: e.tensor_copy(out=vals5[v2][:, :, 0], in_=r1), reads=["r1"], writes=[f"vals{v2}"])
        S.add("dve", lambda e, v2=v2: e.tensor_copy(out=vals5[v2][:, :, 1], in_=lof[:, 0:1].broadcast_to([128, NT])), reads=["lof"], writes=[f"vals{v2}"])
    ohc = 0
    for ex_ in range(16):
        v2 = ex_ % 2
        bb = 4 + 2 * (ex_ % 2)
        V = vals5[v2]
        S.add("dve", lambda e, V=V, ex_=ex_: e.tensor_copy(out=V[:, :, 2], in_=aff_sb[:, :, ex_]), reads=["aff_sb"], writes=[f"vals{v2}"])
        S.add("dve", lambda e, V=V: e.tensor_copy(out=a1f, in_=V[:, :, 2]), reads=[f"vals{v2}"], writes=["a1f"])
        S.add("dve", lambda e, ex_=ex_: e.tensor_tensor(out=r1, in0=aff_sb[:, :, ex_], in1=a1f, op=ALU.subtract), reads=["aff_sb", "a1f"], writes=["r1"])
        S.add("dve", lambda e, V=V: e.tensor_copy(out=V[:, :, 3], in_=r1), reads=["r1"], writes=[f"vals{v2}"])
        S.add("dve", lambda e, V=V: e.tensor_copy(out=a1f, in_=V[:, :, 3]), reads=[f"vals{v2}"], writes=["a1f"])
        S.add("dve", lambda e: e.tensor_tensor(out=r2, in0=r1, in1=a1f, op=ALU.subtract), reads=["r1", "a1f"], writes=["r2"])
        S.add("dve", lambda e, V=V: e.tensor_copy(out=V[:, :, 4], in_=r2), reads=["r2"], writes=[f"vals{v2}"])
        for j in range(NT):
            so = ohc % 4
            eng_ = "dve"
            ohc += 1
            S.add(eng_, lambda e, so=so, j=j, ex_=ex_: e.tensor_scalar(out=oh[so], in0=iota_sb, scalar1=pos[:, j, ex_:ex_ + 1], scalar2=maskb[:, j, ex_:ex_ + 1],
                                                                     op0=ALU.is_equal, op1=ALU.mult),
                  reads=["iota_sb", "pos", "maskb"], writes=[f"oh{so}"])
            for h in range(2):
                S.add("pe", lambda e, so=so, j=j, h=h, bb=bb, V=V: e.matmul(ps[bb + h][0:5, :], lhsT=V[:, j, :], rhs=oh[so][:, h * 512:(h + 1) * 512],
                                                                        start=(j == 0), stop=(j == NT - 1)),
                      reads=[f"oh{so}", f"vals{v2}"], writes=[f"ps{bb + h}"])
        for h in range(2):
            S.add("act", lambda e, h=h, bb=bb: e.activation(out=rows5[:, h * 512:(h + 1) * 512], in_=ps[bb + h][0:5, :], func=AF.Copy), reads=[f"ps{bb + h}"], writes=["rows5"])
        for sc in range(8):
            S.add("pe", lambda e, sc=sc: e.transpose(out=ps[3][:, sc * 5:(sc + 1) * 5], in_=rows5[0:5, sc * 128:(sc + 1) * 128], identity=identf[0:5, 0:5]),
                  reads=["rows5", "identf"], writes=["ps3"])
        S.add("act", lambda e: e.activation(out=pvs, in_=ps[3][:, 0:40], func=AF.Copy), reads=["ps3"], writes=["pvs"])
        pv = pvs.rearrange("p (s t) -> p s t", t=5)
        S.add("dve", lambda e, pv=pv: e.scalar_tensor_tensor(out=tokf, in0=pv[:, :, 0], scalar=128.0, in1=pv[:, :, 1], op0=ALU.mult, op1=ALU.add), reads=["pvs"], writes=["tokf"])
        S.add("dve", lambda e, ex_=ex_: e.tensor_copy(out=idx_all[:, ex_, :], in_=tokf), reads=["tokf"], writes=["idx_all"])
        S.add("dve", lambda e, pv=pv, ex_=ex_: e.tensor_tensor(out=w_all[:, ex_, :], in0=pv[:, :, 2], in1=pv[:, :, 3], op=ALU.add), reads=["pvs"], writes=["w_all"])
        S.add("dve", lambda e, pv=pv, ex_=ex_: e.tensor_tensor(out=w_all[:, ex_, :], in0=w_all[:, ex_, :], in1=pv[:, :, 4], op=ALU.add), reads=["pvs", "w_all"], writes=["w_all"])
    if "idx" in dbg:
        dbg_out["idx"] = nc.dram_tensor("dbg_idx", [128, 16, 8], I32, kind="ExternalOutput")
        dbg_out["wsl"] = nc.dram_tensor("dbg_w", [128, 16, 8], F32, kind="ExternalOutput")
        dma("sp", dbg_out["idx"].ap(), idx_all, reads=["idx_all"], stream="c0")
        dma("sp", dbg_out["wsl"].ap(), w_all, reads=["w_all"], stream="c0")
    if stop_after == "F3":
        S.emit()
        return nc, dbg_out

    S.barrier()
    keep = A.off
    A.off = A.lo
    idx2 = A.alloc([128, 16, 8], I32)
    w2 = A.alloc([128, 16, 8], F32)
    S.add("dve", lambda e: e.tensor_copy(out=idx2, in_=idx_all), reads=["idx_all"], writes=["idx2"])
    S.add("dve", lambda e: e.tensor_copy(out=w2, in_=w_all), reads=["w_all"], writes=["w2"])
    S.barrier()
    wsl = [A.alloc([128, 8 * 2048], BF16) for _ in range(4)]
    xeT = A.alloc([128, 8, 1024], BF16)
    hidT = A.alloc([128, 16, 1024], BF16)
    xeg = [A.alloc([128, D], BF16) for _ in range(2)]
    ye = [A.alloc([128, D], F32) for _ in range(2)]
    sg = [A.alloc([128, 512], BF16) for _ in range(2)]
    wseq = [0]

    def wload(src_ap):
        k = wseq[0] % 4
        wseq[0] += 1
        flat = src_ap.rearrange("p a b -> p (a b)")
        for hh in range(2):
            dma("pool", wsl[k][:, hh * 8192:(hh + 1) * 8192], flat[:, hh * 8192:(hh + 1) * 8192], writes=[f"wsl{k}"], stream=f"w{k}{hh}")
        return k

    slots = {}
    slots[(0, "g")] = wload(wg_d.ap()[0])
    slots[(0, "u")] = wload(wu_d.ap()[0])
    slots[(0, "d")] = wload(wd_d.ap()[0])
    gcnt = 0
    ycnt = 0
    for ex_ in range(16):
        kg_, ku_, kd_ = slots[(ex_, "g")], slots[(ex_, "u")], slots[(ex_, "d")]
        wgv = wsl[kg_].rearrange("p (a b) -> p a b", a=8)
        wuv = wsl[ku_].rearrange("p (a b) -> p a b", a=8)
        wdv = wsl[kd_].rearrange("p (a b) -> p a b", a=16)
        for sc in range(8):
            xs_ = gcnt % 2
            gcnt += 1
            S.add("pool", lambda e, xs_=xs_, sc=sc, ex_=ex_: e.indirect_dma_start(out=xeg[xs_], out_offset=None, in_=h2_s.ap(),
                                                                               in_offset=bass.IndirectOffsetOnAxis(ap=idx2[:, ex_, sc:sc + 1], axis=0)),
                  reads=["idx2", "h2_s"], writes=[f"xeg{xs_}"], dma=f"gath{xs_}")
            for kc in range(8):
                S.add("pe", lambda e, xs_=xs_, kc=kc: e.transpose(out=psb(0)[:, kc * 128:(kc + 1) * 128], in_=xeg[xs_][:, kc * 128:(kc + 1) * 128], identity=I_b),
                      reads=[f"xeg{xs_}", "identb"], writes=["ps0"])
            S.add("act", lambda e, sc=sc: e.activation(out=xeT[:, :, sc * 128:(sc + 1) * 128], in_=psb(0)[:, :].rearrange("p (k t) -> p k t", k=8), func=AF.Copy),
                  reads=["ps0"], writes=["xeT"])
        if ex_ + 1 < 16:
            slots[(ex_ + 1, "g")] = wload(wg_d.ap()[ex_ + 1])
        for ffc in range(16):
            for h in range(2):
                bg, bu = 1 + (ffc * 2 + h) % 2, 3 + (ffc * 2 + h) % 2
                s_ = (ffc * 2 + h) % 2
                for kc in range(8):
                    S.add("pe", lambda e, bg=bg, kc=kc, ffc=ffc, h=h, wgv=wgv: e.matmul(ps[bg][:, :], lhsT=wgv[:, kc, ffc * 128:(ffc + 1) * 128], rhs=xeT[:, kc, h * 512:(h + 1) * 512],
                                                                                    start=(kc == 0), stop=(kc == 7)),
                          reads=[f"wsl{kg_}", "xeT"], writes=[f"ps{bg}"])
                for kc in range(8):
                    S.add("pe", lambda e, bu=bu, kc=kc, ffc=ffc, h=h, wuv=wuv: e.matmul(ps[bu][:, :], lhsT=wuv[:, kc, ffc * 128:(ffc + 1) * 128], rhs=xeT[:, kc, h * 512:(h + 1) * 512],
                                                                                    start=(kc == 0), stop=(kc == 7)),
                          reads=[f"wsl{ku_}", "xeT"], writes=[f"ps{bu}"])
                S.add("act", lambda e, bg=bg, s_=s_: e.activation(out=sg[s_], in_=ps[bg][:, :], func=AF.Silu), reads=[f"ps{bg}"], writes=[f"sg{s_}"])
                S.add("dve", lambda e, bu=bu, s_=s_, ffc=ffc, h=h: e.tensor_tensor(out=hidT[:, ffc, h * 512:(h + 1) * 512], in0=ps[bu][:, :], in1=sg[s_], op=ALU.mult),
                      reads=[f"ps{bu}", f"sg{s_}"], writes=["hidT"])
        if ex_ + 1 < 16:
            slots[(ex_ + 1, "u")] = wload(wu_d.ap()[ex_ + 1])
            slots[(ex_ + 1, "d")] = wload(wd_d.ap()[ex_ + 1])
        for sc in range(8):
            ys = ycnt % 2
            ycnt += 1
            for dh in range(2):
                bd = 5 + (sc * 2 + dh) % 3
                for ffc in range(16):
                    S.add("pe", lambda e, bd=bd, ffc=ffc, sc=sc, dh=dh, wdv=wdv: e.matmul(ps[bd][:, :], lhsT=hidT[:, ffc, sc * 128:(sc + 1) * 128], rhs=wdv[:, ffc, dh * 512:(dh + 1) * 512],
                                                                                      start=(ffc == 0), stop=(ffc == 15)),
                          reads=["hidT", f"wsl{kd_}"], writes=[f"ps{bd}"])
                S.add("dve", lambda e, bd=bd, ys=ys, dh=dh, sc=sc, ex_=ex_: e.scalar_tensor_tensor(out=ye[ys][:, dh * 512:(dh + 1) * 512], in0=ps[bd][:, :], scalar=w2[:, ex_, sc:sc + 1],
                                                                                                in1=gt2b[:, dh * 512:(dh + 1) * 512], op0=ALU.mult, op1=ALU.mult),
                      reads=[f"ps{bd}", "w2", "gt2b"], writes=[f"ye{ys}"])
            S.add("pool", lambda e, ys=ys, sc=sc, ex_=ex_: e.indirect_dma_start(out=out_d.ap(), out_offset=bass.IndirectOffsetOnAxis(ap=idx2[:, ex_, sc:sc + 1], axis=0),
                                                                             in_=ye[ys], in_offset=None, compute_op=ALU.add),
                  reads=[f"ye{ys}", "idx2", "out_d"], writes=["out_d"], dma="scat")

    S.emit()
    return nc, dbg_out


QPERM = np.array([1536 + (hh * 4 + g) * 64 + d for g in range(4) for hh in range(2) for d in range(64)])
COLPERM = np.concatenate([np.arange(0, 512), np.arange(1024, 1536), np.arange(512, 1024), QPERM, np.arange(2048, 4352)])
HPERM = COLPERM[:1536]
ATT_ROWPERM = QPERM - 1536


def consts():
    c = {}
    ident = np.zeros((128, 256), np.float32)
    ident[:, :128] = np.eye(128)
    ident[:, 128:] = np.eye(128)[::-1]
    c["ident"] = ident
    ind = np.ones((4, 5, 128), np.float32)
    ind[0, 1, 0] = 0
    ind[2, 2, 127] = 0
    ind[0, 3, 127] = 0
    ind[2, 4, 0] = 0
    c["ind"] = ind
    t01 = np.linspace(0.0, 1.0, L, dtype=np.float32)
    bands = 16
    f = np.linspace(1e-4, bands - 1, bands, dtype=np.float32)[None, :]
    w = (np.float32(2.0 * math.pi) * np.arange(L, dtype=np.float32)[:, None] / np.float32(L)).astype(np.float32)
    z = np.concatenate([t01[:, None], np.cos(f * w), -np.sin(f * w)], axis=-1).astype(np.float32)
    c["zT"] = np.ascontiguousarray(np.stack([z.T, z[::-1].T], axis=1))
    c["t01"] = np.ascontiguousarray(np.stack([t01, t01[::-1]], axis=0))
    max_decay = math.log(1e-2) / 0.3
    min_decay = math.log(1e-2) / 1.5
    deltas = np.abs(np.linspace(min_decay, max_decay, 512, dtype=np.float32))
    c["deltacol"] = np.ascontiguousarray((-deltas).reshape(4, 128).T)
    rows = L // 64
    row = np.repeat(np.arange(rows), 64).astype(np.float32)
    col = np.tile(np.arange(64), rows).astype(np.float32)
    inv = (10000.0 ** (-np.arange(0, 32, 2, dtype=np.float32) / 32)).astype(np.float32)
    ang = np.concatenate([row[:, None] * inv, col[:, None] * inv], axis=-1)
    rope = np.concatenate([np.cos(ang), np.sin(ang)], axis=-1).astype(np.float32)
    c["rope"] = np.ascontiguousarray(rope.reshape(NT, 128, 64).transpose(1, 0, 2))
    c["iota"] = np.tile(np.arange(1024, dtype=np.float32)[None], (128, 1))
    c["tokid"] = (np.arange(NT)[None, :] * 128 + np.arange(128)[:, None]).astype(np.float32)
    c["tri"] = np.triu(np.ones((128, 128), np.float32))
    return c


def prep_shared(inp):
    l = 0
    m = dict(consts())
    f32 = lambda a: np.ascontiguousarray(a, dtype=np.float32)
    m["wada"] = f32(inp["w_ada"][l].reshape(8, 128, 6144).transpose(1, 0, 2))
    m["bada"] = f32(inp["b_ada"][l][None])
    m["gmixcol"] = f32(inp["g_mix"][l].reshape(8, 128).T)
    m["gffnrow"] = f32(inp["g_ffn"][l][None])
    m["win"] = f32(inp["w_in"][l][:, COLPERM].reshape(8, 128, 4352).transpose(1, 0, 2))
    bin_p = inp["b_in"][l][COLPERM]
    m["binh"] = f32(bin_p[None, :1536])
    m["bino"] = f32(bin_p[None, 1536:])
    m["sw"] = f32(inp["short_w"][l][:, HPERM])
    m["sb"] = f32(inp["short_b"][l][HPERM][None])
    m["hw1"] = f32(inp["hy_w1"][l]); m["hw2"] = f32(inp["hy_w2"][l]); m["hw3"] = f32(inp["hy_w3"][l]); m["hw4"] = f32(inp["hy_w4"][l])
    m["hbcol"] = f32(np.stack([inp["hy_b1"][l], inp["hy_b2"][l], inp["hy_b3"][l]], axis=1))
    m["hfrcol"] = f32(inp["hy_freq"][l].T)
    m["fbcol"] = f32(inp["hy_bias"][l].reshape(2, 4, 128).transpose(2, 0, 1).reshape(128, 8))
    m["qgain"] = f32(inp["q_gain"][l][None]); m["kgain"] = f32(inp["k_gain"][l][None])
    m["why"] = f32(inp["w_hy_out"][l].reshape(4, 128, D).transpose(1, 0, 2))
    m["wat"] = f32(inp["w_att_out"][l][ATT_ROWPERM].reshape(4, 128, D).transpose(1, 0, 2))
    m["wout"] = f32(inp["w_out"][l].reshape(8, 128, D).transpose(1, 0, 2))
    m["wr"] = f32(inp["w_router"][l].reshape(8, 128, 16).transpose(1, 0, 2))
    m["wg"] = f32(inp["w_gate"][l].reshape(16, 8, 128, 2048).transpose(0, 2, 1, 3))
    m["wu"] = f32(inp["w_up"][l].reshape(16, 8, 128, 2048).transpose(0, 2, 1, 3))
    m["wd"] = f32(inp["w_down"][l].reshape(16, 16, 128, D).transpose(0, 2, 1, 3))
    return m


def prep(inp, b, shared=None):
    m = dict(shared if shared is not None else prep_shared(inp))
    m["x"] = np.ascontiguousarray(inp["x"][b], dtype=np.float32)
    m["ccol"] = np.ascontiguousarray(inp["c"][b].reshape(8, 128).T, dtype=np.float32)
    return m


_CACHE = {}


REAL_CORES = (0, 1, 4, 5)


def kernel(**inputs):
    inp = {k: np.asarray(v) for k, v in inputs.items()}
    if "nc" not in _CACHE:
        _CACHE["nc"] = build("F")[0]
    nc = _CACHE["nc"]
    shared = prep_shared(inp)
    real = {core: prep(inp, b, shared) for b, core in enumerate(REAL_CORES)}
    ck = set(consts().keys())
    zero = {k: (v if k in ck else np.zeros_like(v)) for k, v in real[0].items()}
    maps = [real.get(core, zero) for core in range(8)]
    res = run_bass_kernel_spmd(nc, maps, core_ids=list(range(8)))
    return np.stack([res.results[core]["out"] for core in REAL_CORES], axis=0).astype(np.float32)
```

```python
import math
import numpy as np
import concourse.bass as bass
import concourse.mybir as mybir
from concourse.bass_utils import run_bass_kernel_spmd

F32 = mybir.dt.float32
BF16 = mybir.dt.bfloat16
I32 = mybir.dt.int32
AF = mybir.ActivationFunctionType
ALU = mybir.AluOpType
AX = mybir.AxisListType

L = 8192
D = 1024
NT = 64
ENGS = ("pe", "act", "dve", "pool", "sp")


class _Op:
    __slots__ = ("eng", "fn", "deps", "needs_inc", "dma", "inc_idx", "dma_val", "epoch")

    def __init__(self, eng, fn, dma):
        self.eng = eng
        self.fn = fn
        self.deps = []
        self.needs_inc = False
        self.dma = dma
        self.inc_idx = 0
        self.dma_val = 0
        self.epoch = 0


class Sched:
    def __init__(self, nc):
        self.nc = nc
        self.ops = {e: [] for e in ENGS}
        self.last_w = {}
        self.readers = {}
        self.dma_cnt = {}
        self.dma_last = {}
        self.barrier_deps = []
        self.epoch = 0

    def add(self, eng, fn, reads=(), writes=(), dma=None):
        op = _Op(eng, fn, dma)
        op.epoch = self.epoch
        deps = []
        for k in reads:
            w = self.last_w.get(k)
            if w is not None:
                deps.append(w)
        for k in writes:
            w = self.last_w.get(k)
            if w is not None:
                deps.append(w)
            deps.extend(self.readers.get(k, {}).values())
        deps.extend(self.barrier_deps)
        if dma is not None:
            p = self.dma_last.get(dma)
            if p is not None:
                deps.append(p)
            self.dma_cnt[dma] = self.dma_cnt.get(dma, 0) + 16
            op.dma_val = self.dma_cnt[dma]
            self.dma_last[dma] = op
        seen = set()
        for d in deps:
            if id(d) in seen:
                continue
            seen.add(id(d))
            if d.dma is None and d.eng == "pe" and eng == "pe" and dma is None:
                continue
            op.deps.append(d)
            if d.dma is None:
                d.needs_inc = True
        rk = ("dma", dma) if dma is not None else ("eng", eng)
        for k in reads:
            self.readers.setdefault(k, {})[rk] = op
        for k in writes:
            self.last_w[k] = op
            self.readers[k] = {}
        self.ops[eng].append(op)
        return op

    def barrier(self):
        deps = []
        for e in ENGS:
            for op in reversed(self.ops[e]):
                if op.dma is None:
                    deps.append(op)
                    break
        deps.extend(self.dma_last.values())
        self.barrier_deps = deps
        self.epoch += 1

    def emit(self):
        nc = self.nc
        self.barrier()
        fin = _Op("sp", None, None)
        for d in self.barrier_deps:
            fin.deps.append(d)
            if d.dma is None:
                d.needs_inc = True
        self.ops["sp"].append(fin)
        esem = {}
        for e in ENGS:
            c = {}
            for op in self.ops[e]:
                if op.dma is None and op.needs_inc:
                    c[op.epoch] = c.get(op.epoch, 0) + 1
                    op.inc_idx = c[op.epoch]
                    if (e, op.epoch) not in esem:
                        esem[(e, op.epoch)] = nc.alloc_semaphore(name=f"s_{e}_{op.epoch}")
        dsem = {s: nc.alloc_semaphore(name=f"d_{s}") for s in self.dma_cnt}
        ops = self.ops

        def run(eng_name, eng):
            waited = {}
            for op in ops[eng_name]:
                for d in op.deps:
                    if d.dma is not None:
                        key, sem, val = ("d", d.dma), dsem[d.dma], d.dma_val
                    else:
                        key, sem, val = ("e", d.eng, d.epoch), esem[(d.eng, d.epoch)], d.inc_idx
                    if waited.get(key, 0) >= val:
                        continue
                    waited[key] = val
                    eng.wait_ge(sem, val)
                if op.fn is None:
                    continue
                ins = op.fn(eng)
                if op.dma is not None:
                    ins.then_inc(dsem[op.dma], 16)
                elif op.needs_inc:
                    ins.then_inc(esem[(eng_name, op.epoch)], 1)

        block = nc.Block()
        with block:
            @block.tensor
            def _(e):
                run("pe", e)

            @block.scalar
            def _(e):
                run("act", e)

            @block.vector
            def _(e):
                run("dve", e)

            @block.gpsimd
            def _(e):
                run("pool", e)

            @block.sync
            def _(e):
                run("sp", e)


class Arena:
    def __init__(self, big, lo, hi):
        self.big, self.lo, self.hi = big, lo, hi
        self.off = lo

    def reset(self):
        self.off = self.lo

    def alloc(self, shape, dt):
        n = int(np.prod(shape[1:]))
        esz = 4 if dt in (F32, I32) else 2
        nb = (n * esz + 31) // 32 * 32
        assert self.off + nb <= self.hi, ("SBUF arena overflow", shape, self.off, nb, self.hi)
        a = self.big[0:shape[0], self.off // 4:(self.off + nb) // 4]
        self.off += nb
        if dt != F32:
            a = a.bitcast(dt)
        a = a[:, 0:n]
        if len(shape) > 2:
            names = " ".join(f"d{i}" for i in range(1, len(shape)))
            a = a.rearrange(f"p ({names}) -> p {names}", **{f"d{i}": shape[i] for i in range(1, len(shape))})
        return a


def build(stop_after="F", dbg=()):
    nc = bass.Bass("TRN2", target_bir_lowering=False)
    S = Sched(nc)
    dbg_out = {}

    def din(name, shape, dt=F32):
        return nc.dram_tensor(name, list(shape), dt, kind="ExternalInput")

    def dscr(name, shape, dt):
        if name in dbg:
            t = nc.dram_tensor(name, list(shape), dt, kind="ExternalOutput")
            dbg_out[name] = t
            return t
        return nc.dram_tensor(name, list(shape), dt)

    x_d = din("x", [L, D])
    ccol_d = din("ccol", [128, 8])
    wada_d = din("wada", [128, 8, 6144])
    bada_d = din("bada", [1, 6144])
    gmix_d = din("gmixcol", [128, 8])
    gffn_d = din("gffnrow", [1, D])
    win_d = din("win", [128, 8, 4352])
    binh_d = din("binh", [1, 1536])
    bino_d = din("bino", [1, 2816])
    sw_d = din("sw", [3, 1536])
    sb_d = din("sb", [1, 1536])
    ind_d = din("ind", [4, 5, 128])
    ident_d = din("ident", [128, 256])
    zT_d = din("zT", [33, 2, L])
    t01_d = din("t01", [2, L])
    delta_d = din("deltacol", [128, 4])
    hw1_d = din("hw1", [33, 64])
    hw2_d = din("hw2", [64, 64])
    hw3_d = din("hw3", [64, 64])
    hw4_d = din("hw4", [64, 2048])
    hb_d = din("hbcol", [64, 3])
    hfr_d = din("hfrcol", [64, 3])
    fb_d = din("fbcol", [128, 8])
    rope_d = din("rope", [128, NT, 64])
    qg_d = din("qgain", [1, 64])
    kg_d = din("kgain", [1, 64])
    why_d = din("why", [128, 4, D])
    wat_d = din("wat", [128, 4, D])
    wout_d = din("wout", [128, 8, D])
    wr_d = din("wr", [128, 8, 16])
    wg_d = din("wg", [16, 128, 8, 2048])
    wu_d = din("wu", [16, 128, 8, 2048])
    wd_d = din("wd", [16, 128, 16, D])
    iota_d = din("iota", [128, 1024])
    tok_d = din("tokid", [128, NT])
    tri_d = din("tri", [128, 128])
    out_d = nc.dram_tensor("out", [L, D], F32, kind="ExternalOutput")

    mod_s = dscr("mod_s", [1, 6144], F32)
    v_s = dscr("v_s", [NT, 128, 512], BF16)
    x2_s = dscr("x2_s", [NT, 128, 512], BF16)
    x1_s = dscr("x1_s", [NT, 128, 512], BF16)
    q_s = dscr("q_s", [NT, 128, 512], BF16)
    kv_s = dscr("kv_s", [NT, 128, 256], BF16)
    gt_s = dscr("gt_s", [NT, 128, 2048], BF16)
    a_s = dscr("a_s", [2, 512, 16384], BF16)
    hy_s = dscr("hy_s", [NT, 128, 512], BF16)
    att_s = dscr("att_s", [NT, 128, 512], BF16)
    h2_s = dscr("h2_s", [L, D], BF16)

    NB = nc.sbuf_bytes_remaining
    big = nc.alloc_sbuf_tensor("big", [128, (NB - 128) // 4], F32)
    TOT = (NB - 128) // 4 * 4
    PERS = 14848
    P = Arena(big, 0, PERS)
    A = Arena(big, PERS, TOT)
    ps = [nc.alloc_psum_tensor(f"ps{i}", [128, 512], F32) for i in range(8)]

    def psb(i):
        return ps[i][:].bitcast(BF16)

    def dma(q, out, in_, reads=(), writes=(), stream=None, slow=False):
        if slow:
            return S.add(q, lambda e: e.dma_start(out=out, in_=in_, allow_slow_non_contiguous=True), reads=reads, writes=writes, dma=stream)
        return S.add(q, lambda e: e.dma_start(out=out, in_=in_), reads=reads, writes=writes, dma=stream)

    identf = P.alloc([128, 256], F32)
    identb = P.alloc([128, 256], BF16)
    modcol = P.alloc([128, 48], F32)
    gt1b = P.alloc([128, D], F32)
    gt2b = P.alloc([128, D], F32)
    A1col = P.alloc([128, 8], F32)
    gmixc = P.alloc([128, 8], F32)
    ones_b = P.alloc([128, 128], BF16)
    ones_f = P.alloc([128, 128], F32)
    aff_sb = P.alloc([128, NT, 16], F32)
    dma("sp", identf, ident_d.ap(), writes=["identf"], stream="c0")
    S.add("dve", lambda e: e.tensor_copy(out=identb, in_=identf), reads=["identf"], writes=["identb"])
    S.add("dve", lambda e: e.memset(ones_b, 1.0), writes=["ones_b"])
    S.add("dve", lambda e: e.memset(ones_f, 1.0), writes=["ones_f"])
    I_b = identb[:, 0:128]
    J_b = identb[:, 128:256]

    ccol = A.alloc([128, 8], F32)
    sil = A.alloc([128, 8], F32)
    modrow = A.alloc([1, 6144], F32)
    badar = A.alloc([1, 6144], F32)
    wa = A.alloc([128, 8, 2048], F32)
    dma("sp", ccol, ccol_d.ap(), writes=["ccol"], stream="c0")
    dma("sp", badar, bada_d.ap(), writes=["badar"], stream="c0")
    dma("sp", gmixc, gmix_d.ap(), writes=["gmixc"], stream="c0")
    S.add("act", lambda e: e.activation(out=sil, in_=ccol, func=AF.Silu), reads=["ccol"], writes=["sil"])
    for ch in range(3):
        dma("sp", wa, wada_d.ap()[:, :, ch * 2048:(ch + 1) * 2048], writes=["wa"], stream="wa")
        for n in range(4):
            for kc in range(8):
                S.add("pe", lambda e, n=n, kc=kc: e.matmul(ps[n][0:1, :], lhsT=sil[:, kc:kc + 1], rhs=wa[:, kc, n * 512:(n + 1) * 512],
                                                         start=(kc == 0), stop=(kc == 7)),
                      reads=["sil", "wa"], writes=[f"ps{n}"])
            c0 = ch * 2048 + n * 512
            S.add("dve", lambda e, n=n, c0=c0: e.tensor_tensor(out=modrow[:, c0:c0 + 512], in0=ps[n][0:1, :], in1=badar[:, c0:c0 + 512], op=ALU.add),
                  reads=[f"ps{n}", "badar"], writes=["modrow"])
    dma("sp", mod_s.ap(), modrow, reads=["modrow"], writes=["mod_s"], stream="c0")
    dma("sp", modcol, mod_s.ap().rearrange("o (q p) -> p (o q)", p=128), reads=["mod_s"], writes=["modcol"], stream="c0", slow=True)
    dma("sp", gt1b, mod_s.ap()[:, 2048:3072].partition_broadcast(128), reads=["mod_s"], writes=["gt1b"], stream="c0")
    dma("sp", gt2b, mod_s.ap()[:, 5120:6144].partition_broadcast(128), reads=["mod_s"], writes=["gt2b"], stream="c0")
    S.add("dve", lambda e: e.scalar_tensor_tensor(out=A1col, in0=modcol[:, 8:16], scalar=1.0, in1=gmixc, op0=ALU.add, op1=ALU.mult),
          reads=["modcol", "gmixc"], writes=["A1col"])
    sh1col = modcol[:, 0:8]
    if "mod" in dbg:
        dbg_out["mod"] = nc.dram_tensor("dbg_mod", [1, 6144], F32, kind="ExternalOutput")
        dma("sp", dbg_out["mod"].ap(), modrow, reads=["modrow"], stream="c0")

    S.barrier()
    A.reset()
    wb = A.alloc([128, 8, 4352], BF16)
    wh0 = A.alloc([128, 8, 1536], BF16)
    wh2 = A.alloc([128, 8, 1536], BF16)
    swb_lo = A.off
    swb = A.alloc([128, 3, 1536], BF16)
    swb_hi = A.off
    swf = A.alloc([3, 1536], F32)
    bh3 = A.alloc([4, 1536], F32)
    brow4 = A.alloc([4, 1536], BF16)
    bof = A.alloc([1, 2816], F32)
    bob = A.alloc([1, 2816], BF16)
    indf = A.alloc([4, 5, 128], F32)
    indb = A.alloc([4, 5, 128], BF16)
    for kc in range(8):
        dma("pool", wb[:, kc, :], win_d.ap()[:, kc, :], writes=["wb"], stream=f"wb{kc % 2}")
    dma("pool", swb.rearrange("p a c -> p (a c)"), sw_d.ap().rearrange("a c -> (a c)").partition_broadcast(128), writes=["swb"], stream="wb0")
    dma("sp", swf, sw_d.ap(), writes=["swf"], stream="c0")
    for r in range(3):
        dma("sp", bh3[r:r + 1, :], binh_d.ap(), writes=["bh3"], stream="c0")
    dma("sp", bh3[3:4, :], sb_d.ap(), writes=["bh3"], stream="c0")
    dma("sp", bof, bino_d.ap(), writes=["bof"], stream="c0")
    dma("sp", indf, ind_d.ap(), writes=["indf"], stream="c0")
    S.add("dve", lambda e: e.tensor_tensor(out=bh3[0:3, :], in0=bh3[0:3, :], in1=swf, op=ALU.mult), reads=["bh3", "swf"], writes=["bh3"])
    S.add("dve", lambda e: e.tensor_copy(out=brow4, in_=bh3), reads=["bh3"], writes=["brow4"])
    S.add("dve", lambda e: e.tensor_copy(out=bob, in_=bof), reads=["bof"], writes=["bob"])
    S.add("dve", lambda e: e.tensor_copy(out=indb, in_=indf), reads=["indf"], writes=["indb"])
    for kc in range(8):
        S.add("dve", lambda e, kc=kc: e.tensor_tensor(out=wh0[:, kc, :], in0=wb[:, kc, 0:1536], in1=swb[:, 0, :], op=ALU.mult),
              reads=["wb", "swb"], writes=["wh0"])
        S.add("pool", lambda e, kc=kc: e.tensor_tensor(out=wh2[:, kc, :], in0=wb[:, kc, 0:1536], in1=swb[:, 2, :], op=ALU.mult),
              reads=["wb", "swb"], writes=["wh2"])
    for kc in range(8):
        S.add("dve", lambda e, kc=kc: e.tensor_tensor(out=wb[:, kc, 0:1536], in0=wb[:, kc, 0:1536], in1=swb[:, 1, :], op=ALU.mult),
              reads=["wb", "swb", "wh0", "wh2"], writes=["wb"])
    whs = [wh0, wb, wh2]

    NSL = 4
    S.barrier()
    A2_ = Arena(big, swb_lo, swb_hi)
    xt = [A.alloc([128, D], F32)] * 3
    junk = A.alloc([128, D], BF16)
    ssq = [A.alloc([128, 1], F32) for _ in range(2)] + [A2_.alloc([128, 1], F32)]
    xb = [A.alloc([128, D], BF16) for _ in range(2)] + [A2_.alloc([128, D], BF16)]
    hN = [A.alloc([128, 8, 130], BF16) for _ in range(3)] + [A2_.alloc([128, 8, 130], BF16)]
    hR = [A.alloc([128, 8, 130], BF16) for _ in range(3)] + [A2_.alloc([128, 8, 130], BF16)]
    stg = [A.alloc([128, 4352], BF16)] * 2
    for s in range(NSL):
        S.add("dve", lambda e, s=s: e.memset(hN[s], 0.0), writes=[f"hN{s}"])
        S.add("dve", lambda e, s=s: e.memset(hR[s], 0.0), writes=[f"hR{s}"])

    def stage1(j):
        s2, s3 = j % 3, j % NSL
        dma("sp", xt[s2], x_d.ap()[j * 128:(j + 1) * 128, :], writes=["xt"], stream="x0")
        S.add("act", lambda e: e.activation(out=junk, in_=xt[s2], func=AF.Square, accum_out=ssq[s2]), reads=["xt"], writes=["junk", f"ssq{s2}"])
        S.add("act", lambda e: e.activation(out=ssq[s2], in_=ssq[s2], func=AF.Sqrt, scale=1.0 / D, bias=1e-6), reads=[f"ssq{s2}"], writes=[f"ssq{s2}"])
        S.add("dve", lambda e: e.reciprocal(out=ssq[s2], in_=ssq[s2]), reads=[f"ssq{s2}"], writes=[f"ssq{s2}"])
        S.add("dve", lambda e: e.tensor_scalar(out=xb[s2], in0=xt[s2], scalar1=ssq[s2][:, 0:1], scalar2=None, op0=ALU.mult),
              reads=["xt", f"ssq{s2}"], writes=[f"xb{s2}"])
        for kc in range(8):
            S.add("pe", lambda e, kc=kc: e.transpose(out=psb(0)[:, kc * 128:(kc + 1) * 128], in_=xb[s2][:, kc * 128:(kc + 1) * 128], identity=I_b),
                  reads=[f"xb{s2}", "identb"], writes=["ps0"])
        for kc in range(8):
            S.add("pe", lambda e, kc=kc: e.transpose(out=psb(1)[:, kc * 128:(kc + 1) * 128], in_=xb[s2][:, kc * 128:(kc + 1) * 128], identity=J_b),
                  reads=[f"xb{s2}", "identb"], writes=["ps1"])
        for kc in range(8):
            S.add("act", lambda e, kc=kc: e.activation(out=hN[s3][:, kc, 1:129], in_=psb(0)[:, kc * 128:(kc + 1) * 128], func=AF.Identity,
                                                        scale=A1col[:, kc:kc + 1], bias=sh1col[:, kc:kc + 1]),
                  reads=["ps0", "A1col", "modcol"], writes=[f"hN{s3}"])
            S.add("act", lambda e, kc=kc: e.activation(out=hR[s3][:, kc, 1:129], in_=psb(1)[:, kc * 128:(kc + 1) * 128], func=AF.Identity,
                                                        scale=A1col[:, kc:kc + 1], bias=sh1col[:, kc:kc + 1]),
                  reads=["ps1", "A1col", "modcol"], writes=[f"hR{s3}"])
        if j > 0:
            sp = (j - 1) % NSL
            S.add("dve", lambda e: e.tensor_copy(out=hN[sp][:, :, 129:130], in_=hN[s3][:, :, 1:2]), reads=[f"hN{s3}"], writes=[f"hN{sp}"])
            S.add("dve", lambda e: e.tensor_copy(out=hN[s3][:, :, 0:1], in_=hN[sp][:, :, 128:129]), reads=[f"hN{sp}"], writes=[f"hN{s3}"])
            S.add("dve", lambda e: e.tensor_copy(out=hR[sp][:, :, 0:1], in_=hR[s3][:, :, 128:129]), reads=[f"hR{s3}"], writes=[f"hR{sp}"])
            S.add("dve", lambda e: e.tensor_copy(out=hR[s3][:, :, 129:130], in_=hR[sp][:, :, 1:2]), reads=[f"hR{sp}"], writes=[f"hR{s3}"])
        else:
            S.add("dve", lambda e: e.memset(hN[s3][:, :, 0:1], 0.0), writes=[f"hN{s3}"])
            S.add("dve", lambda e: e.memset(hR[s3][:, :, 129:130], 0.0), writes=[f"hR{s3}"])
        if j == NT - 1:
            S.add("dve", lambda e: e.memset(hN[s3][:, :, 129:130], 0.0), writes=[f"hN{s3}"])
            S.add("dve", lambda e: e.memset(hR[s3][:, :, 0:1], 0.0), writes=[f"hR{s3}"])

    bank_rr = [2]

    def nextbank():
        b = bank_rr[0]
        bank_rr[0] = 2 + (b - 2 + 1) % 6
        return b

    def inproj(j):
        s3, s2 = j % NSL, j % 2
        iN = 1 if j == 0 else (2 if j == NT - 1 else 0)
        iR = 3 if j == 0 else (4 if j == NT - 1 else 0)
        offR = [2, 1, 0]
        offN = [0, 1, 2]
        groups = []
        for h in range(2):
            groups.append(("R", h * 512, 512))
        groups.append(("N", 1024, 512))
        for c0, w in ((1536, 512), (2048, 256), (2304, 512), (2816, 512), (3328, 512), (3840, 512)):
            groups.append(("O", c0, w))
        for gi, (kind, c0, w) in enumerate(groups):
            b = nextbank()
            key = f"ps{b}"
            if kind in ("R", "N"):
                src, srck, offs, ii = (hR[s3], f"hR{s3}", offR, iR) if kind == "R" else (hN[s3], f"hN{s3}", offN, iN)
                first = True
                for sh in range(3):
                    for kc in range(8):
                        S.add("pe", lambda e, b=b, sh=sh, kc=kc, src=src, offs=offs, first=first, c0=c0, w=w:
                              e.matmul(ps[b][:, 0:w], lhsT=src[:, kc, offs[sh]:offs[sh] + 128], rhs=whs[sh][:, kc, c0:c0 + w], start=first, stop=False),
                              reads=[srck, "wb", "wh0", "wh2"], writes=[key])
                        first = False
                S.add("pe", lambda e, b=b, ii=ii, c0=c0, w=w: e.matmul(ps[b][:, 0:w], lhsT=indb[:, ii, :], rhs=brow4[:, c0:c0 + w], start=False, stop=True),
                      reads=["indb", "brow4"], writes=[key])
            else:
                for kc in range(8):
                    S.add("pe", lambda e, b=b, kc=kc, c0=c0, w=w: e.matmul(ps[b][:, 0:w], lhsT=hN[s3][:, kc, 1:129], rhs=wb[:, kc, c0:c0 + w], start=(kc == 0), stop=False),
                          reads=[f"hN{s3}", "wb"], writes=[key])
                S.add("pe", lambda e, b=b, c0=c0, w=w: e.matmul(ps[b][:, 0:w], lhsT=ones_b[0:1, :], rhs=bob[:, c0 - 1536:c0 - 1536 + w], start=False, stop=True),
                      reads=["ones_b", "bob"], writes=[key])
            if c0 >= 2304:
                S.add("act", lambda e, b=b, c0=c0, w=w: e.activation(out=stg[s2][:, c0:c0 + w], in_=ps[b][:, 0:w], func=AF.Sigmoid),
                      reads=[key], writes=["stg"])
            else:
                S.add("dve", lambda e, b=b, c0=c0, w=w: e.tensor_copy(out=stg[s2][:, c0:c0 + w], in_=ps[b][:, 0:w]),
                      reads=[key], writes=["stg"])
        st = stg[s2]
        for dst, c0, w in ((v_s, 0, 512), (x2_s, 512, 512), (x1_s, 1024, 512), (q_s, 1536, 512), (kv_s, 2048, 256), (gt_s, 2304, 2048)):
            dma("sp", dst.ap()[j], st[:, c0:c0 + w], reads=["stg"], writes=[dst.name], stream="st0")

    stage1(0)
    stage1(1)
    for j in range(NT):
        if j + 2 < NT:
            stage1(j + 2)
        inproj(j)

    if stop_after == "B":
        S.emit()
        return nc, dbg_out

    S.barrier()
    A.reset()
    TWO_PI = 2.0 * math.pi
    w1f = A.alloc([33, 64], F32)
    w2f = A.alloc([64, 64], F32)
    w3f = A.alloc([64, 64], F32)
    w4f = A.alloc([64, 2048], F32)
    hbc = A.alloc([64, 3], F32)
    hfc = A.alloc([64, 3], F32)
    hoff = A.alloc([64, 3], F32)
    fbc = A.alloc([128, 8], F32)
    dlc = A.alloc([128, 4], F32)
    l1 = A.alloc([128, 2], F32)
    cen = A.alloc([128, 2], F32)
    negpi = A.alloc([128, 1], F32)
    h3 = [A.alloc([64, L], F32) for _ in range(2)]
    zTs = A.alloc([33, L], F32)
    h1s = A.alloc([64, L], F32)
    h2s = A.alloc([64, L], F32)
    for t, d_ in ((w1f, hw1_d), (w2f, hw2_d), (w3f, hw3_d), (w4f, hw4_d), (hbc, hb_d), (hfc, hfr_d), (fbc, fb_d), (dlc, delta_d)):
        dma("sp", t, d_.ap(), writes=["fw"], stream="c0")
    S.add("dve", lambda e: e.memset(negpi, -math.pi), writes=["negpi"])
    hfs = A.alloc([64, 3], F32)
    S.add("dve", lambda e: e.tensor_scalar(out=hfs, in0=hfc, scalar1=1.0 / TWO_PI, scalar2=None, op0=ALU.mult), reads=["fw"], writes=["hfs"])
    S.add("dve", lambda e: e.tensor_tensor(out=hoff, in0=hbc, in1=hfs, op=ALU.mult), reads=["fw", "hfs"], writes=["hoff"])
    tmpc = [A.alloc([64, 512], F32) for _ in range(2)]
    tmpi = [A.alloc([64, 512], I32) for _ in range(2)]
    tmpf = [A.alloc([64, 512], F32) for _ in range(2)]
    for o2 in range(2):
        dma("sp", zTs, zT_d.ap()[:, o2, :], writes=["zTs"], stream="c0")
        layers = ((w1f, zTs, "zTs", h1s, "h1s", 33), (w2f, h1s, "h1s", h2s, "h2s", 64), (w3f, h2s, "h2s", h3[o2], f"h3{o2}", 64))
        for li, (wf, src, srck, dst, dstk, kk) in enumerate(layers):
            for ch in range(16):
                b = ch % 4
                tb = ch % 2
                sl = slice(ch * 512, (ch + 1) * 512)
                S.add("pe", lambda e, b=b, wf=wf, src=src, sl=sl, kk=kk: e.matmul(ps[b][0:64, :], lhsT=wf[0:kk, :], rhs=src[0:kk, sl], start=True, stop=True),
                      reads=["fw", srck], writes=[f"ps{b}"])
                S.add("dve", lambda e, b=b, tb=tb, li=li: e.tensor_scalar(out=tmpc[tb], in0=ps[b][0:64, :], scalar1=hfs[:, li:li + 1], scalar2=hoff[:, li:li + 1],
                                                                         op0=ALU.mult, op1=ALU.add),
                      reads=[f"ps{b}", "hfs", "hoff"], writes=[f"tmpc{tb}"])
                S.add("dve", lambda e, tb=tb: e.tensor_copy(out=tmpi[tb], in_=tmpc[tb]), reads=[f"tmpc{tb}"], writes=[f"tmpi{tb}"])
                S.add("pool", lambda e, tb=tb: e.tensor_copy(out=tmpf[tb], in_=tmpi[tb]), reads=[f"tmpi{tb}"], writes=[f"tmpf{tb}"])
                S.add("pool", lambda e, tb=tb: e.tensor_tensor(out=tmpf[tb], in0=tmpc[tb], in1=tmpf[tb], op=ALU.subtract), reads=[f"tmpc{tb}", f"tmpf{tb}"], writes=[f"tmpf{tb}"])
                S.add("act", lambda e, tb=tb, dst=dst, sl=sl: e.activation(out=dst[:, sl], in_=tmpf[tb], func=AF.Sin, scale=TWO_PI - 1e-6),
                      reads=[f"tmpf{tb}"], writes=[dstk])
    if "h3" in dbg:
        dbg_out["h3"] = nc.dram_tensor("dbg_h3", [2, 64, L], F32, kind="ExternalOutput")
        for o2 in range(2):
            dma("sp", dbg_out["h3"].ap()[o2], h3[o2], reads=[f"h3{o2}"], stream="c0")
    S.barrier()
    A.off -= (33 + 64 + 64) * 0
    A.off = A.off - 3 * L * 4 - 6 * 512 * 4 - 32
    wnd = A.alloc([128, L], F32)
    hfull = A.alloc([128, L], F32)
    Abuf = A.alloc([128, 16384], BF16)
    S.add("dve", lambda e: e.memset(Abuf[:, 16382:16384], 0.0), writes=["Abuf"])
    for o in range(2):
        for g in range(4):
            parts = ((1, 1 - o), (0, o))
            for pi_, (o2, dr) in enumerate(parts):
                col0 = o * 1024 + dr * 512 + g * 128
                dma("sp", wnd, t01_d.ap()[o2:o2 + 1, :].partition_broadcast(128), writes=["wnd"], stream="c0")
                S.add("act", lambda e, g=g: e.activation(out=wnd, in_=wnd, func=AF.Exp, scale=dlc[:, g:g + 1]), reads=["wnd", "fw"], writes=["wnd"])
                for ch in range(16):
                    b = ch % 4
                    sl = slice(ch * 512, (ch + 1) * 512)
                    S.add("pe", lambda e, b=b, col0=col0, o2=o2, sl=sl: e.matmul(ps[b][:, :], lhsT=w4f[:, col0:col0 + 128], rhs=h3[o2][:, sl], start=True, stop=True),
                          reads=["fw", f"h3{o2}"], writes=[f"ps{b}"])
                    S.add("dve", lambda e, b=b, sl=sl: e.tensor_tensor(out=hfull[:, sl], in0=ps[b][:, :], in1=wnd[:, sl], op=ALU.mult),
                          reads=[f"ps{b}", "wnd"], writes=["hfull"])
                S.add("act", lambda e, pi_=pi_: e.activation(out=wnd, in_=hfull, func=AF.Abs, accum_out=l1[:, pi_:pi_ + 1]), reads=["hfull"], writes=["wnd", "l1"])
                S.add("dve", lambda e, pi_=pi_: e.tensor_scalar(out=l1[:, pi_:pi_ + 1], in0=l1[:, pi_:pi_ + 1], scalar1=1e-6, scalar2=None, op0=ALU.add), reads=["l1"], writes=["l1"])
                S.add("dve", lambda e, pi_=pi_: e.reciprocal(out=l1[:, pi_:pi_ + 1], in_=l1[:, pi_:pi_ + 1]), reads=["l1"], writes=["l1"])
                if pi_ == 0:
                    S.add("dve", lambda e: e.tensor_scalar(out=Abuf[:, 0:8192], in0=hfull, scalar1=l1[:, 0:1], scalar2=None, op0=ALU.mult), reads=["hfull", "l1"], writes=["Abuf"])
                    S.add("dve", lambda e: e.tensor_scalar(out=cen[:, 0:1], in0=hfull[:, 8191:8192], scalar1=l1[:, 0:1], scalar2=None, op0=ALU.mult), reads=["hfull", "l1"], writes=["cen"])
                else:
                    S.add("dve", lambda e: e.tensor_scalar(out=Abuf[:, 8191:16383], in0=hfull, scalar1=l1[:, 1:2], scalar2=None, op0=ALU.mult), reads=["hfull", "l1"], writes=["Abuf"])
                    S.add("dve", lambda e: e.tensor_scalar(out=cen[:, 1:2], in0=hfull[:, 0:1], scalar1=l1[:, 1:2], scalar2=None, op0=ALU.mult), reads=["hfull", "l1"], writes=["cen"])
            fcol = o * 4 + g
            S.add("dve", lambda e: e.tensor_tensor(out=cen[:, 0:1], in0=cen[:, 0:1], in1=cen[:, 1:2], op=ALU.add), reads=["cen"], writes=["cen"])
            S.add("dve", lambda e, fcol=fcol: e.tensor_tensor(out=Abuf[:, 8191:8192], in0=cen[:, 0:1], in1=fbc[:, fcol:fcol + 1], op=ALU.add), reads=["cen", "fw", "Abuf"], writes=["Abuf"])
            dma("sp", a_s.ap()[o, g * 128:(g + 1) * 128, :], Abuf, reads=["Abuf"], writes=["a_s"], stream="c0")

    if stop_after == "A2":
        S.emit()
        return nc, dbg_out

    S.barrier()
    A.reset()
    NSTRIP = 3
    SW = 127 * 128
    vg = A.alloc([128, NT, 128], BF16)
    x1g = A.alloc([128, NT, 128], BF16)
    x2g = A.alloc([128, NT, 128], BF16)
    wgb = A.alloc([128, NT, 128], BF16)
    hyo = A.alloc([128, NT, 128], BF16)
    strips = [A.alloc([128, SW], BF16) for _ in range(NSTRIP)]
    sctr = [0]
    cb = [0]
    dlist = [0] + [d for d in range(-63, 64) if d != 0]

    def conv(o, g, src, srck, gate, gatek, dst, dstk):
        for c in range(128):
            si = sctr[0] % NSTRIP
            sctr[0] += 1
            ch = g * 128 + c
            base = a_s.ap()[o, ch:ch + 1, 0:1]
            hk = bass.AP(a_s, base.offset, [[1, 128], [1, SW]])
            dma("sp", strips[si], hk, reads=["a_s"], writes=[f"strip{si}"], stream=f"strip{si}")
            b = 4 + cb[0] % 4
            cb[0] += 1
            for n_, d in enumerate(dlist):
                i0, i1 = max(0, d), min(64, 64 + d)
                off = 128 * (d + 63) if o == 0 else 128 * (63 - d)
                S.add("pe", lambda e, b=b, si=si, off=off, i0=i0, i1=i1, d=d, c=c, n_=n_:
                      e.matmul(ps[b][:, i0:i1], lhsT=strips[si][:, off:off + 128], rhs=src[:, i0 - d:i1 - d, c], start=(n_ == 0), stop=(n_ == 126)),
                      reads=[f"strip{si}", srck], writes=[f"ps{b}"])
            S.add("dve", lambda e, b=b, c=c: e.tensor_tensor(out=dst[:, :, c], in0=ps[b][:, 0:64], in1=gate[:, :, c], op=ALU.mult),
                  reads=[f"ps{b}", gatek], writes=[dstk])

    for g in range(4):
        cs = slice(g * 128, (g + 1) * 128)
        dma("sp", vg, v_s.ap()[:, :, cs].rearrange("j p c -> p j c"), reads=["v_s"], writes=["vg"], stream="hl0")
        dma("sp", x1g, x1_s.ap()[:, :, cs].rearrange("j p c -> p j c"), reads=["x1_s"], writes=["x1g"], stream="hl1")
        dma("sp", x2g, x2_s.ap()[:, :, cs].rearrange("j p c -> p j c"), reads=["x2_s"], writes=["x2g"], stream="hl2")
        conv(0, g, vg, "vg", x1g, "x1g", wgb, "wgb")
        conv(1, g, wgb, "wgb", x2g, "x2g", hyo, "hyo")
        dma("sp", hy_s.ap()[:, :, cs].rearrange("j p c -> p j c"), hyo, reads=["hyo"], writes=["hy_s"], stream="hl3")

    if stop_after == "C":
        S.emit()
        return nc, dbg_out

    S.barrier()
    A.reset()
    qT = A.alloc([128, 4, L], BF16)
    kTA = A.alloc([128, L], BF16)
    kTB = A.alloc([128, L], BF16)
    Vx = A.alloc([128, NT, 2, 128], BF16)
    ropeT = A.alloc([128, NT, 64], F32)
    gq = A.alloc([128, 64], F32)
    gk = A.alloc([128, 64], F32)
    dma("sp", ropeT, rope_d.ap(), writes=["ropeT"], stream="c0")
    dma("sp", gq, qg_d.ap().partition_broadcast(128), writes=["gq"], stream="c0")
    dma("sp", gk, kg_d.ap().partition_broadcast(128), writes=["gk"], stream="c0")
    S.add("pool", lambda e: e.memset(Vx, 0.0), writes=["Vx"])
    S.add("pool", lambda e: e.memset(Vx[:, :, :, 64:65], 1.0), writes=["Vx"])
    S.add("dve", lambda e: e.memset(kTA[64:128, :], 0.0), writes=["kT"])
    S.add("dve", lambda e: e.memset(kTB[0:64, :], 0.0), writes=["kT"])
    qb = [A.alloc([128, 512], BF16) for _ in range(2)]
    kvb = [A.alloc([128, 256], BF16) for _ in range(2)]
    wf_ = A.alloc([128, 512], F32)
    wsq = A.alloc([128, 512], F32)
    wss = A.alloc([128, 8], F32)
    wn = A.alloc([128, 512], F32)
    wtA = A.alloc([128, 256], F32)
    wtB = A.alloc([128, 256], F32)
    qr = A.alloc([128, 512], BF16)
    kr = A.alloc([128, 128], BF16)

    def normrope(srcv, nh, gain, gaink, j, outv, tag):
        W = nh * 64
        f, sq_, ss_, n_ = wf_[:, 0:W], wsq[:, 0:W], wss[:, 0:nh], wn[:, 0:W]
        f3 = f.rearrange("p (h d) -> p h d", h=nh)
        n3 = n_.rearrange("p (h d) -> p h d", h=nh)
        S.add("dve", lambda e: e.tensor_copy(out=f, in_=srcv), reads=[tag], writes=["wf"])
        S.add("dve", lambda e: e.tensor_tensor(out=sq_, in0=f, in1=f, op=ALU.mult), reads=["wf"], writes=["wsq"])
        S.add("dve", lambda e: e.tensor_reduce(out=ss_, in_=sq_.rearrange("p (h d) -> p h d", h=nh), axis=AX.X, op=ALU.add), reads=["wsq"], writes=["wss"])
        S.add("act", lambda e: e.activation(out=ss_, in_=ss_, func=AF.Sqrt, scale=1.0 / 64, bias=1e-6), reads=["wss"], writes=["wss"])
        S.add("dve", lambda e: e.reciprocal(out=ss_, in_=ss_), reads=["wss"], writes=["wss"])
        S.add("dve", lambda e: e.tensor_tensor(out=n3, in0=f3, in1=ss_.unsqueeze(2).broadcast_to([128, nh, 64]), op=ALU.mult), reads=["wf", "wss"], writes=["wn"])
        S.add("dve", lambda e: e.tensor_tensor(out=n3, in0=n3, in1=gain.unsqueeze(1).broadcast_to([128, nh, 64]), op=ALU.mult), reads=["wn", gaink], writes=["wn"])
        o3 = outv.rearrange("p (h d) -> p h d", h=nh)
        for hf in range(2):
            nh4 = n3[:, :, hf * 32:(hf + 1) * 32].rearrange("p h (u d) -> p h u d", u=2)
            oh4 = o3[:, :, hf * 32:(hf + 1) * 32].rearrange("p h (u d) -> p h u d", u=2)
            cosb = ropeT[:, j, hf * 16:(hf + 1) * 16]
            sinb = ropeT[:, j, 32 + hf * 16:32 + (hf + 1) * 16]
            tA = wtA[:, 0:nh * 32].rearrange("p (h u d) -> p h u d", h=nh, u=2)
            tB = wtB[:, 0:nh * 32].rearrange("p (h u d) -> p h u d", h=nh, u=2)
            S.add("dve", lambda e, nh4=nh4, tA=tA, cosb=cosb: e.tensor_tensor(out=tA, in0=nh4, in1=cosb.unsqueeze(1).unsqueeze(1).broadcast_to([128, nh, 2, 16]), op=ALU.mult),
                  reads=["wn", "ropeT"], writes=["wtA"])
            S.add("dve", lambda e, nh4=nh4, tB=tB, sinb=sinb: e.tensor_tensor(out=tB[:, :, 0, :], in0=nh4[:, :, 1, :], in1=sinb.unsqueeze(1).broadcast_to([128, nh, 16]), op=ALU.mult),
                  reads=["wn", "ropeT"], writes=["wtB"])
            S.add("dve", lambda e, nh4=nh4, tB=tB, sinb=sinb: e.tensor_tensor(out=tB[:, :, 1, :], in0=nh4[:, :, 0, :], in1=sinb.unsqueeze(1).broadcast_to([128, nh, 16]), op=ALU.mult),
                  reads=["wn", "ropeT", "wtB"], writes=["wtB"])
            S.add("dve", lambda e, oh4=oh4, tA=tA, tB=tB: e.tensor_tensor(out=oh4[:, :, 0, :], in0=tA[:, :, 0, :], in1=tB[:, :, 0, :], op=ALU.subtract),
                  reads=["wtA", "wtB"], writes=[tag + "r"])
            S.add("dve", lambda e, oh4=oh4, tA=tA, tB=tB: e.tensor_tensor(out=oh4[:, :, 1, :], in0=tA[:, :, 1, :], in1=tB[:, :, 1, :], op=ALU.add),
                  reads=["wtA", "wtB", tag + "r"], writes=[tag + "r"])

    for j in range(NT):
        s2 = j % 2
        dma("sp", qb[s2], q_s.ap()[j], reads=["q_s"], writes=[f"qb{s2}"], stream=f"ql{s2}")
        dma("sp", kvb[s2], kv_s.ap()[j], reads=["kv_s"], writes=[f"kvb{s2}"], stream=f"kl{s2}")
        normrope(qb[s2], 8, gq, "gq", j, qr, f"qb{s2}")
        for g in range(4):
            S.add("pe", lambda e, g=g: e.transpose(out=psb(0)[:, g * 128:(g + 1) * 128], in_=qr[:, g * 128:(g + 1) * 128], identity=I_b),
                  reads=[f"qb{s2}r", "identb"], writes=["ps0"])
        S.add("act", lambda e, j=j: e.activation(out=qT[:, :, j * 128:(j + 1) * 128], in_=psb(0)[:, 0:512].rearrange("p (g t) -> p g t", g=4), func=AF.Copy),
              reads=["ps0"], writes=["qT"])
        normrope(kvb[s2][:, 0:128], 2, gk, "gk", j, kr, f"kvb{s2}")
        S.add("pe", lambda e: e.transpose(out=psb(1)[:, 0:128], in_=kr, identity=I_b), reads=[f"kvb{s2}r", "identb"], writes=["ps1"])
        S.add("act", lambda e, j=j: e.activation(out=kTA[0:64, j * 128:(j + 1) * 128], in_=psb(1)[0:64, 0:128], func=AF.Copy), reads=["ps1"], writes=["kT"])
        S.add("act", lambda e, j=j: e.activation(out=kTB[64:128, j * 128:(j + 1) * 128], in_=psb(1)[64:128, 0:128], func=AF.Copy), reads=["ps1"], writes=["kT"])
        S.add("pool", lambda e, j=j, s2=s2: e.tensor_copy(out=Vx[:, j, :, 0:64], in_=kvb[s2][:, 128:256].rearrange("p (h d) -> p h d", h=2)),
              reads=[f"kvb{s2}"], writes=["Vx"])

    PT = [A.alloc([128, 512], BF16) for _ in range(4)]
    rc = A.alloc([128, 8], F32)
    oT = [A.alloc([65, 512], F32) for _ in range(2)]
    attst = [A.alloc([128, 4, 128], BF16) for _ in range(2)]
    iters = [(g, qg, kt, hh) for g in range(4) for qg in range(16) for kt in range(NT) for hh in range(2)]
    NI = len(iters)
    LA = 2

    def emit_S(i):
        g, qg, kt, hh = iters[i]
        sb_ = i % 4
        kTx = kTA if hh == 0 else kTB
        S.add("pe", lambda e: e.matmul(ps[sb_][:, :], lhsT=kTx[:, kt * 128:(kt + 1) * 128], rhs=qT[:, g, qg * 512:(qg + 1) * 512], start=True, stop=True),
              reads=["kT", "qT"], writes=[f"ps{sb_}"])
        S.add("act", lambda e: e.activation(out=PT[sb_], in_=ps[sb_][:, :], func=AF.Exp, scale=0.125), reads=[f"ps{sb_}"], writes=[f"PT{sb_}"])

    def emit_PV(i):
        g, qg, kt, hh = iters[i]
        sb_ = i % 4
        it_ = g * 16 + qg
        ob = 4 + 2 * (it_ % 2)
        S.add("pe", lambda e: e.matmul(ps[ob + hh][:, :], lhsT=Vx[:, kt, hh, :], rhs=PT[sb_], start=(kt == 0), stop=(kt == NT - 1)),
              reads=[f"PT{sb_}", "Vx"], writes=[f"ps{ob + hh}"])
        if kt == NT - 1 and hh == 1:
            a2 = it_ % 2
            for h2_ in range(2):
                S.add("act", lambda e, h2_=h2_: e.activation(out=oT[h2_], in_=ps[ob + h2_][0:65, :], func=AF.Copy), reads=[f"ps{ob + h2_}"], writes=[f"oT{h2_}"])
                for qs in range(4):
                    S.add("pe", lambda e, h2_=h2_, qs=qs: e.transpose(out=ps[ob + h2_][:, qs * 65:(qs + 1) * 65], in_=oT[h2_][0:65, qs * 128:(qs + 1) * 128], identity=identf[0:65, 0:65]),
                          reads=[f"oT{h2_}", "identf"], writes=[f"ps{ob + h2_}"])
                o4 = ps[ob + h2_][:, 0:260].rearrange("p (q d) -> p q d", q=4)
                S.add("dve", lambda e, o4=o4, h2_=h2_: e.reciprocal(out=rc[:, h2_ * 4:(h2_ + 1) * 4].unsqueeze(2), in_=o4[:, :, 64:65]), reads=[f"ps{ob + h2_}"], writes=["rc"])
                S.add("dve", lambda e, o4=o4, h2_=h2_: e.tensor_tensor(out=attst[a2][:, :, h2_ * 64:(h2_ + 1) * 64], in0=o4[:, :, 0:64],
                                                                     in1=rc[:, h2_ * 4:(h2_ + 1) * 4].unsqueeze(2).broadcast_to([128, 4, 64]), op=ALU.mult),
                      reads=[f"ps{ob + h2_}", "rc"], writes=[f"attst{a2}"])
            dma("sp", att_s.ap()[qg * 4:(qg + 1) * 4, :, g * 128:(g + 1) * 128].rearrange("t p c -> p t c"), attst[a2], reads=[f"attst{a2}"], writes=["att_s"], stream=f"as{a2}")

    for i in range(NI + LA):
        if i < NI:
            emit_S(i)
        if i - LA >= 0:
            emit_PV(i - LA)

    if stop_after == "D":
        S.emit()
        return nc, dbg_out

    S.barrier()
    A.reset()
    why_sb = A.alloc([128, 4, D], BF16)
    wat_sb = A.alloc([128, 4, D], BF16)
    wout_sb = A.alloc([128, 8, D], BF16)
    wr_sb = A.alloc([128, 8, 16], F32)
    A2row = A.alloc([128, D], F32)
    sh2row = A.alloc([128, D], F32)
    gfb = A.alloc([128, D], F32)
    dma("pool", why_sb, why_d.ap(), writes=["why_sb"], stream="wb0")
    dma("pool", wat_sb, wat_d.ap(), writes=["wat_sb"], stream="wb1")
    dma("pool", wout_sb, wout_d.ap(), writes=["wout_sb"], stream="wb0")
    dma("sp", wr_sb, wr_d.ap(), writes=["wr_sb"], stream="c0")
    dma("sp", A2row, mod_s.ap()[:, 4096:5120].partition_broadcast(128), reads=["mod_s"], writes=["A2row"], stream="c0")
    dma("sp", sh2row, mod_s.ap()[:, 3072:4096].partition_broadcast(128), reads=["mod_s"], writes=["sh2row"], stream="c0")
    dma("sp", gfb, gffn_d.ap().partition_broadcast(128), writes=["gfb"], stream="c0")
    S.add("dve", lambda e: e.scalar_tensor_tensor(out=A2row, in0=A2row, scalar=1.0, in1=gfb, op0=ALU.add, op1=ALU.mult), reads=["A2row", "gfb"], writes=["A2row"])
    hyb = [A.alloc([128, 512], BF16) for _ in range(2)]
    atb = [A.alloc([128, 512], BF16) for _ in range(2)]
    gtb = [A.alloc([128, 2048], BF16) for _ in range(2)]
    xe_ = [A.alloc([128, D], F32) for _ in range(2)]
    hatT_ = [A.alloc([128, 8, 128], BF16) for _ in range(2)]
    t1_ = [A.alloc([128, D], F32) for _ in range(2)]
    t2_ = [A.alloc([128, D], F32) for _ in range(2)]
    Pb_ = [A.alloc([128, D], BF16) for _ in range(2)]
    PTt_ = [A.alloc([128, 8, 128], BF16) for _ in range(2)]
    x1t_ = [A.alloc([128, D], F32) for _ in range(2)]
    junk2 = A.alloc([128, D], BF16)
    ss2 = A.alloc([128, 1], F32)
    xs2_ = [A.alloc([128, D], F32) for _ in range(2)]
    h2b_ = [A.alloc([128, D], BF16) for _ in range(2)]
    h2T_ = [A.alloc([128, 8, 128], F32) for _ in range(2)]
    mx = A.alloc([128, 1], F32)
    sm = A.alloc([128, 1], F32)
    ex = A.alloc([128, 16], F32)
    def efirst(j):
        s2 = j % 2
        dma("sp", hyb[s2], hy_s.ap()[j], reads=["hy_s"], writes=[f"hyb{s2}"], stream=f"e0{s2}")
        dma("sp", atb[s2], att_s.ap()[j], reads=["att_s"], writes=[f"atb{s2}"], stream=f"e1{s2}")
        dma("sp", gtb[s2], gt_s.ap()[j], reads=["gt_s"], writes=[f"gtb{s2}"], stream=f"e2{s2}")
        dma("sp", xe_[s2], x_d.ap()[j * 128:(j + 1) * 128, :], writes=[f"xe{s2}"], stream=f"e3{s2}")
        for cc in range(4):
            S.add("pe", lambda e, cc=cc: e.transpose(out=psb(0)[:, cc * 128:(cc + 1) * 128], in_=hyb[s2][:, cc * 128:(cc + 1) * 128], identity=J_b),
                  reads=[f"hyb{s2}", "identb"], writes=["ps0"])
        for cc in range(4):
            S.add("pe", lambda e, cc=cc: e.transpose(out=psb(0)[:, 512 + cc * 128:512 + (cc + 1) * 128], in_=atb[s2][:, cc * 128:(cc + 1) * 128], identity=I_b),
                  reads=[f"atb{s2}", "identb"], writes=["ps0"])
        S.add("act", lambda e: e.activation(out=hatT_[s2].rearrange("p k t -> p (k t)"), in_=psb(0)[:, :], func=AF.Copy), reads=["ps0"], writes=[f"hatT{s2}"])
        for h in range(2):
            for cc in range(4):
                S.add("pe", lambda e, h=h, cc=cc: e.matmul(ps[2 + h][:, :], lhsT=hatT_[s2][:, cc, :], rhs=why_sb[:, cc, h * 512:(h + 1) * 512], start=(cc == 0), stop=(cc == 3)),
                      reads=[f"hatT{s2}", "why_sb"], writes=[f"ps{2 + h}"])
            for cc in range(4):
                S.add("pe", lambda e, h=h, cc=cc: e.matmul(ps[4 + h][:, :], lhsT=hatT_[s2][:, 4 + cc, :], rhs=wat_sb[:, cc, h * 512:(h + 1) * 512], start=(cc == 0), stop=(cc == 3)),
                      reads=[f"hatT{s2}", "wat_sb"], writes=[f"ps{4 + h}"])
        for h in range(2):
            hs = slice(h * 512, (h + 1) * 512)
            S.add("dve", lambda e, h=h, hs=hs: e.tensor_tensor(out=t1_[s2][:, hs], in0=ps[2 + h][:, :], in1=gtb[s2][:, hs], op=ALU.mult), reads=[f"ps{2 + h}", f"gtb{s2}"], writes=[f"t1{s2}"])
            S.add("dve", lambda e, h=h, hs=hs: e.tensor_tensor(out=t2_[s2][:, hs], in0=ps[4 + h][:, :], in1=gtb[s2][:, 1024 + h * 512:1024 + (h + 1) * 512], op=ALU.mult),
                  reads=[f"ps{4 + h}", f"gtb{s2}"], writes=[f"t2{s2}"])
        S.add("pool", lambda e: e.tensor_tensor(out=Pb_[s2], in0=t1_[s2], in1=t2_[s2], op=ALU.add), reads=[f"t1{s2}", f"t2{s2}"], writes=[f"Pb{s2}"])
    def esecond(j):
        s2 = j % 2
        for kc in range(8):
            S.add("pe", lambda e, kc=kc: e.transpose(out=psb(1)[:, kc * 128:(kc + 1) * 128], in_=Pb_[s2][:, kc * 128:(kc + 1) * 128], identity=I_b),
                  reads=[f"Pb{s2}", "identb"], writes=["ps1"])
        S.add("act", lambda e: e.activation(out=PTt_[s2].rearrange("p k t -> p (k t)"), in_=psb(1)[:, :], func=AF.Copy), reads=["ps1"], writes=[f"PTt{s2}"])
        for h in range(2):
            for kc in range(8):
                S.add("pe", lambda e, h=h, kc=kc: e.matmul(ps[6 + h][:, :], lhsT=PTt_[s2][:, kc, :], rhs=wout_sb[:, kc, h * 512:(h + 1) * 512], start=(kc == 0), stop=(kc == 7)),
                      reads=[f"PTt{s2}", "wout_sb"], writes=[f"ps{6 + h}"])
        for h in range(2):
            hs = slice(h * 512, (h + 1) * 512)
            S.add("dve", lambda e, h=h, hs=hs: e.tensor_tensor(out=t1_[s2][:, hs], in0=ps[6 + h][:, :], in1=gt1b[:, hs], op=ALU.mult), reads=[f"ps{6 + h}", "gt1b"], writes=[f"t1{s2}"])
        S.add("pool", lambda e: e.tensor_tensor(out=x1t_[s2], in0=t1_[s2], in1=xe_[s2], op=ALU.add), reads=[f"t1{s2}", f"xe{s2}"], writes=[f"x1t{s2}"])
        dma("sp", out_d.ap()[j * 128:(j + 1) * 128, :], x1t_[s2], reads=[f"x1t{s2}"], writes=["out_d"], stream="eo")
        S.add("act", lambda e: e.activation(out=junk2, in_=x1t_[s2], func=AF.Square, accum_out=ss2), reads=[f"x1t{s2}"], writes=["junk2", "ss2"])
        S.add("act", lambda e: e.activation(out=ss2, in_=ss2, func=AF.Sqrt, scale=1.0 / D, bias=1e-6), reads=["ss2"], writes=["ss2"])
        S.add("dve", lambda e: e.reciprocal(out=ss2, in_=ss2), reads=["ss2"], writes=["ss2"])
        S.add("dve", lambda e: e.scalar_tensor_tensor(out=xs2_[s2], in0=x1t_[s2], scalar=ss2[:, 0:1], in1=A2row, op0=ALU.mult, op1=ALU.mult), reads=[f"x1t{s2}", "ss2", "A2row"], writes=[f"xs2{s2}"])
        S.add("pool", lambda e: e.tensor_tensor(out=h2b_[s2], in0=xs2_[s2], in1=sh2row, op=ALU.add), reads=[f"xs2{s2}", "sh2row"], writes=[f"h2b{s2}"])
        dma("sp", h2_s.ap()[j * 128:(j + 1) * 128, :], h2b_[s2], reads=[f"h2b{s2}"], writes=["h2_s"], stream="eh")
        S.add("pool", lambda e: e.tensor_tensor(out=xs2_[s2], in0=xs2_[s2], in1=sh2row, op=ALU.add), reads=[f"xs2{s2}", "sh2row", f"h2b{s2}"], writes=[f"xs2{s2}"])
        for hh in range(2):
            for kc in range(4):
                S.add("pe", lambda e, kc=kc, hh=hh: e.transpose(out=ps[6 + hh][:, kc * 128:(kc + 1) * 128], in_=xs2_[s2][:, (hh * 4 + kc) * 128:(hh * 4 + kc + 1) * 128], identity=identf[:, 0:128]),
                      reads=[f"xs2{s2}", "identf"], writes=[f"ps{6 + hh}"])
            S.add("act", lambda e, hh=hh: e.activation(out=h2T_[s2][:, hh * 4:(hh + 1) * 4, :].rearrange("p k t -> p (k t)"), in_=ps[6 + hh][:, :], func=AF.Copy), reads=[f"ps{6 + hh}"], writes=[f"h2T{s2}"])
        for kc in range(8):
            S.add("pe", lambda e, kc=kc: e.matmul(ps[1][:, 0:16], lhsT=h2T_[s2][:, kc, :], rhs=wr_sb[:, kc, :], start=(kc == 0), stop=(kc == 7)),
                  reads=[f"h2T{s2}", "wr_sb"], writes=["ps1"])
        S.add("dve", lambda e: e.tensor_reduce(out=mx, in_=ps[1][:, 0:16], axis=AX.X, op=ALU.max), reads=["ps1"], writes=["mx"])
        S.add("dve", lambda e: e.tensor_scalar(out=mx, in0=mx, scalar1=-1.0, scalar2=None, op0=ALU.mult), reads=["mx"], writes=["mx"])
        S.add("act", lambda e: e.activation(out=ex, in_=ps[1][:, 0:16], func=AF.Exp, bias=mx[:, 0:1], accum_out=sm), reads=["ps1", "mx"], writes=["ex", "sm"])
        S.add("dve", lambda e: e.reciprocal(out=sm, in_=sm), reads=["sm"], writes=["sm"])
        S.add("dve", lambda e, j=j: e.tensor_scalar(out=aff_sb[:, j, :], in0=ex, scalar1=sm[:, 0:1], scalar2=None, op0=ALU.mult), reads=["ex", "sm"], writes=["aff_sb"])
    efirst(0)
    for j in range(NT):
        if j + 1 < NT:
            efirst(j + 1)
        esecond(j)
    if "aff" in dbg:
        dbg_out["aff"] = nc.dram_tensor("dbg_aff", [128, NT, 16], F32, kind="ExternalOutput")
        dma("sp", dbg_out["aff"].ap(), aff_sb, reads=["aff_sb"], stream="c0")

    if stop_after == "E":
        S.emit()
        return nc, dbg_out

    S.barrier()
    A.reset()
    lo = A.alloc([128, 16], F32)
    hi = A.alloc([128, 16], F32)
    mid = A.alloc([128, 16], F32)
    cnt = A.alloc([128, 16], F32)
    sel = A.alloc([128, 16], F32)
    dl = A.alloc([128, 16], F32)
    dh_ = A.alloc([128, 16], F32)
    cmpb = A.alloc([128, NT, 16], F32)
    maskb = A.alloc([128, NT, 16], F32)
    S.add("dve", lambda e: e.memset(lo, 0.0), writes=["lo"])
    S.add("dve", lambda e: e.memset(hi, 1.0), writes=["hi"])
    for _ in range(30):
        S.add("dve", lambda e: e.tensor_tensor(out=mid, in0=lo, in1=hi, op=ALU.add), reads=["lo", "hi"], writes=["mid"])
        S.add("dve", lambda e: e.tensor_scalar(out=mid, in0=mid, scalar1=0.5, scalar2=None, op0=ALU.mult), reads=["mid"], writes=["mid"])
        S.add("dve", lambda e: e.tensor_tensor(out=cmpb, in0=aff_sb, in1=mid.unsqueeze(1).broadcast_to([128, NT, 16]), op=ALU.is_gt), reads=["aff_sb", "mid"], writes=["cmpb"])
        S.add("dve", lambda e: e.tensor_reduce(out=cnt, in_=cmpb.rearrange("p j e -> p e j"), axis=AX.X, op=ALU.add), reads=["cmpb"], writes=["cnt"])
        S.add("pe", lambda e: e.matmul(ps[0][:, 0:16], lhsT=ones_f, rhs=cnt, start=True, stop=True), reads=["cnt", "ones_f"], writes=["ps0"])
        S.add("dve", lambda e: e.tensor_single_scalar(out=sel, in_=ps[0][:, 0:16], scalar=1023.5, op=ALU.is_ge), reads=["ps0"], writes=["sel"])
        S.add("dve", lambda e: e.tensor_tensor(out=dl, in0=mid, in1=lo, op=ALU.subtract), reads=["mid", "lo"], writes=["dl"])
        S.add("dve", lambda e: e.tensor_tensor(out=dl, in0=dl, in1=sel, op=ALU.mult), reads=["dl", "sel"], writes=["dl"])
        S.add("dve", lambda e: e.tensor_tensor(out=dh_, in0=hi, in1=mid, op=ALU.subtract), reads=["mid", "hi"], writes=["dh"])
        S.add("dve", lambda e: e.tensor_tensor(out=dh_, in0=dh_, in1=sel, op=ALU.mult), reads=["dh", "sel"], writes=["dh"])
        S.add("dve", lambda e: e.tensor_tensor(out=lo, in0=lo, in1=dl, op=ALU.add), reads=["lo", "dl"], writes=["lo"])
        S.add("dve", lambda e: e.tensor_tensor(out=hi, in0=mid, in1=dh_, op=ALU.add), reads=["mid", "dh"], writes=["hi"])
    S.add("dve", lambda e: e.tensor_tensor(out=maskb, in0=aff_sb, in1=lo.unsqueeze(1).broadcast_to([128, NT, 16]), op=ALU.is_gt), reads=["aff_sb", "lo"], writes=["maskb"])
    tri = A.alloc([128, 128], F32)
    iota_sb = A.alloc([128, 1024], F32)
    tokid = A.alloc([128, NT], F32)
    csb = A.alloc([128, NT, 16], F32)
    totb = A.alloc([128, NT, 16], F32)
    sca = A.alloc([128, NT, 16], F32)
    scb = A.alloc([128, NT, 16], F32)
    pos = A.alloc([128, NT, 16], F32)
    dma("sp", tri, tri_d.ap(), writes=["tri"], stream="c0")
    dma("sp", iota_sb, iota_d.ap(), writes=["iota_sb"], stream="c0")
    dma("sp", tokid, tok_d.ap(), writes=["tokid"], stream="c0")
    mflat = maskb.rearrange("p j e -> p (j e)")
    for h in range(2):
        S.add("pe", lambda e, h=h: e.matmul(ps[2 + h][:, :], lhsT=tri, rhs=mflat[:, h * 512:(h + 1) * 512], start=True, stop=True), reads=["tri", "maskb"], writes=[f"ps{2 + h}"])
        S.add("pe", lambda e, h=h: e.matmul(ps[4 + h][:, :], lhsT=ones_f, rhs=mflat[:, h * 512:(h + 1) * 512], start=True, stop=True), reads=["ones_f", "maskb"], writes=[f"ps{4 + h}"])
        S.add("dve", lambda e, h=h: e.tensor_copy(out=csb.rearrange("p j e -> p (j e)")[:, h * 512:(h + 1) * 512], in_=ps[2 + h][:, :]), reads=[f"ps{2 + h}"], writes=["csb"])
        S.add("dve", lambda e, h=h: e.tensor_copy(out=totb.rearrange("p j e -> p (j e)")[:, h * 512:(h + 1) * 512], in_=ps[4 + h][:, :]), reads=[f"ps{4 + h}"], writes=["totb"])
    cur, curk, oth, othk = totb, "totb", sca, "sca"
    for sft in (1, 2, 4, 8, 16, 32):
        S.add("dve", lambda e, cur=cur, oth=oth, sft=sft: e.tensor_tensor(out=oth[:, sft:, :], in0=cur[:, sft:, :], in1=cur[:, 0:NT - sft, :], op=ALU.add), reads=[curk], writes=[othk])
        S.add("dve", lambda e, cur=cur, oth=oth, sft=sft: e.tensor_copy(out=oth[:, 0:sft, :], in_=cur[:, 0:sft, :]), reads=[curk, othk], writes=[othk])
        if cur is totb:
            cur, curk, oth, othk = sca, "sca", scb, "scb"
        else:
            cur, curk, oth, othk = oth, othk, cur, curk
    S.add("dve", lambda e, cur=cur: e.tensor_tensor(out=pos, in0=cur, in1=totb, op=ALU.subtract), reads=[curk, "totb"], writes=["pos"])
    S.add("dve", lambda e: e.tensor_tensor(out=pos, in0=pos, in1=csb, op=ALU.add), reads=["pos", "csb"], writes=["pos"])
    S.add("dve", lambda e: e.tensor_scalar(out=pos, in0=pos, scalar1=-1.0, scalar2=None, op0=ALU.add), reads=["pos"], writes=["pos"])
    idx_all = A.alloc([128, 16, 8], I32)
    w_all = A.alloc([128, 16, 8], F32)
    vals5 = [A.alloc([128, NT, 5], BF16) for _ in range(2)]
    r1 = A.alloc([128, NT], F32)
    r2 = A.alloc([128, NT], F32)
    a1f = A.alloc([128, NT], F32)
    oh = [A.alloc([128, 1024], BF16) for _ in range(4)]
    rows5 = A.alloc([5, 1024], F32)
    tokf = A.alloc([128, 8], F32)
    pvs = A.alloc([128, 40], F32)
    lof = A.alloc([128, 1], F32)
    S.add("dve", lambda e: e.tensor_scalar(out=lof, in0=tokid[:, 0:1], scalar1=1.0, scalar2=None, op0=ALU.mult), reads=["tokid"], writes=["lof"])
    for v2 in range(2):
        S.add("dve", lambda e, v2=v2: e.tensor_scalar(out=r1, in0=tokid, scalar1=lof[:, 0:1], scalar2=1.0 / 128, op0=ALU.subtract, op1=ALU.mult), reads=["tokid", "lof"], writes=["r1"])
        S.add("dve", lambda e, v2=v2: e.tensor_copy(out=vals5[v2][:, :, 0], in_=r1), reads=["r1"], writes=[f"vals{v2}"])
        S.add("dve", lambda e, v2=v2: e.tensor_copy(out=vals5[v2][:, :, 1], in_=lof[:, 0:1].broadcast_to([128, NT])), reads=["lof"], writes=[f"vals{v2}"])
    ohc = 0
    for ex_ in range(16):
        v2 = ex_ % 2
        bb = 4 + 2 * (ex_ % 2)
        V = vals5[v2]
        S.add("dve", lambda e, V=V, ex_=ex_: e.tensor_copy(out=V[:, :, 2], in_=aff_sb[:, :, ex_]), reads=["aff_sb"], writes=[f"vals{v2}"])
        S.add("dve", lambda e, V=V: e.tensor_copy(out=a1f, in_=V[:, :, 2]), reads=[f"vals{v2}"], writes=["a1f"])
        S.add("dve", lambda e, ex_=ex_: e.tensor_tensor(out=r1, in0=aff_sb[:, :, ex_], in1=a1f, op=ALU.subtract), reads=["aff_sb", "a1f"], writes=["r1"])
        S.add("dve", lambda e, V=V: e.tensor_copy(out=V[:, :, 3], in_=r1), reads=["r1"], writes=[f"vals{v2}"])
        S.add("dve", lambda e, V=V: e.tensor_copy(out=a1f, in_=V[:, :, 3]), reads=[f"vals{v2}"], writes=["a1f"])
        S.add("dve", lambda e: e.tensor_tensor(out=r2, in0=r1, in1=a1f, op=ALU.subtract), reads=["r1", "a1f"], writes=["r2"])
        S.add("dve", lambda e, V=V: e.tensor_copy(out=V[:, :, 4], in_=r2), reads=["r2"], writes=[f"vals{v2}"])
        for j in range(NT):
            so = ohc % 4
            eng_ = "dve"
            ohc += 1
            S.add(eng_, lambda e, so=so, j=j, ex_=ex_: e.tensor_scalar(out=oh[so], in0=iota_sb, scalar1=pos[:, j, ex_:ex_ + 1], scalar2=maskb[:, j, ex_:ex_ + 1],
                                                                     op0=ALU.is_equal, op1=ALU.mult),
                  reads=["iota_sb", "pos", "maskb"], writes=[f"oh{so}"])
            for h in range(2):
                S.add("pe", lambda e, so=so, j=j, h=h, bb=bb, V=V: e.matmul(ps[bb + h][0:5, :], lhsT=V[:, j, :], rhs=oh[so][:, h * 512:(h + 1) * 512],
                                                                        start=(j == 0), stop=(j == NT - 1)),
                      reads=[f"oh{so}", f"vals{v2}"], writes=[f"ps{bb + h}"])
        for h in range(2):
            S.add("act", lambda e, h=h, bb=bb: e.activation(out=rows5[:, h * 512:(h + 1) * 512], in_=ps[bb + h][0:5, :], func=AF.Copy), reads=[f"ps{bb + h}"], writes=["rows5"])
        for sc in range(8):
            S.add("pe", lambda e, sc=sc: e.transpose(out=ps[3][:, sc * 5:(sc + 1) * 5], in_=rows5[0:5, sc * 128:(sc + 1) * 128], identity=identf[0:5, 0:5]),
                  reads=["rows5", "identf"], writes=["ps3"])
        S.add("act", lambda e: e.activation(out=pvs, in_=ps[3][:, 0:40], func=AF.Copy), reads=["ps3"], writes=["pvs"])
        pv = pvs.rearrange("p (s t) -> p s t", t=5)
        S.add("dve", lambda e, pv=pv: e.scalar_tensor_tensor(out=tokf, in0=pv[:, :, 0], scalar=128.0, in1=pv[:, :, 1], op0=ALU.mult, op1=ALU.add), reads=["pvs"], writes=["tokf"])
        S.add("dve", lambda e, ex_=ex_: e.tensor_copy(out=idx_all[:, ex_, :], in_=tokf), reads=["tokf"], writes=["idx_all"])
        S.add("dve", lambda e, pv=pv, ex_=ex_: e.tensor_tensor(out=w_all[:, ex_, :], in0=pv[:, :, 2], in1=pv[:, :, 3], op=ALU.add), reads=["pvs"], writes=["w_all"])
        S.add("dve", lambda e, pv=pv, ex_=ex_: e.tensor_tensor(out=w_all[:, ex_, :], in0=w_all[:, ex_, :], in1=pv[:, :, 4], op=ALU.add), reads=["pvs", "w_all"], writes=["w_all"])
    if "idx" in dbg:
        dbg_out["idx"] = nc.dram_tensor("dbg_idx", [128, 16, 8], I32, kind="ExternalOutput")
        dbg_out["wsl"] = nc.dram_tensor("dbg_w", [128, 16, 8], F32, kind="ExternalOutput")
        dma("sp", dbg_out["idx"].ap(), idx_all, reads=["idx_all"], stream="c0")
        dma("sp", dbg_out["wsl"].ap(), w_all, reads=["w_all"], stream="c0")
    if stop_after == "F3":
        S.emit()
        return nc, dbg_out

    S.barrier()
    keep = A.off
    A.off = A.lo
    idx2 = A.alloc([128, 16, 8], I32)
    w2 = A.alloc([128, 16, 8], F32)
    S.add("dve", lambda e: e.tensor_copy(out=idx2, in_=idx_all), reads=["idx_all"], writes=["idx2"])
    S.add("dve", lambda e: e.tensor_copy(out=w2, in_=w_all), reads=["w_all"], writes=["w2"])
    S.barrier()
    wsl = [A.alloc([128, 8 * 2048], BF16) for _ in range(4)]
    xeT = A.alloc([128, 8, 1024], BF16)
    hidT = A.alloc([128, 16, 1024], BF16)
    xeg = [A.alloc([128, D], BF16) for _ in range(2)]
    ye = [A.alloc([128, D], F32) for _ in range(2)]
    sg = [A.alloc([128, 512], BF16) for _ in range(2)]
    wseq = [0]

    def wload(src_ap):
        k = wseq[0] % 4
        wseq[0] += 1
        flat = src_ap.rearrange("p a b -> p (a b)")
        for hh in range(2):
            dma("pool", wsl[k][:, hh * 8192:(hh + 1) * 8192], flat[:, hh * 8192:(hh + 1) * 8192], writes=[f"wsl{k}"], stream=f"w{k}{hh}")
        return k

    slots = {}
    slots[(0, "g")] = wload(wg_d.ap()[0])
    slots[(0, "u")] = wload(wu_d.ap()[0])
    slots[(0, "d")] = wload(wd_d.ap()[0])
    gcnt = [0]
    ycnt = 0

    def g_issue(ex_, sc):
        xs_ = gcnt[0] % 2
        gcnt[0] += 1
        S.add("pool", lambda e: e.indirect_dma_start(out=xeg[xs_], out_offset=None, in_=h2_s.ap(),
                                                     in_offset=bass.IndirectOffsetOnAxis(ap=idx2[:, ex_, sc:sc + 1], axis=0)),
              reads=["idx2", "h2_s"], writes=[f"xeg{xs_}"], dma=f"gath{xs_}")
        return xs_

    def g_transpose(xs_, sc):
        for kc in range(8):
            S.add("pe", lambda e, kc=kc: e.transpose(out=psb(0)[:, kc * 128:(kc + 1) * 128], in_=xeg[xs_][:, kc * 128:(kc + 1) * 128], identity=I_b),
                  reads=[f"xeg{xs_}", "identb"], writes=["ps0"])
        S.add("act", lambda e: e.activation(out=xeT[:, :, sc * 128:(sc + 1) * 128], in_=psb(0)[:, :].rearrange("p (k t) -> p k t", k=8), func=AF.Copy),
              reads=["ps0"], writes=["xeT"])

    for ex_ in range(16):
        kg_, ku_, kd_ = slots[(ex_, "g")], slots[(ex_, "u")], slots[(ex_, "d")]
        wgv = wsl[kg_].rearrange("p (a b) -> p a b", a=8)
        wuv = wsl[ku_].rearrange("p (a b) -> p a b", a=8)
        wdv = wsl[kd_].rearrange("p (a b) -> p a b", a=16)
        if ex_ == 0:
            for sc in range(8):
                xs_ = g_issue(0, sc)
                g_transpose(xs_, sc)
        if ex_ + 1 < 16:
            slots[(ex_ + 1, "g")] = wload(wg_d.ap()[ex_ + 1])
        for ffc in range(16):
            for h in range(2):
                bg, bu = 1 + (ffc * 2 + h) % 2, 3 + (ffc * 2 + h) % 2
                s_ = (ffc * 2 + h) % 2
                for kc in range(8):
                    S.add("pe", lambda e, bg=bg, kc=kc, ffc=ffc, h=h, wgv=wgv: e.matmul(ps[bg][:, :], lhsT=wgv[:, kc, ffc * 128:(ffc + 1) * 128], rhs=xeT[:, kc, h * 512:(h + 1) * 512],
                                                                                    start=(kc == 0), stop=(kc == 7)),
                          reads=[f"wsl{kg_}", "xeT"], writes=[f"ps{bg}"])
                for kc in range(8):
                    S.add("pe", lambda e, bu=bu, kc=kc, ffc=ffc, h=h, wuv=wuv: e.matmul(ps[bu][:, :], lhsT=wuv[:, kc, ffc * 128:(ffc + 1) * 128], rhs=xeT[:, kc, h * 512:(h + 1) * 512],
                                                                                    start=(kc == 0), stop=(kc == 7)),
                          reads=[f"wsl{ku_}", "xeT"], writes=[f"ps{bu}"])
                S.add("act", lambda e, bg=bg, s_=s_: e.activation(out=sg[s_], in_=ps[bg][:, :], func=AF.Silu), reads=[f"ps{bg}"], writes=[f"sg{s_}"])
                S.add("dve", lambda e, bu=bu, s_=s_, ffc=ffc, h=h: e.tensor_tensor(out=hidT[:, ffc, h * 512:(h + 1) * 512], in0=ps[bu][:, :], in1=sg[s_], op=ALU.mult),
                      reads=[f"ps{bu}", f"sg{s_}"], writes=["hidT"])
        if ex_ + 1 < 16:
            slots[(ex_ + 1, "u")] = wload(wu_d.ap()[ex_ + 1])
            slots[(ex_ + 1, "d")] = wload(wd_d.ap()[ex_ + 1])
        for sc in range(8):
            ys = ycnt % 2
            ycnt += 1
            nxs = g_issue(ex_ + 1, sc) if ex_ + 1 < 16 else None
            for dh in range(2):
                bd = 5 + (sc * 2 + dh) % 3
                for ffc in range(16):
                    S.add("pe", lambda e, bd=bd, ffc=ffc, sc=sc, dh=dh, wdv=wdv: e.matmul(ps[bd][:, :], lhsT=hidT[:, ffc, sc * 128:(sc + 1) * 128], rhs=wdv[:, ffc, dh * 512:(dh + 1) * 512],
                                                                                      start=(ffc == 0), stop=(ffc == 15)),
                          reads=["hidT", f"wsl{kd_}"], writes=[f"ps{bd}"])
                S.add("dve", lambda e, bd=bd, ys=ys, dh=dh, sc=sc, ex_=ex_: e.scalar_tensor_tensor(out=ye[ys][:, dh * 512:(dh + 1) * 512], in0=ps[bd][:, :], scalar=w2[:, ex_, sc:sc + 1],
                                                                                                in1=gt2b[:, dh * 512:(dh + 1) * 512], op0=ALU.mult, op1=ALU.mult),
                      reads=[f"ps{bd}", "w2", "gt2b"], writes=[f"ye{ys}"])
            S.add("pool", lambda e, ys=ys, sc=sc, ex_=ex_: e.indirect_dma_start(out=out_d.ap(), out_offset=bass.IndirectOffsetOnAxis(ap=idx2[:, ex_, sc:sc + 1], axis=0),
                                                                             in_=ye[ys], in_offset=None, compute_op=ALU.add),
                  reads=[f"ye{ys}", "idx2", "out_d"], writes=["out_d"], dma="scat")
            if nxs is not None:
                g_transpose(nxs, sc)

    S.emit()
    return nc, dbg_out


QPERM = np.array([1536 + (hh * 4 + g) * 64 + d for g in range(4) for hh in range(2) for d in range(64)])
COLPERM = np.concatenate([np.arange(0, 512), np.arange(1024, 1536), np.arange(512, 1024), QPERM, np.arange(2048, 4352)])
HPERM = COLPERM[:1536]
ATT_ROWPERM = QPERM - 1536


def consts():
    c = {}
    ident = np.zeros((128, 256), np.float32)
    ident[:, :128] = np.eye(128)
    ident[:, 128:] = np.eye(128)[::-1]
    c["ident"] = ident
    ind = np.ones((4, 5, 128), np.float32)
    ind[0, 1, 0] = 0
    ind[2, 2, 127] = 0
    ind[0, 3, 127] = 0
    ind[2, 4, 0] = 0
    c["ind"] = ind
    t01 = np.linspace(0.0, 1.0, L, dtype=np.float32)
    bands = 16
    f = np.linspace(1e-4, bands - 1, bands, dtype=np.float32)[None, :]
    w = (np.float32(2.0 * math.pi) * np.arange(L, dtype=np.float32)[:, None] / np.float32(L)).astype(np.float32)
    z = np.concatenate([t01[:, None], np.cos(f * w), -np.sin(f * w)], axis=-1).astype(np.float32)
    c["zT"] = np.ascontiguousarray(np.stack([z.T, z[::-1].T], axis=1))
    c["t01"] = np.ascontiguousarray(np.stack([t01, t01[::-1]], axis=0))
    max_decay = math.log(1e-2) / 0.3
    min_decay = math.log(1e-2) / 1.5
    deltas = np.abs(np.linspace(min_decay, max_decay, 512, dtype=np.float32))
    c["deltacol"] = np.ascontiguousarray((-deltas).reshape(4, 128).T)
    rows = L // 64
    row = np.repeat(np.arange(rows), 64).astype(np.float32)
    col = np.tile(np.arange(64), rows).astype(np.float32)
    inv = (10000.0 ** (-np.arange(0, 32, 2, dtype=np.float32) / 32)).astype(np.float32)
    ang = np.concatenate([row[:, None] * inv, col[:, None] * inv], axis=-1)
    rope = np.concatenate([np.cos(ang), np.sin(ang)], axis=-1).astype(np.float32)
    c["rope"] = np.ascontiguousarray(rope.reshape(NT, 128, 64).transpose(1, 0, 2))
    c["iota"] = np.tile(np.arange(1024, dtype=np.float32)[None], (128, 1))
    c["tokid"] = (np.arange(NT)[None, :] * 128 + np.arange(128)[:, None]).astype(np.float32)
    c["tri"] = np.triu(np.ones((128, 128), np.float32))
    return c


def prep_shared(inp):
    l = 0
    m = dict(consts())
    f32 = lambda a: np.ascontiguousarray(a, dtype=np.float32)
    m["wada"] = f32(inp["w_ada"][l].reshape(8, 128, 6144).transpose(1, 0, 2))
    m["bada"] = f32(inp["b_ada"][l][None])
    m["gmixcol"] = f32(inp["g_mix"][l].reshape(8, 128).T)
    m["gffnrow"] = f32(inp["g_ffn"][l][None])
    m["win"] = f32(inp["w_in"][l][:, COLPERM].reshape(8, 128, 4352).transpose(1, 0, 2))
    bin_p = inp["b_in"][l][COLPERM]
    m["binh"] = f32(bin_p[None, :1536])
    m["bino"] = f32(bin_p[None, 1536:])
    m["sw"] = f32(inp["short_w"][l][:, HPERM])
    m["sb"] = f32(inp["short_b"][l][HPERM][None])
    m["hw1"] = f32(inp["hy_w1"][l]); m["hw2"] = f32(inp["hy_w2"][l]); m["hw3"] = f32(inp["hy_w3"][l]); m["hw4"] = f32(inp["hy_w4"][l])
    m["hbcol"] = f32(np.stack([inp["hy_b1"][l], inp["hy_b2"][l], inp["hy_b3"][l]], axis=1))
    m["hfrcol"] = f32(inp["hy_freq"][l].T)
    m["fbcol"] = f32(inp["hy_bias"][l].reshape(2, 4, 128).transpose(2, 0, 1).reshape(128, 8))
    m["qgain"] = f32(inp["q_gain"][l][None]); m["kgain"] = f32(inp["k_gain"][l][None])
    m["why"] = f32(inp["w_hy_out"][l].reshape(4, 128, D).transpose(1, 0, 2))
    m["wat"] = f32(inp["w_att_out"][l][ATT_ROWPERM].reshape(4, 128, D).transpose(1, 0, 2))
    m["wout"] = f32(inp["w_out"][l].reshape(8, 128, D).transpose(1, 0, 2))
    m["wr"] = f32(inp["w_router"][l].reshape(8, 128, 16).transpose(1, 0, 2))
    m["wg"] = f32(inp["w_gate"][l].reshape(16, 8, 128, 2048).transpose(0, 2, 1, 3))
    m["wu"] = f32(inp["w_up"][l].reshape(16, 8, 128, 2048).transpose(0, 2, 1, 3))
    m["wd"] = f32(inp["w_down"][l].reshape(16, 16, 128, D).transpose(0, 2, 1, 3))
    return m


def prep(inp, b, shared=None):
    m = dict(shared if shared is not None else prep_shared(inp))
    m["x"] = np.ascontiguousarray(inp["x"][b], dtype=np.float32)
    m["ccol"] = np.ascontiguousarray(inp["c"][b].reshape(8, 128).T, dtype=np.float32)
    return m


_CACHE = {}


REAL_CORES = (0, 1, 4, 5)


def kernel(**inputs):
    inp = {k: np.asarray(v) for k, v in inputs.items()}
    if "nc" not in _CACHE:
        _CACHE["nc"] = build("F")[0]
    nc = _CACHE["nc"]
    shared = prep_shared(inp)
    real = {core: prep(inp, b, shared) for b, core in enumerate(REAL_CORES)}
    ck = set(consts().keys())
    zero = {k: (v if k in ck else np.zeros_like(v)) for k, v in real[0].items()}
    maps = [real.get(core, zero) for core in range(8)]
    res = run_bass_kernel_spmd(nc, maps, core_ids=list(range(8)))
    return np.stack([res.results[core]["out"] for core in REAL_CORES], axis=0).astype(np.float32)
```

```python
import math
import numpy as np
import concourse.bass as bass
import concourse.mybir as mybir
from concourse.bass_utils import run_bass_kernel_spmd

F32 = mybir.dt.float32
BF16 = mybir.dt.bfloat16
I32 = mybir.dt.int32
AF = mybir.ActivationFunctionType
ALU = mybir.AluOpType
AX = mybir.AxisListType

L = 8192
D = 1024
NT = 64
ENGS = ("pe", "act", "dve", "pool", "sp")


class _Op:
    __slots__ = ("eng", "fn", "deps", "needs_inc", "dma", "inc_idx", "dma_val", "epoch")

    def __init__(self, eng, fn, dma):
        self.eng = eng
        self.fn = fn
        self.deps = []
        self.needs_inc = False
        self.dma = dma
        self.inc_idx = 0
        self.dma_val = 0
        self.epoch = 0


class Sched:
    def __init__(self, nc):
        self.nc = nc
        self.ops = {e: [] for e in ENGS}
        self.last_w = {}
        self.readers = {}
        self.dma_cnt = {}
        self.dma_last = {}
        self.barrier_deps = []
        self.epoch = 0

    def add(self, eng, fn, reads=(), writes=(), dma=None):
        op = _Op(eng, fn, dma)
        op.epoch = self.epoch
        deps = []
        for k in reads:
            w = self.last_w.get(k)
            if w is not None:
                deps.append(w)
        for k in writes:
            w = self.last_w.get(k)
            if w is not None:
                deps.append(w)
            deps.extend(self.readers.get(k, {}).values())
        deps.extend(self.barrier_deps)
        if dma is not None:
            p = self.dma_last.get(dma)
            if p is not None:
                deps.append(p)
            self.dma_cnt[dma] = self.dma_cnt.get(dma, 0) + 16
            op.dma_val = self.dma_cnt[dma]
            self.dma_last[dma] = op
        seen = set()
        for d in deps:
            if id(d) in seen:
                continue
            seen.add(id(d))
            if d.dma is None and d.eng == "pe" and eng == "pe" and dma is None:
                continue
            op.deps.append(d)
            if d.dma is None:
                d.needs_inc = True
        rk = ("dma", dma) if dma is not None else ("eng", eng)
        for k in reads:
            self.readers.setdefault(k, {})[rk] = op
        for k in writes:
            self.last_w[k] = op
            self.readers[k] = {}
        self.ops[eng].append(op)
        return op

    def barrier(self):
        deps = []
        for e in ENGS:
            for op in reversed(self.ops[e]):
                if op.dma is None:
                    deps.append(op)
                    break
        deps.extend(self.dma_last.values())
        self.barrier_deps = deps
        self.epoch += 1

    def emit(self):
        nc = self.nc
        self.barrier()
        fin = _Op("sp", None, None)
        for d in self.barrier_deps:
            fin.deps.append(d)
            if d.dma is None:
                d.needs_inc = True
        self.ops["sp"].append(fin)
        esem = {}
        for e in ENGS:
            c = {}
            for op in self.ops[e]:
                if op.dma is None and op.needs_inc:
                    c[op.epoch] = c.get(op.epoch, 0) + 1
                    op.inc_idx = c[op.epoch]
                    if (e, op.epoch) not in esem:
                        esem[(e, op.epoch)] = nc.alloc_semaphore(name=f"s_{e}_{op.epoch}")
        dsem = {s: nc.alloc_semaphore(name=f"d_{s}") for s in self.dma_cnt}
        ops = self.ops

        def run(eng_name, eng):
            waited = {}
            for op in ops[eng_name]:
                for d in op.deps:
                    if d.dma is not None:
                        key, sem, val = ("d", d.dma), dsem[d.dma], d.dma_val
                    else:
                        key, sem, val = ("e", d.eng, d.epoch), esem[(d.eng, d.epoch)], d.inc_idx
                    if waited.get(key, 0) >= val:
                        continue
                    waited[key] = val
                    eng.wait_ge(sem, val)
                if op.fn is None:
                    continue
                ins = op.fn(eng)
                if op.dma is not None:
                    ins.then_inc(dsem[op.dma], 16)
                elif op.needs_inc:
                    ins.then_inc(esem[(eng_name, op.epoch)], 1)

        block = nc.Block()
        with block:
            @block.tensor
            def _(e):
                run("pe", e)

            @block.scalar
            def _(e):
                run("act", e)

            @block.vector
            def _(e):
                run("dve", e)

            @block.gpsimd
            def _(e):
                run("pool", e)

            @block.sync
            def _(e):
                run("sp", e)


class Arena:
    def __init__(self, big, lo, hi):
        self.big, self.lo, self.hi = big, lo, hi
        self.off = lo

    def reset(self):
        self.off = self.lo

    def alloc(self, shape, dt):
        n = int(np.prod(shape[1:]))
        esz = 4 if dt in (F32, I32) else 2
        nb = (n * esz + 31) // 32 * 32
        assert self.off + nb <= self.hi, ("SBUF arena overflow", shape, self.off, nb, self.hi)
        a = self.big[0:shape[0], self.off // 4:(self.off + nb) // 4]
        self.off += nb
        if dt != F32:
            a = a.bitcast(dt)
        a = a[:, 0:n]
        if len(shape) > 2:
            names = " ".join(f"d{i}" for i in range(1, len(shape)))
            a = a.rearrange(f"p ({names}) -> p {names}", **{f"d{i}": shape[i] for i in range(1, len(shape))})
        return a


def build(stop_after="F", dbg=()):
    nc = bass.Bass("TRN2", target_bir_lowering=False)
    S = Sched(nc)
    dbg_out = {}

    def din(name, shape, dt=F32):
        return nc.dram_tensor(name, list(shape), dt, kind="ExternalInput")

    def dscr(name, shape, dt):
        if name in dbg:
            t = nc.dram_tensor(name, list(shape), dt, kind="ExternalOutput")
            dbg_out[name] = t
            return t
        return nc.dram_tensor(name, list(shape), dt)

    x_d = din("x", [L, D])
    ccol_d = din("ccol", [128, 8])
    wada_d = din("wada", [128, 8, 6144])
    bada_d = din("bada", [1, 6144])
    gmix_d = din("gmixcol", [128, 8])
    gffn_d = din("gffnrow", [1, D])
    win_d = din("win", [128, 8, 4352])
    binh_d = din("binh", [1, 1536])
    bino_d = din("bino", [1, 2816])
    sw_d = din("sw", [3, 1536])
    sb_d = din("sb", [1, 1536])
    ind_d = din("ind", [4, 5, 128])
    ident_d = din("ident", [128, 256])
    zT_d = din("zT", [33, 2, L])
    t01_d = din("t01", [2, L])
    delta_d = din("deltacol", [128, 4])
    hw1_d = din("hw1", [33, 64])
    hw2_d = din("hw2", [64, 64])
    hw3_d = din("hw3", [64, 64])
    hw4_d = din("hw4", [64, 2048])
    hb_d = din("hbcol", [64, 3])
    hfr_d = din("hfrcol", [64, 3])
    fb_d = din("fbcol", [128, 8])
    rope_d = din("rope", [128, NT, 64])
    qg_d = din("qgain", [1, 64])
    kg_d = din("kgain", [1, 64])
    why_d = din("why", [128, 4, D])
    wat_d = din("wat", [128, 4, D])
    wout_d = din("wout", [128, 8, D])
    wr_d = din("wr", [128, 8, 16])
    wg_d = din("wg", [16, 128, 8, 2048])
    wu_d = din("wu", [16, 128, 8, 2048])
    wd_d = din("wd", [16, 128, 16, D])
    iota_d = din("iota", [128, 1024])
    tok_d = din("tokid", [128, NT])
    tri_d = din("tri", [128, 128])
    out_d = nc.dram_tensor("out", [L, D], F32, kind="ExternalOutput")

    mod_s = dscr("mod_s", [1, 6144], F32)
    v_s = dscr("v_s", [NT, 128, 512], BF16)
    x2_s = dscr("x2_s", [NT, 128, 512], BF16)
    x1_s = dscr("x1_s", [NT, 128, 512], BF16)
    q_s = dscr("q_s", [NT, 128, 512], BF16)
    kv_s = dscr("kv_s", [NT, 128, 256], BF16)
    gt_s = dscr("gt_s", [NT, 128, 2048], BF16)
    a_s = dscr("a_s", [2, 512, 16384], BF16)
    hy_s = dscr("hy_s", [NT, 128, 512], BF16)
    att_s = dscr("att_s", [NT, 128, 512], BF16)
    h2_s = dscr("h2_s", [L, D], BF16)

    NB = nc.sbuf_bytes_remaining
    big = nc.alloc_sbuf_tensor("big", [128, (NB - 128) // 4], F32)
    TOT = (NB - 128) // 4 * 4
    PERS = 14848
    P = Arena(big, 0, PERS)
    A = Arena(big, PERS, TOT)
    ps = [nc.alloc_psum_tensor(f"ps{i}", [128, 512], F32) for i in range(8)]

    def psb(i):
        return ps[i][:].bitcast(BF16)

    def dma(q, out, in_, reads=(), writes=(), stream=None, slow=False):
        if slow:
            return S.add(q, lambda e: e.dma_start(out=out, in_=in_, allow_slow_non_contiguous=True), reads=reads, writes=writes, dma=stream)
        return S.add(q, lambda e: e.dma_start(out=out, in_=in_), reads=reads, writes=writes, dma=stream)

    identf = P.alloc([128, 256], F32)
    identb = P.alloc([128, 256], BF16)
    modcol = P.alloc([128, 48], F32)
    gt1b = P.alloc([128, D], F32)
    gt2b = P.alloc([128, D], F32)
    A1col = P.alloc([128, 8], F32)
    gmixc = P.alloc([128, 8], F32)
    ones_b = P.alloc([128, 128], BF16)
    ones_f = P.alloc([128, 128], F32)
    aff_sb = P.alloc([128, NT, 16], F32)
    dma("sp", identf, ident_d.ap(), writes=["identf"], stream="c0")
    S.add("dve", lambda e: e.tensor_copy(out=identb, in_=identf), reads=["identf"], writes=["identb"])
    S.add("dve", lambda e: e.memset(ones_b, 1.0), writes=["ones_b"])
    S.add("dve", lambda e: e.memset(ones_f, 1.0), writes=["ones_f"])
    I_b = identb[:, 0:128]
    J_b = identb[:, 128:256]

    ccol = A.alloc([128, 8], F32)
    sil = A.alloc([128, 8], F32)
    modrow = A.alloc([1, 6144], F32)
    badar = A.alloc([1, 6144], F32)
    wa = A.alloc([128, 8, 2048], F32)
    dma("sp", ccol, ccol_d.ap(), writes=["ccol"], stream="c0")
    dma("sp", badar, bada_d.ap(), writes=["badar"], stream="c0")
    dma("sp", gmixc, gmix_d.ap(), writes=["gmixc"], stream="c0")
    S.add("act", lambda e: e.activation(out=sil, in_=ccol, func=AF.Silu), reads=["ccol"], writes=["sil"])
    for ch in range(3):
        dma("sp", wa, wada_d.ap()[:, :, ch * 2048:(ch + 1) * 2048], writes=["wa"], stream="wa")
        for n in range(4):
            for kc in range(8):
                S.add("pe", lambda e, n=n, kc=kc: e.matmul(ps[n][0:1, :], lhsT=sil[:, kc:kc + 1], rhs=wa[:, kc, n * 512:(n + 1) * 512],
                                                         start=(kc == 0), stop=(kc == 7)),
                      reads=["sil", "wa"], writes=[f"ps{n}"])
            c0 = ch * 2048 + n * 512
            S.add("dve", lambda e, n=n, c0=c0: e.tensor_tensor(out=modrow[:, c0:c0 + 512], in0=ps[n][0:1, :], in1=badar[:, c0:c0 + 512], op=ALU.add),
                  reads=[f"ps{n}", "badar"], writes=["modrow"])
    dma("sp", mod_s.ap(), modrow, reads=["modrow"], writes=["mod_s"], stream="c0")
    dma("sp", modcol, mod_s.ap().rearrange("o (q p) -> p (o q)", p=128), reads=["mod_s"], writes=["modcol"], stream="c0", slow=True)
    dma("sp", gt1b, mod_s.ap()[:, 2048:3072].partition_broadcast(128), reads=["mod_s"], writes=["gt1b"], stream="c0")
    dma("sp", gt2b, mod_s.ap()[:, 5120:6144].partition_broadcast(128), reads=["mod_s"], writes=["gt2b"], stream="c0")
    S.add("dve", lambda e: e.scalar_tensor_tensor(out=A1col, in0=modcol[:, 8:16], scalar=1.0, in1=gmixc, op0=ALU.add, op1=ALU.mult),
          reads=["modcol", "gmixc"], writes=["A1col"])
    sh1col = modcol[:, 0:8]
    if "mod" in dbg:
        dbg_out["mod"] = nc.dram_tensor("dbg_mod", [1, 6144], F32, kind="ExternalOutput")
        dma("sp", dbg_out["mod"].ap(), modrow, reads=["modrow"], stream="c0")

    S.barrier()
    A.reset()
    wb = A.alloc([128, 8, 4352], BF16)
    wh0 = A.alloc([128, 8, 1536], BF16)
    wh2 = A.alloc([128, 8, 1536], BF16)
    swb_lo = A.off
    swb = A.alloc([128, 3, 1536], BF16)
    swb_hi = A.off
    swf = A.alloc([3, 1536], F32)
    bh3 = A.alloc([4, 1536], F32)
    brow4 = A.alloc([4, 1536], BF16)
    bof = A.alloc([1, 2816], F32)
    bob = A.alloc([1, 2816], BF16)
    indf = A.alloc([4, 5, 128], F32)
    indb = A.alloc([4, 5, 128], BF16)
    for kc in range(8):
        dma("pool", wb[:, kc, :], win_d.ap()[:, kc, :], writes=["wb"], stream=f"wb{kc % 2}")
    dma("pool", swb.rearrange("p a c -> p (a c)"), sw_d.ap().rearrange("a c -> (a c)").partition_broadcast(128), writes=["swb"], stream="wb0")
    dma("sp", swf, sw_d.ap(), writes=["swf"], stream="c0")
    for r in range(3):
        dma("sp", bh3[r:r + 1, :], binh_d.ap(), writes=["bh3"], stream="c0")
    dma("sp", bh3[3:4, :], sb_d.ap(), writes=["bh3"], stream="c0")
    dma("sp", bof, bino_d.ap(), writes=["bof"], stream="c0")
    dma("sp", indf, ind_d.ap(), writes=["indf"], stream="c0")
    S.add("dve", lambda e: e.tensor_tensor(out=bh3[0:3, :], in0=bh3[0:3, :], in1=swf, op=ALU.mult), reads=["bh3", "swf"], writes=["bh3"])
    S.add("dve", lambda e: e.tensor_copy(out=brow4, in_=bh3), reads=["bh3"], writes=["brow4"])
    S.add("dve", lambda e: e.tensor_copy(out=bob, in_=bof), reads=["bof"], writes=["bob"])
    S.add("dve", lambda e: e.tensor_copy(out=indb, in_=indf), reads=["indf"], writes=["indb"])
    for kc in range(8):
        S.add("dve", lambda e, kc=kc: e.tensor_tensor(out=wh0[:, kc, :], in0=wb[:, kc, 0:1536], in1=swb[:, 0, :], op=ALU.mult),
              reads=["wb", "swb"], writes=["wh0"])
        S.add("pool", lambda e, kc=kc: e.tensor_tensor(out=wh2[:, kc, :], in0=wb[:, kc, 0:1536], in1=swb[:, 2, :], op=ALU.mult),
              reads=["wb", "swb"], writes=["wh2"])
    for kc in range(8):
        S.add("dve", lambda e, kc=kc: e.tensor_tensor(out=wb[:, kc, 0:1536], in0=wb[:, kc, 0:1536], in1=swb[:, 1, :], op=ALU.mult),
              reads=["wb", "swb", "wh0", "wh2"], writes=["wb"])
    whs = [wh0, wb, wh2]

    NSL = 4
    S.barrier()
    A2_ = Arena(big, swb_lo, swb_hi)
    xt = [A.alloc([128, D], F32)] * 3
    junk = A.alloc([128, D], BF16)
    ssq = [A.alloc([128, 1], F32) for _ in range(2)] + [A2_.alloc([128, 1], F32)]
    xb = [A.alloc([128, D], BF16) for _ in range(2)] + [A2_.alloc([128, D], BF16)]
    hN = [A.alloc([128, 8, 130], BF16) for _ in range(3)] + [A2_.alloc([128, 8, 130], BF16)]
    hR = [A.alloc([128, 8, 130], BF16) for _ in range(3)] + [A2_.alloc([128, 8, 130], BF16)]
    stg = [A.alloc([128, 4352], BF16)] * 2
    for s in range(NSL):
        S.add("dve", lambda e, s=s: e.memset(hN[s], 0.0), writes=[f"hN{s}"])
        S.add("dve", lambda e, s=s: e.memset(hR[s], 0.0), writes=[f"hR{s}"])

    def stage1(j):
        s2, s3 = j % 3, j % NSL
        dma("sp", xt[s2], x_d.ap()[j * 128:(j + 1) * 128, :], writes=["xt"], stream="x0")
        S.add("act", lambda e: e.activation(out=junk, in_=xt[s2], func=AF.Square, accum_out=ssq[s2]), reads=["xt"], writes=["junk", f"ssq{s2}"])
        S.add("act", lambda e: e.activation(out=ssq[s2], in_=ssq[s2], func=AF.Sqrt, scale=1.0 / D, bias=1e-6), reads=[f"ssq{s2}"], writes=[f"ssq{s2}"])
        S.add("dve", lambda e: e.reciprocal(out=ssq[s2], in_=ssq[s2]), reads=[f"ssq{s2}"], writes=[f"ssq{s2}"])
        S.add("dve", lambda e: e.tensor_scalar(out=xb[s2], in0=xt[s2], scalar1=ssq[s2][:, 0:1], scalar2=None, op0=ALU.mult),
              reads=["xt", f"ssq{s2}"], writes=[f"xb{s2}"])
        for kc in range(8):
            S.add("pe", lambda e, kc=kc: e.transpose(out=psb(0)[:, kc * 128:(kc + 1) * 128], in_=xb[s2][:, kc * 128:(kc + 1) * 128], identity=I_b),
                  reads=[f"xb{s2}", "identb"], writes=["ps0"])
        for kc in range(8):
            S.add("pe", lambda e, kc=kc: e.transpose(out=psb(1)[:, kc * 128:(kc + 1) * 128], in_=xb[s2][:, kc * 128:(kc + 1) * 128], identity=J_b),
                  reads=[f"xb{s2}", "identb"], writes=["ps1"])
        for kc in range(8):
            S.add("act", lambda e, kc=kc: e.activation(out=hN[s3][:, kc, 1:129], in_=psb(0)[:, kc * 128:(kc + 1) * 128], func=AF.Identity,
                                                        scale=A1col[:, kc:kc + 1], bias=sh1col[:, kc:kc + 1]),
                  reads=["ps0", "A1col", "modcol"], writes=[f"hN{s3}"])
            S.add("act", lambda e, kc=kc: e.activation(out=hR[s3][:, kc, 1:129], in_=psb(1)[:, kc * 128:(kc + 1) * 128], func=AF.Identity,
                                                        scale=A1col[:, kc:kc + 1], bias=sh1col[:, kc:kc + 1]),
                  reads=["ps1", "A1col", "modcol"], writes=[f"hR{s3}"])
        if j > 0:
            sp = (j - 1) % NSL
            S.add("dve", lambda e: e.tensor_copy(out=hN[sp][:, :, 129:130], in_=hN[s3][:, :, 1:2]), reads=[f"hN{s3}"], writes=[f"hN{sp}"])
            S.add("dve", lambda e: e.tensor_copy(out=hN[s3][:, :, 0:1], in_=hN[sp][:, :, 128:129]), reads=[f"hN{sp}"], writes=[f"hN{s3}"])
            S.add("dve", lambda e: e.tensor_copy(out=hR[sp][:, :, 0:1], in_=hR[s3][:, :, 128:129]), reads=[f"hR{s3}"], writes=[f"hR{sp}"])
            S.add("dve", lambda e: e.tensor_copy(out=hR[s3][:, :, 129:130], in_=hR[sp][:, :, 1:2]), reads=[f"hR{sp}"], writes=[f"hR{s3}"])
        else:
            S.add("dve", lambda e: e.memset(hN[s3][:, :, 0:1], 0.0), writes=[f"hN{s3}"])
            S.add("dve", lambda e: e.memset(hR[s3][:, :, 129:130], 0.0), writes=[f"hR{s3}"])
        if j == NT - 1:
            S.add("dve", lambda e: e.memset(hN[s3][:, :, 129:130], 0.0), writes=[f"hN{s3}"])
            S.add("dve", lambda e: e.memset(hR[s3][:, :, 0:1], 0.0), writes=[f"hR{s3}"])

    bank_rr = [2]

    def nextbank():
        b = bank_rr[0]
        bank_rr[0] = 2 + (b - 2 + 1) % 6
        return b

    def inproj(j):
        s3, s2 = j % NSL, j % 2
        iN = 1 if j == 0 else (2 if j == NT - 1 else 0)
        iR = 3 if j == 0 else (4 if j == NT - 1 else 0)
        offR = [2, 1, 0]
        offN = [0, 1, 2]
        groups = []
        for h in range(2):
            groups.append(("R", h * 512, 512))
        groups.append(("N", 1024, 512))
        for c0, w in ((1536, 512), (2048, 256), (2304, 512), (2816, 512), (3328, 512), (3840, 512)):
            groups.append(("O", c0, w))
        for gi, (kind, c0, w) in enumerate(groups):
            b = nextbank()
            key = f"ps{b}"
            if kind in ("R", "N"):
                src, srck, offs, ii = (hR[s3], f"hR{s3}", offR, iR) if kind == "R" else (hN[s3], f"hN{s3}", offN, iN)
                first = True
                for sh in range(3):
                    for kc in range(8):
                        S.add("pe", lambda e, b=b, sh=sh, kc=kc, src=src, offs=offs, first=first, c0=c0, w=w:
                              e.matmul(ps[b][:, 0:w], lhsT=src[:, kc, offs[sh]:offs[sh] + 128], rhs=whs[sh][:, kc, c0:c0 + w], start=first, stop=False),
                              reads=[srck, "wb", "wh0", "wh2"], writes=[key])
                        first = False
                S.add("pe", lambda e, b=b, ii=ii, c0=c0, w=w: e.matmul(ps[b][:, 0:w], lhsT=indb[:, ii, :], rhs=brow4[:, c0:c0 + w], start=False, stop=True),
                      reads=["indb", "brow4"], writes=[key])
            else:
                for kc in range(8):
                    S.add("pe", lambda e, b=b, kc=kc, c0=c0, w=w: e.matmul(ps[b][:, 0:w], lhsT=hN[s3][:, kc, 1:129], rhs=wb[:, kc, c0:c0 + w], start=(kc == 0), stop=False),
                          reads=[f"hN{s3}", "wb"], writes=[key])
                S.add("pe", lambda e, b=b, c0=c0, w=w: e.matmul(ps[b][:, 0:w], lhsT=ones_b[0:1, :], rhs=bob[:, c0 - 1536:c0 - 1536 + w], start=False, stop=True),
                      reads=["ones_b", "bob"], writes=[key])
            if c0 >= 2304:
                S.add("act", lambda e, b=b, c0=c0, w=w: e.activation(out=stg[s2][:, c0:c0 + w], in_=ps[b][:, 0:w], func=AF.Sigmoid),
                      reads=[key], writes=["stg"])
            else:
                S.add("dve", lambda e, b=b, c0=c0, w=w: e.tensor_copy(out=stg[s2][:, c0:c0 + w], in_=ps[b][:, 0:w]),
                      reads=[key], writes=["stg"])
        st = stg[s2]
        for dst, c0, w in ((v_s, 0, 512), (x2_s, 512, 512), (x1_s, 1024, 512), (q_s, 1536, 512), (kv_s, 2048, 256), (gt_s, 2304, 2048)):
            dma("sp", dst.ap()[j], st[:, c0:c0 + w], reads=["stg"], writes=[dst.name], stream="st0")

    stage1(0)
    stage1(1)
    for j in range(NT):
        if j + 2 < NT:
            stage1(j + 2)
        inproj(j)

    if stop_after == "B":
        S.emit()
        return nc, dbg_out

    S.barrier()
    A.reset()
    TWO_PI = 2.0 * math.pi
    w1f = A.alloc([33, 64], F32)
    w2f = A.alloc([64, 64], F32)
    w3f = A.alloc([64, 64], F32)
    w4f = A.alloc([64, 2048], F32)
    hbc = A.alloc([64, 3], F32)
    hfc = A.alloc([64, 3], F32)
    hoff = A.alloc([64, 3], F32)
    fbc = A.alloc([128, 8], F32)
    dlc = A.alloc([128, 4], F32)
    l1 = A.alloc([128, 2], F32)
    cen = A.alloc([128, 2], F32)
    negpi = A.alloc([128, 1], F32)
    h3 = [A.alloc([64, L], F32) for _ in range(2)]
    zTs = A.alloc([33, L], F32)
    h1s = A.alloc([64, L], F32)
    h2s = A.alloc([64, L], F32)
    for t, d_ in ((w1f, hw1_d), (w2f, hw2_d), (w3f, hw3_d), (w4f, hw4_d), (hbc, hb_d), (hfc, hfr_d), (fbc, fb_d), (dlc, delta_d)):
        dma("sp", t, d_.ap(), writes=["fw"], stream="c0")
    S.add("dve", lambda e: e.memset(negpi, -math.pi), writes=["negpi"])
    hfs = A.alloc([64, 3], F32)
    S.add("dve", lambda e: e.tensor_scalar(out=hfs, in0=hfc, scalar1=1.0 / TWO_PI, scalar2=None, op0=ALU.mult), reads=["fw"], writes=["hfs"])
    S.add("dve", lambda e: e.tensor_tensor(out=hoff, in0=hbc, in1=hfs, op=ALU.mult), reads=["fw", "hfs"], writes=["hoff"])
    tmpc = [A.alloc([64, 512], F32) for _ in range(2)]
    tmpi = [A.alloc([64, 512], I32) for _ in range(2)]
    tmpf = [A.alloc([64, 512], F32) for _ in range(2)]
    for o2 in range(2):
        dma("sp", zTs, zT_d.ap()[:, o2, :], writes=["zTs"], stream="c0")
        layers = ((w1f, zTs, "zTs", h1s, "h1s", 33), (w2f, h1s, "h1s", h2s, "h2s", 64), (w3f, h2s, "h2s", h3[o2], f"h3{o2}", 64))
        for li, (wf, src, srck, dst, dstk, kk) in enumerate(layers):
            for ch in range(16):
                b = ch % 4
                tb = ch % 2
                sl = slice(ch * 512, (ch + 1) * 512)
                S.add("pe", lambda e, b=b, wf=wf, src=src, sl=sl, kk=kk: e.matmul(ps[b][0:64, :], lhsT=wf[0:kk, :], rhs=src[0:kk, sl], start=True, stop=True),
                      reads=["fw", srck], writes=[f"ps{b}"])
                S.add("dve", lambda e, b=b, tb=tb, li=li: e.tensor_scalar(out=tmpc[tb], in0=ps[b][0:64, :], scalar1=hfs[:, li:li + 1], scalar2=hoff[:, li:li + 1],
                                                                         op0=ALU.mult, op1=ALU.add),
                      reads=[f"ps{b}", "hfs", "hoff"], writes=[f"tmpc{tb}"])
                S.add("dve", lambda e, tb=tb: e.tensor_copy(out=tmpi[tb], in_=tmpc[tb]), reads=[f"tmpc{tb}"], writes=[f"tmpi{tb}"])
                S.add("pool", lambda e, tb=tb: e.tensor_copy(out=tmpf[tb], in_=tmpi[tb]), reads=[f"tmpi{tb}"], writes=[f"tmpf{tb}"])
                S.add("pool", lambda e, tb=tb: e.tensor_tensor(out=tmpf[tb], in0=tmpc[tb], in1=tmpf[tb], op=ALU.subtract), reads=[f"tmpc{tb}", f"tmpf{tb}"], writes=[f"tmpf{tb}"])
                S.add("act", lambda e, tb=tb, dst=dst, sl=sl: e.activation(out=dst[:, sl], in_=tmpf[tb], func=AF.Sin, scale=TWO_PI - 1e-6),
                      reads=[f"tmpf{tb}"], writes=[dstk])
    if "h3" in dbg:
        dbg_out["h3"] = nc.dram_tensor("dbg_h3", [2, 64, L], F32, kind="ExternalOutput")
        for o2 in range(2):
            dma("sp", dbg_out["h3"].ap()[o2], h3[o2], reads=[f"h3{o2}"], stream="c0")
    S.barrier()
    A.off -= (33 + 64 + 64) * 0
    A.off = A.off - 3 * L * 4 - 6 * 512 * 4 - 32
    wnd = A.alloc([128, L], F32)
    hfull = A.alloc([128, L], F32)
    Abuf = A.alloc([128, 16384], BF16)
    S.add("dve", lambda e: e.memset(Abuf[:, 16382:16384], 0.0), writes=["Abuf"])
    for o in range(2):
        for g in range(4):
            parts = ((1, 1 - o), (0, o))
            for pi_, (o2, dr) in enumerate(parts):
                col0 = o * 1024 + dr * 512 + g * 128
                dma("sp", wnd, t01_d.ap()[o2:o2 + 1, :].partition_broadcast(128), writes=["wnd"], stream="c0")
                S.add("act", lambda e, g=g: e.activation(out=wnd, in_=wnd, func=AF.Exp, scale=dlc[:, g:g + 1]), reads=["wnd", "fw"], writes=["wnd"])
                for ch in range(16):
                    b = ch % 4
                    sl = slice(ch * 512, (ch + 1) * 512)
                    S.add("pe", lambda e, b=b, col0=col0, o2=o2, sl=sl: e.matmul(ps[b][:, :], lhsT=w4f[:, col0:col0 + 128], rhs=h3[o2][:, sl], start=True, stop=True),
                          reads=["fw", f"h3{o2}"], writes=[f"ps{b}"])
                    S.add("dve", lambda e, b=b, sl=sl: e.tensor_tensor(out=hfull[:, sl], in0=ps[b][:, :], in1=wnd[:, sl], op=ALU.mult),
                          reads=[f"ps{b}", "wnd"], writes=["hfull"])
                S.add("act", lambda e, pi_=pi_: e.activation(out=wnd, in_=hfull, func=AF.Abs, accum_out=l1[:, pi_:pi_ + 1]), reads=["hfull"], writes=["wnd", "l1"])
                S.add("dve", lambda e, pi_=pi_: e.tensor_scalar(out=l1[:, pi_:pi_ + 1], in0=l1[:, pi_:pi_ + 1], scalar1=1e-6, scalar2=None, op0=ALU.add), reads=["l1"], writes=["l1"])
                S.add("dve", lambda e, pi_=pi_: e.reciprocal(out=l1[:, pi_:pi_ + 1], in_=l1[:, pi_:pi_ + 1]), reads=["l1"], writes=["l1"])
                if pi_ == 0:
                    S.add("dve", lambda e: e.tensor_scalar(out=Abuf[:, 0:8192], in0=hfull, scalar1=l1[:, 0:1], scalar2=None, op0=ALU.mult), reads=["hfull", "l1"], writes=["Abuf"])
                    S.add("dve", lambda e: e.tensor_scalar(out=cen[:, 0:1], in0=hfull[:, 8191:8192], scalar1=l1[:, 0:1], scalar2=None, op0=ALU.mult), reads=["hfull", "l1"], writes=["cen"])
                else:
                    S.add("dve", lambda e: e.tensor_scalar(out=Abuf[:, 8191:16383], in0=hfull, scalar1=l1[:, 1:2], scalar2=None, op0=ALU.mult), reads=["hfull", "l1"], writes=["Abuf"])
                    S.add("dve", lambda e: e.tensor_scalar(out=cen[:, 1:2], in0=hfull[:, 0:1], scalar1=l1[:, 1:2], scalar2=None, op0=ALU.mult), reads=["hfull", "l1"], writes=["cen"])
            fcol = o * 4 + g
            S.add("dve", lambda e: e.tensor_tensor(out=cen[:, 0:1], in0=cen[:, 0:1], in1=cen[:, 1:2], op=ALU.add), reads=["cen"], writes=["cen"])
            S.add("dve", lambda e, fcol=fcol: e.tensor_tensor(out=Abuf[:, 8191:8192], in0=cen[:, 0:1], in1=fbc[:, fcol:fcol + 1], op=ALU.add), reads=["cen", "fw", "Abuf"], writes=["Abuf"])
            dma("sp", a_s.ap()[o, g * 128:(g + 1) * 128, :], Abuf, reads=["Abuf"], writes=["a_s"], stream="c0")

    if stop_after == "A2":
        S.emit()
        return nc, dbg_out

    S.barrier()
    A.reset()
    NSTRIP = 3
    SW = 127 * 128
    vg = A.alloc([128, NT, 128], BF16)
    x1g = A.alloc([128, NT, 128], BF16)
    x2g = A.alloc([128, NT, 128], BF16)
    wgb = A.alloc([128, NT, 128], BF16)
    hyo = A.alloc([128, NT, 128], BF16)
    strips = [A.alloc([128, SW], BF16) for _ in range(NSTRIP)]
    sctr = [0]
    cb = [0]
    dlist = [0] + [d for d in range(-63, 64) if d != 0]

    def conv(o, g, src, srck, gate, gatek, dst, dstk):
        for c in range(128):
            si = sctr[0] % NSTRIP
            sctr[0] += 1
            ch = g * 128 + c
            base = a_s.ap()[o, ch:ch + 1, 0:1]
            hk = bass.AP(a_s, base.offset, [[1, 128], [1, SW]])
            dma("sp" if si % 2 == 0 else "act", strips[si], hk, reads=["a_s"], writes=[f"strip{si}"], stream=f"strip{si}")
            b = 4 + cb[0] % 4
            cb[0] += 1
            for n_, d in enumerate(dlist):
                i0, i1 = max(0, d), min(64, 64 + d)
                off = 128 * (d + 63) if o == 0 else 128 * (63 - d)
                S.add("pe", lambda e, b=b, si=si, off=off, i0=i0, i1=i1, d=d, c=c, n_=n_:
                      e.matmul(ps[b][:, i0:i1], lhsT=strips[si][:, off:off + 128], rhs=src[:, i0 - d:i1 - d, c], start=(n_ == 0), stop=(n_ == 126)),
                      reads=[f"strip{si}", srck], writes=[f"ps{b}"])
            S.add("dve", lambda e, b=b, c=c: e.tensor_tensor(out=dst[:, :, c], in0=ps[b][:, 0:64], in1=gate[:, :, c], op=ALU.mult),
                  reads=[f"ps{b}", gatek], writes=[dstk])

    for g in range(4):
        cs = slice(g * 128, (g + 1) * 128)
        dma("sp", vg, v_s.ap()[:, :, cs].rearrange("j p c -> p j c"), reads=["v_s"], writes=["vg"], stream="hl0")
        dma("sp", x1g, x1_s.ap()[:, :, cs].rearrange("j p c -> p j c"), reads=["x1_s"], writes=["x1g"], stream="hl1")
        dma("sp", x2g, x2_s.ap()[:, :, cs].rearrange("j p c -> p j c"), reads=["x2_s"], writes=["x2g"], stream="hl2")
        conv(0, g, vg, "vg", x1g, "x1g", wgb, "wgb")
        conv(1, g, wgb, "wgb", x2g, "x2g", hyo, "hyo")
        dma("sp", hy_s.ap()[:, :, cs].rearrange("j p c -> p j c"), hyo, reads=["hyo"], writes=["hy_s"], stream="hl3")

    if stop_after == "C":
        S.emit()
        return nc, dbg_out

    S.barrier()
    A.reset()
    qT = A.alloc([128, 4, L], BF16)
    kTA = A.alloc([128, L], BF16)
    kTB = A.alloc([128, L], BF16)
    Vx = A.alloc([128, NT, 2, 128], BF16)
    ropeT = A.alloc([128, NT, 64], F32)
    gq = A.alloc([128, 64], F32)
    gk = A.alloc([128, 64], F32)
    dma("sp", ropeT, rope_d.ap(), writes=["ropeT"], stream="c0")
    dma("sp", gq, qg_d.ap().partition_broadcast(128), writes=["gq"], stream="c0")
    dma("sp", gk, kg_d.ap().partition_broadcast(128), writes=["gk"], stream="c0")
    S.add("pool", lambda e: e.memset(Vx, 0.0), writes=["Vx"])
    S.add("pool", lambda e: e.memset(Vx[:, :, :, 64:65], 1.0), writes=["Vx"])
    S.add("dve", lambda e: e.memset(kTA[64:128, :], 0.0), writes=["kT"])
    S.add("dve", lambda e: e.memset(kTB[0:64, :], 0.0), writes=["kT"])
    qb = [A.alloc([128, 512], BF16) for _ in range(2)]
    kvb = [A.alloc([128, 256], BF16) for _ in range(2)]
    wf_ = A.alloc([128, 512], F32)
    wsq = A.alloc([128, 512], F32)
    wss = A.alloc([128, 8], F32)
    wn = A.alloc([128, 512], F32)
    wtA = A.alloc([128, 256], F32)
    wtB = A.alloc([128, 256], F32)
    qr = A.alloc([128, 512], BF16)
    kr = A.alloc([128, 128], BF16)

    def normrope(srcv, nh, gain, gaink, j, outv, tag):
        W = nh * 64
        f, sq_, ss_, n_ = wf_[:, 0:W], wsq[:, 0:W], wss[:, 0:nh], wn[:, 0:W]
        f3 = f.rearrange("p (h d) -> p h d", h=nh)
        n3 = n_.rearrange("p (h d) -> p h d", h=nh)
        S.add("dve", lambda e: e.tensor_copy(out=f, in_=srcv), reads=[tag], writes=["wf"])
        S.add("dve", lambda e: e.tensor_tensor(out=sq_, in0=f, in1=f, op=ALU.mult), reads=["wf"], writes=["wsq"])
        S.add("dve", lambda e: e.tensor_reduce(out=ss_, in_=sq_.rearrange("p (h d) -> p h d", h=nh), axis=AX.X, op=ALU.add), reads=["wsq"], writes=["wss"])
        S.add("act", lambda e: e.activation(out=ss_, in_=ss_, func=AF.Sqrt, scale=1.0 / 64, bias=1e-6), reads=["wss"], writes=["wss"])
        S.add("dve", lambda e: e.reciprocal(out=ss_, in_=ss_), reads=["wss"], writes=["wss"])
        S.add("dve", lambda e: e.tensor_tensor(out=n3, in0=f3, in1=ss_.unsqueeze(2).broadcast_to([128, nh, 64]), op=ALU.mult), reads=["wf", "wss"], writes=["wn"])
        S.add("dve", lambda e: e.tensor_tensor(out=n3, in0=n3, in1=gain.unsqueeze(1).broadcast_to([128, nh, 64]), op=ALU.mult), reads=["wn", gaink], writes=["wn"])
        o3 = outv.rearrange("p (h d) -> p h d", h=nh)
        for hf in range(2):
            nh4 = n3[:, :, hf * 32:(hf + 1) * 32].rearrange("p h (u d) -> p h u d", u=2)
            oh4 = o3[:, :, hf * 32:(hf + 1) * 32].rearrange("p h (u d) -> p h u d", u=2)
            cosb = ropeT[:, j, hf * 16:(hf + 1) * 16]
            sinb = ropeT[:, j, 32 + hf * 16:32 + (hf + 1) * 16]
            tA = wtA[:, 0:nh * 32].rearrange("p (h u d) -> p h u d", h=nh, u=2)
            tB = wtB[:, 0:nh * 32].rearrange("p (h u d) -> p h u d", h=nh, u=2)
            S.add("dve", lambda e, nh4=nh4, tA=tA, cosb=cosb: e.tensor_tensor(out=tA, in0=nh4, in1=cosb.unsqueeze(1).unsqueeze(1).broadcast_to([128, nh, 2, 16]), op=ALU.mult),
                  reads=["wn", "ropeT"], writes=["wtA"])
            S.add("dve", lambda e, nh4=nh4, tB=tB, sinb=sinb: e.tensor_tensor(out=tB[:, :, 0, :], in0=nh4[:, :, 1, :], in1=sinb.unsqueeze(1).broadcast_to([128, nh, 16]), op=ALU.mult),
                  reads=["wn", "ropeT"], writes=["wtB"])
            S.add("dve", lambda e, nh4=nh4, tB=tB, sinb=sinb: e.tensor_tensor(out=tB[:, :, 1, :], in0=nh4[:, :, 0, :], in1=sinb.unsqueeze(1).broadcast_to([128, nh, 16]), op=ALU.mult),
                  reads=["wn", "ropeT", "wtB"], writes=["wtB"])
            S.add("dve", lambda e, oh4=oh4, tA=tA, tB=tB: e.tensor_tensor(out=oh4[:, :, 0, :], in0=tA[:, :, 0, :], in1=tB[:, :, 0, :], op=ALU.subtract),
                  reads=["wtA", "wtB"], writes=[tag + "r"])
            S.add("dve", lambda e, oh4=oh4, tA=tA, tB=tB: e.tensor_tensor(out=oh4[:, :, 1, :], in0=tA[:, :, 1, :], in1=tB[:, :, 1, :], op=ALU.add),
                  reads=["wtA", "wtB", tag + "r"], writes=[tag + "r"])

    for j in range(NT):
        s2 = j % 2
        dma("sp", qb[s2], q_s.ap()[j], reads=["q_s"], writes=[f"qb{s2}"], stream=f"ql{s2}")
        dma("sp", kvb[s2], kv_s.ap()[j], reads=["kv_s"], writes=[f"kvb{s2}"], stream=f"kl{s2}")
        normrope(qb[s2], 8, gq, "gq", j, qr, f"qb{s2}")
        for g in range(4):
            S.add("pe", lambda e, g=g: e.transpose(out=psb(0)[:, g * 128:(g + 1) * 128], in_=qr[:, g * 128:(g + 1) * 128], identity=I_b),
                  reads=[f"qb{s2}r", "identb"], writes=["ps0"])
        S.add("act", lambda e, j=j: e.activation(out=qT[:, :, j * 128:(j + 1) * 128], in_=psb(0)[:, 0:512].rearrange("p (g t) -> p g t", g=4), func=AF.Copy),
              reads=["ps0"], writes=["qT"])
        normrope(kvb[s2][:, 0:128], 2, gk, "gk", j, kr, f"kvb{s2}")
        S.add("pe", lambda e: e.transpose(out=psb(1)[:, 0:128], in_=kr, identity=I_b), reads=[f"kvb{s2}r", "identb"], writes=["ps1"])
        S.add("act", lambda e, j=j: e.activation(out=kTA[0:64, j * 128:(j + 1) * 128], in_=psb(1)[0:64, 0:128], func=AF.Copy), reads=["ps1"], writes=["kT"])
        S.add("act", lambda e, j=j: e.activation(out=kTB[64:128, j * 128:(j + 1) * 128], in_=psb(1)[64:128, 0:128], func=AF.Copy), reads=["ps1"], writes=["kT"])
        S.add("pool", lambda e, j=j, s2=s2: e.tensor_copy(out=Vx[:, j, :, 0:64], in_=kvb[s2][:, 128:256].rearrange("p (h d) -> p h d", h=2)),
              reads=[f"kvb{s2}"], writes=["Vx"])

    PT = [A.alloc([128, 512], BF16) for _ in range(4)]
    rc = A.alloc([128, 8], F32)
    oT = [A.alloc([65, 512], F32) for _ in range(2)]
    attst = [A.alloc([128, 4, 128], BF16) for _ in range(2)]
    iters = [(g, qg, kt, hh) for g in range(4) for qg in range(16) for kt in range(NT) for hh in range(2)]
    NI = len(iters)
    LA = 2

    def emit_S(i):
        g, qg, kt, hh = iters[i]
        sb_ = i % 4
        kTx = kTA if hh == 0 else kTB
        S.add("pe", lambda e: e.matmul(ps[sb_][:, :], lhsT=kTx[:, kt * 128:(kt + 1) * 128], rhs=qT[:, g, qg * 512:(qg + 1) * 512], start=True, stop=True),
              reads=["kT", "qT"], writes=[f"ps{sb_}"])
        S.add("act", lambda e: e.activation(out=PT[sb_], in_=ps[sb_][:, :], func=AF.Exp, scale=0.125), reads=[f"ps{sb_}"], writes=[f"PT{sb_}"])

    def emit_PV(i):
        g, qg, kt, hh = iters[i]
        sb_ = i % 4
        it_ = g * 16 + qg
        ob = 4 + 2 * (it_ % 2)
        S.add("pe", lambda e: e.matmul(ps[ob + hh][:, :], lhsT=Vx[:, kt, hh, :], rhs=PT[sb_], start=(kt == 0), stop=(kt == NT - 1)),
              reads=[f"PT{sb_}", "Vx"], writes=[f"ps{ob + hh}"])
        if kt == NT - 1 and hh == 1:
            a2 = it_ % 2
            for h2_ in range(2):
                S.add("act", lambda e, h2_=h2_: e.activation(out=oT[h2_], in_=ps[ob + h2_][0:65, :], func=AF.Copy), reads=[f"ps{ob + h2_}"], writes=[f"oT{h2_}"])
                for qs in range(4):
                    S.add("pe", lambda e, h2_=h2_, qs=qs: e.transpose(out=ps[ob + h2_][:, qs * 65:(qs + 1) * 65], in_=oT[h2_][0:65, qs * 128:(qs + 1) * 128], identity=identf[0:65, 0:65]),
                          reads=[f"oT{h2_}", "identf"], writes=[f"ps{ob + h2_}"])
                o4 = ps[ob + h2_][:, 0:260].rearrange("p (q d) -> p q d", q=4)
                S.add("dve", lambda e, o4=o4, h2_=h2_: e.reciprocal(out=rc[:, h2_ * 4:(h2_ + 1) * 4].unsqueeze(2), in_=o4[:, :, 64:65]), reads=[f"ps{ob + h2_}"], writes=["rc"])
                S.add("dve", lambda e, o4=o4, h2_=h2_: e.tensor_tensor(out=attst[a2][:, :, h2_ * 64:(h2_ + 1) * 64], in0=o4[:, :, 0:64],
                                                                     in1=rc[:, h2_ * 4:(h2_ + 1) * 4].unsqueeze(2).broadcast_to([128, 4, 64]), op=ALU.mult),
                      reads=[f"ps{ob + h2_}", "rc"], writes=[f"attst{a2}"])
            dma("sp", att_s.ap()[qg * 4:(qg + 1) * 4, :, g * 128:(g + 1) * 128].rearrange("t p c -> p t c"), attst[a2], reads=[f"attst{a2}"], writes=["att_s"], stream=f"as{a2}")

    for i in range(NI + LA):
        if i < NI:
            emit_S(i)
        if i - LA >= 0:
            emit_PV(i - LA)

    if stop_after == "D":
        S.emit()
        return nc, dbg_out

    S.barrier()
    A.reset()
    why_sb = A.alloc([128, 4, D], BF16)
    wat_sb = A.alloc([128, 4, D], BF16)
    wout_sb = A.alloc([128, 8, D], BF16)
    wr_sb = A.alloc([128, 8, 16], F32)
    A2row = A.alloc([128, D], F32)
    sh2row = A.alloc([128, D], F32)
    gfb = A.alloc([128, D], F32)
    dma("pool", why_sb, why_d.ap(), writes=["why_sb"], stream="wb0")
    dma("pool", wat_sb, wat_d.ap(), writes=["wat_sb"], stream="wb1")
    dma("pool", wout_sb, wout_d.ap(), writes=["wout_sb"], stream="wb0")
    dma("sp", wr_sb, wr_d.ap(), writes=["wr_sb"], stream="c0")
    dma("sp", A2row, mod_s.ap()[:, 4096:5120].partition_broadcast(128), reads=["mod_s"], writes=["A2row"], stream="c0")
    dma("sp", sh2row, mod_s.ap()[:, 3072:4096].partition_broadcast(128), reads=["mod_s"], writes=["sh2row"], stream="c0")
    dma("sp", gfb, gffn_d.ap().partition_broadcast(128), writes=["gfb"], stream="c0")
    S.add("dve", lambda e: e.scalar_tensor_tensor(out=A2row, in0=A2row, scalar=1.0, in1=gfb, op0=ALU.add, op1=ALU.mult), reads=["A2row", "gfb"], writes=["A2row"])
    hyb = [A.alloc([128, 512], BF16) for _ in range(2)]
    atb = [A.alloc([128, 512], BF16) for _ in range(2)]
    gtb = [A.alloc([128, 2048], BF16) for _ in range(2)]
    xe_ = [A.alloc([128, D], F32) for _ in range(2)]
    hatT_ = [A.alloc([128, 8, 128], BF16) for _ in range(2)]
    t1_ = [A.alloc([128, D], F32) for _ in range(2)]
    t2_ = [A.alloc([128, D], F32) for _ in range(2)]
    Pb_ = [A.alloc([128, D], BF16) for _ in range(2)]
    PTt_ = [A.alloc([128, 8, 128], BF16) for _ in range(2)]
    x1t_ = [A.alloc([128, D], F32) for _ in range(2)]
    junk2 = A.alloc([128, D], BF16)
    ss2 = A.alloc([128, 1], F32)
    xs2_ = [A.alloc([128, D], F32) for _ in range(2)]
    h2b_ = [A.alloc([128, D], BF16) for _ in range(2)]
    h2T_ = [A.alloc([128, 8, 128], F32) for _ in range(2)]
    mx = A.alloc([128, 1], F32)
    sm = A.alloc([128, 1], F32)
    ex = A.alloc([128, 16], F32)
    def efirst(j):
        s2 = j % 2
        dma("sp", hyb[s2], hy_s.ap()[j], reads=["hy_s"], writes=[f"hyb{s2}"], stream=f"e0{s2}")
        dma("sp", atb[s2], att_s.ap()[j], reads=["att_s"], writes=[f"atb{s2}"], stream=f"e1{s2}")
        dma("sp", gtb[s2], gt_s.ap()[j], reads=["gt_s"], writes=[f"gtb{s2}"], stream=f"e2{s2}")
        dma("sp", xe_[s2], x_d.ap()[j * 128:(j + 1) * 128, :], writes=[f"xe{s2}"], stream=f"e3{s2}")
        for cc in range(4):
            S.add("pe", lambda e, cc=cc: e.transpose(out=psb(0)[:, cc * 128:(cc + 1) * 128], in_=hyb[s2][:, cc * 128:(cc + 1) * 128], identity=J_b),
                  reads=[f"hyb{s2}", "identb"], writes=["ps0"])
        for cc in range(4):
            S.add("pe", lambda e, cc=cc: e.transpose(out=psb(0)[:, 512 + cc * 128:512 + (cc + 1) * 128], in_=atb[s2][:, cc * 128:(cc + 1) * 128], identity=I_b),
                  reads=[f"atb{s2}", "identb"], writes=["ps0"])
        S.add("act", lambda e: e.activation(out=hatT_[s2].rearrange("p k t -> p (k t)"), in_=psb(0)[:, :], func=AF.Copy), reads=["ps0"], writes=[f"hatT{s2}"])
        for h in range(2):
            for cc in range(4):
                S.add("pe", lambda e, h=h, cc=cc: e.matmul(ps[2 + h][:, :], lhsT=hatT_[s2][:, cc, :], rhs=why_sb[:, cc, h * 512:(h + 1) * 512], start=(cc == 0), stop=(cc == 3)),
                      reads=[f"hatT{s2}", "why_sb"], writes=[f"ps{2 + h}"])
            for cc in range(4):
                S.add("pe", lambda e, h=h, cc=cc: e.matmul(ps[4 + h][:, :], lhsT=hatT_[s2][:, 4 + cc, :], rhs=wat_sb[:, cc, h * 512:(h + 1) * 512], start=(cc == 0), stop=(cc == 3)),
                      reads=[f"hatT{s2}", "wat_sb"], writes=[f"ps{4 + h}"])
        for h in range(2):
            hs = slice(h * 512, (h + 1) * 512)
            S.add("dve", lambda e, h=h, hs=hs: e.tensor_tensor(out=t1_[s2][:, hs], in0=ps[2 + h][:, :], in1=gtb[s2][:, hs], op=ALU.mult), reads=[f"ps{2 + h}", f"gtb{s2}"], writes=[f"t1{s2}"])
            S.add("dve", lambda e, h=h, hs=hs: e.tensor_tensor(out=t2_[s2][:, hs], in0=ps[4 + h][:, :], in1=gtb[s2][:, 1024 + h * 512:1024 + (h + 1) * 512], op=ALU.mult),
                  reads=[f"ps{4 + h}", f"gtb{s2}"], writes=[f"t2{s2}"])
        S.add("pool", lambda e: e.tensor_tensor(out=Pb_[s2], in0=t1_[s2], in1=t2_[s2], op=ALU.add), reads=[f"t1{s2}", f"t2{s2}"], writes=[f"Pb{s2}"])
    def esecond(j):
        s2 = j % 2
        for kc in range(8):
            S.add("pe", lambda e, kc=kc: e.transpose(out=psb(1)[:, kc * 128:(kc + 1) * 128], in_=Pb_[s2][:, kc * 128:(kc + 1) * 128], identity=I_b),
                  reads=[f"Pb{s2}", "identb"], writes=["ps1"])
        S.add("act", lambda e: e.activation(out=PTt_[s2].rearrange("p k t -> p (k t)"), in_=psb(1)[:, :], func=AF.Copy), reads=["ps1"], writes=[f"PTt{s2}"])
        for h in range(2):
            for kc in range(8):
                S.add("pe", lambda e, h=h, kc=kc: e.matmul(ps[6 + h][:, :], lhsT=PTt_[s2][:, kc, :], rhs=wout_sb[:, kc, h * 512:(h + 1) * 512], start=(kc == 0), stop=(kc == 7)),
                      reads=[f"PTt{s2}", "wout_sb"], writes=[f"ps{6 + h}"])
        for h in range(2):
            hs = slice(h * 512, (h + 1) * 512)
            S.add("dve", lambda e, h=h, hs=hs: e.tensor_tensor(out=t1_[s2][:, hs], in0=ps[6 + h][:, :], in1=gt1b[:, hs], op=ALU.mult), reads=[f"ps{6 + h}", "gt1b"], writes=[f"t1{s2}"])
        S.add("pool", lambda e: e.tensor_tensor(out=x1t_[s2], in0=t1_[s2], in1=xe_[s2], op=ALU.add), reads=[f"t1{s2}", f"xe{s2}"], writes=[f"x1t{s2}"])
        dma("sp", out_d.ap()[j * 128:(j + 1) * 128, :], x1t_[s2], reads=[f"x1t{s2}"], writes=["out_d"], stream="eo")
        S.add("act", lambda e: e.activation(out=junk2, in_=x1t_[s2], func=AF.Square, accum_out=ss2), reads=[f"x1t{s2}"], writes=["junk2", "ss2"])
        S.add("act", lambda e: e.activation(out=ss2, in_=ss2, func=AF.Sqrt, scale=1.0 / D, bias=1e-6), reads=["ss2"], writes=["ss2"])
        S.add("dve", lambda e: e.reciprocal(out=ss2, in_=ss2), reads=["ss2"], writes=["ss2"])
        S.add("dve", lambda e: e.scalar_tensor_tensor(out=xs2_[s2], in0=x1t_[s2], scalar=ss2[:, 0:1], in1=A2row, op0=ALU.mult, op1=ALU.mult), reads=[f"x1t{s2}", "ss2", "A2row"], writes=[f"xs2{s2}"])
        S.add("pool", lambda e: e.tensor_tensor(out=h2b_[s2], in0=xs2_[s2], in1=sh2row, op=ALU.add), reads=[f"xs2{s2}", "sh2row"], writes=[f"h2b{s2}"])
        dma("sp", h2_s.ap()[j * 128:(j + 1) * 128, :], h2b_[s2], reads=[f"h2b{s2}"], writes=["h2_s"], stream="eh")
        S.add("pool", lambda e: e.tensor_tensor(out=xs2_[s2], in0=xs2_[s2], in1=sh2row, op=ALU.add), reads=[f"xs2{s2}", "sh2row", f"h2b{s2}"], writes=[f"xs2{s2}"])
        for hh in range(2):
            for kc in range(4):
                S.add("pe", lambda e, kc=kc, hh=hh: e.transpose(out=ps[6 + hh][:, kc * 128:(kc + 1) * 128], in_=xs2_[s2][:, (hh * 4 + kc) * 128:(hh * 4 + kc + 1) * 128], identity=identf[:, 0:128]),
                      reads=[f"xs2{s2}", "identf"], writes=[f"ps{6 + hh}"])
            S.add("act", lambda e, hh=hh: e.activation(out=h2T_[s2][:, hh * 4:(hh + 1) * 4, :].rearrange("p k t -> p (k t)"), in_=ps[6 + hh][:, :], func=AF.Copy), reads=[f"ps{6 + hh}"], writes=[f"h2T{s2}"])
        for kc in range(8):
            S.add("pe", lambda e, kc=kc: e.matmul(ps[1][:, 0:16], lhsT=h2T_[s2][:, kc, :], rhs=wr_sb[:, kc, :], start=(kc == 0), stop=(kc == 7)),
                  reads=[f"h2T{s2}", "wr_sb"], writes=["ps1"])
        S.add("dve", lambda e: e.tensor_reduce(out=mx, in_=ps[1][:, 0:16], axis=AX.X, op=ALU.max), reads=["ps1"], writes=["mx"])
        S.add("dve", lambda e: e.tensor_scalar(out=mx, in0=mx, scalar1=-1.0, scalar2=None, op0=ALU.mult), reads=["mx"], writes=["mx"])
        S.add("act", lambda e: e.activation(out=ex, in_=ps[1][:, 0:16], func=AF.Exp, bias=mx[:, 0:1], accum_out=sm), reads=["ps1", "mx"], writes=["ex", "sm"])
        S.add("dve", lambda e: e.reciprocal(out=sm, in_=sm), reads=["sm"], writes=["sm"])
        S.add("dve", lambda e, j=j: e.tensor_scalar(out=aff_sb[:, j, :], in0=ex, scalar1=sm[:, 0:1], scalar2=None, op0=ALU.mult), reads=["ex", "sm"], writes=["aff_sb"])
    efirst(0)
    for j in range(NT):
        if j + 1 < NT:
            efirst(j + 1)
        esecond(j)
    if "aff" in dbg:
        dbg_out["aff"] = nc.dram_tensor("dbg_aff", [128, NT, 16], F32, kind="ExternalOutput")
        dma("sp", dbg_out["aff"].ap(), aff_sb, reads=["aff_sb"], stream="c0")

    if stop_after == "E":
        S.emit()
        return nc, dbg_out

    S.barrier()
    A.reset()
    lo = A.alloc([128, 16], F32)
    hi = A.alloc([128, 16], F32)
    mid = A.alloc([128, 16], F32)
    cnt = A.alloc([128, 16], F32)
    sel = A.alloc([128, 16], F32)
    dl = A.alloc([128, 16], F32)
    dh_ = A.alloc([128, 16], F32)
    cmpb = A.alloc([128, NT, 16], F32)
    maskb = A.alloc([128, NT, 16], F32)
    S.add("dve", lambda e: e.memset(lo, 0.0), writes=["lo"])
    S.add("dve", lambda e: e.memset(hi, 1.0), writes=["hi"])
    for _ in range(30):
        S.add("dve", lambda e: e.tensor_tensor(out=mid, in0=lo, in1=hi, op=ALU.add), reads=["lo", "hi"], writes=["mid"])
        S.add("dve", lambda e: e.tensor_scalar(out=mid, in0=mid, scalar1=0.5, scalar2=None, op0=ALU.mult), reads=["mid"], writes=["mid"])
        S.add("dve", lambda e: e.tensor_tensor(out=cmpb, in0=aff_sb, in1=mid.unsqueeze(1).broadcast_to([128, NT, 16]), op=ALU.is_gt), reads=["aff_sb", "mid"], writes=["cmpb"])
        S.add("dve", lambda e: e.tensor_reduce(out=cnt, in_=cmpb.rearrange("p j e -> p e j"), axis=AX.X, op=ALU.add), reads=["cmpb"], writes=["cnt"])
        S.add("pe", lambda e: e.matmul(ps[0][:, 0:16], lhsT=ones_f, rhs=cnt, start=True, stop=True), reads=["cnt", "ones_f"], writes=["ps0"])
        S.add("dve", lambda e: e.tensor_single_scalar(out=sel, in_=ps[0][:, 0:16], scalar=1023.5, op=ALU.is_ge), reads=["ps0"], writes=["sel"])
        S.add("dve", lambda e: e.tensor_tensor(out=dl, in0=mid, in1=lo, op=ALU.subtract), reads=["mid", "lo"], writes=["dl"])
        S.add("dve", lambda e: e.tensor_tensor(out=dl, in0=dl, in1=sel, op=ALU.mult), reads=["dl", "sel"], writes=["dl"])
        S.add("dve", lambda e: e.tensor_tensor(out=dh_, in0=hi, in1=mid, op=ALU.subtract), reads=["mid", "hi"], writes=["dh"])
        S.add("dve", lambda e: e.tensor_tensor(out=dh_, in0=dh_, in1=sel, op=ALU.mult), reads=["dh", "sel"], writes=["dh"])
        S.add("dve", lambda e: e.tensor_tensor(out=lo, in0=lo, in1=dl, op=ALU.add), reads=["lo", "dl"], writes=["lo"])
        S.add("dve", lambda e: e.tensor_tensor(out=hi, in0=mid, in1=dh_, op=ALU.add), reads=["mid", "dh"], writes=["hi"])
    S.add("dve", lambda e: e.tensor_tensor(out=maskb, in0=aff_sb, in1=lo.unsqueeze(1).broadcast_to([128, NT, 16]), op=ALU.is_gt), reads=["aff_sb", "lo"], writes=["maskb"])
    tri = A.alloc([128, 128], F32)
    iota_sb = A.alloc([128, 1024], F32)
    tokid = A.alloc([128, NT], F32)
    csb = A.alloc([128, NT, 16], F32)
    totb = A.alloc([128, NT, 16], F32)
    sca = A.alloc([128, NT, 16], F32)
    scb = A.alloc([128, NT, 16], F32)
    pos = A.alloc([128, NT, 16], F32)
    dma("sp", tri, tri_d.ap(), writes=["tri"], stream="c0")
    dma("sp", iota_sb, iota_d.ap(), writes=["iota_sb"], stream="c0")
    dma("sp", tokid, tok_d.ap(), writes=["tokid"], stream="c0")
    mflat = maskb.rearrange("p j e -> p (j e)")
    for h in range(2):
        S.add("pe", lambda e, h=h: e.matmul(ps[2 + h][:, :], lhsT=tri, rhs=mflat[:, h * 512:(h + 1) * 512], start=True, stop=True), reads=["tri", "maskb"], writes=[f"ps{2 + h}"])
        S.add("pe", lambda e, h=h: e.matmul(ps[4 + h][:, :], lhsT=ones_f, rhs=mflat[:, h * 512:(h + 1) * 512], start=True, stop=True), reads=["ones_f", "maskb"], writes=[f"ps{4 + h}"])
        S.add("dve", lambda e, h=h: e.tensor_copy(out=csb.rearrange("p j e -> p (j e)")[:, h * 512:(h + 1) * 512], in_=ps[2 + h][:, :]), reads=[f"ps{2 + h}"], writes=["csb"])
        S.add("dve", lambda e, h=h: e.tensor_copy(out=totb.rearrange("p j e -> p (j e)")[:, h * 512:(h + 1) * 512], in_=ps[4 + h][:, :]), reads=[f"ps{4 + h}"], writes=["totb"])
    cur, curk, oth, othk = totb, "totb", sca, "sca"
    for sft in (1, 2, 4, 8, 16, 32):
        S.add("dve", lambda e, cur=cur, oth=oth, sft=sft: e.tensor_tensor(out=oth[:, sft:, :], in0=cur[:, sft:, :], in1=cur[:, 0:NT - sft, :], op=ALU.add), reads=[curk], writes=[othk])
        S.add("dve", lambda e, cur=cur, oth=oth, sft=sft: e.tensor_copy(out=oth[:, 0:sft, :], in_=cur[:, 0:sft, :]), reads=[curk, othk], writes=[othk])
        if cur is totb:
            cur, curk, oth, othk = sca, "sca", scb, "scb"
        else:
            cur, curk, oth, othk = oth, othk, cur, curk
    S.add("dve", lambda e, cur=cur: e.tensor_tensor(out=pos, in0=cur, in1=totb, op=ALU.subtract), reads=[curk, "totb"], writes=["pos"])
    S.add("dve", lambda e: e.tensor_tensor(out=pos, in0=pos, in1=csb, op=ALU.add), reads=["pos", "csb"], writes=["pos"])
    S.add("dve", lambda e: e.tensor_scalar(out=pos, in0=pos, scalar1=-1.0, scalar2=None, op0=ALU.add), reads=["pos"], writes=["pos"])
    idx_all = A.alloc([128, 16, 8], I32)
    w_all = A.alloc([128, 16, 8], F32)
    vals5 = [A.alloc([128, NT, 5], BF16) for _ in range(2)]
    r1 = A.alloc([128, NT], F32)
    r2 = A.alloc([128, NT], F32)
    a1f = A.alloc([128, NT], F32)
    oh = [A.alloc([128, 1024], BF16) for _ in range(4)]
    rows5 = A.alloc([5, 1024], F32)
    tokf = A.alloc([128, 8], F32)
    pvs = A.alloc([128, 40], F32)
    lof = A.alloc([128, 1], F32)
    S.add("dve", lambda e: e.tensor_scalar(out=lof, in0=tokid[:, 0:1], scalar1=1.0, scalar2=None, op0=ALU.mult), reads=["tokid"], writes=["lof"])
    for v2 in range(2):
        S.add("dve", lambda e, v2=v2: e.tensor_scalar(out=r1, in0=tokid, scalar1=lof[:, 0:1], scalar2=1.0 / 128, op0=ALU.subtract, op1=ALU.mult), reads=["tokid", "lof"], writes=["r1"])
        S.add("dve", lambda e, v2=v2: e.tensor_copy(out=vals5[v2][:, :, 0], in_=r1), reads=["r1"], writes=[f"vals{v2}"])
        S.add("dve", lambda e, v2=v2: e.tensor_copy(out=vals5[v2][:, :, 1], in_=lof[:, 0:1].broadcast_to([128, NT])), reads=["lof"], writes=[f"vals{v2}"])
    ohc = 0
    for ex_ in range(16):
        v2 = ex_ % 2
        bb = 4 + 2 * (ex_ % 2)
        V = vals5[v2]
        S.add("dve", lambda e, V=V, ex_=ex_: e.tensor_copy(out=V[:, :, 2], in_=aff_sb[:, :, ex_]), reads=["aff_sb"], writes=[f"vals{v2}"])
        S.add("dve", lambda e, V=V: e.tensor_copy(out=a1f, in_=V[:, :, 2]), reads=[f"vals{v2}"], writes=["a1f"])
        S.add("dve", lambda e, ex_=ex_: e.tensor_tensor(out=r1, in0=aff_sb[:, :, ex_], in1=a1f, op=ALU.subtract), reads=["aff_sb", "a1f"], writes=["r1"])
        S.add("dve", lambda e, V=V: e.tensor_copy(out=V[:, :, 3], in_=r1), reads=["r1"], writes=[f"vals{v2}"])
        S.add("dve", lambda e, V=V: e.tensor_copy(out=a1f, in_=V[:, :, 3]), reads=[f"vals{v2}"], writes=["a1f"])
        S.add("dve", lambda e: e.tensor_tensor(out=r2, in0=r1, in1=a1f, op=ALU.subtract), reads=["r1", "a1f"], writes=["r2"])
        S.add("dve", lambda e, V=V: e.tensor_copy(out=V[:, :, 4], in_=r2), reads=["r2"], writes=[f"vals{v2}"])
        for j in range(NT):
            so = ohc % 4
            eng_ = "dve"
            ohc += 1
            S.add(eng_, lambda e, so=so, j=j, ex_=ex_: e.tensor_scalar(out=oh[so], in0=iota_sb, scalar1=pos[:, j, ex_:ex_ + 1], scalar2=maskb[:, j, ex_:ex_ + 1],
                                                                     op0=ALU.is_equal, op1=ALU.mult),
                  reads=["iota_sb", "pos", "maskb"], writes=[f"oh{so}"])
            for h in range(2):
                S.add("pe", lambda e, so=so, j=j, h=h, bb=bb, V=V: e.matmul(ps[bb + h][0:5, :], lhsT=V[:, j, :], rhs=oh[so][:, h * 512:(h + 1) * 512],
                                                                        start=(j == 0), stop=(j == NT - 1)),
                      reads=[f"oh{so}", f"vals{v2}"], writes=[f"ps{bb + h}"])
        for h in range(2):
            S.add("act", lambda e, h=h, bb=bb: e.activation(out=rows5[:, h * 512:(h + 1) * 512], in_=ps[bb + h][0:5, :], func=AF.Copy), reads=[f"ps{bb + h}"], writes=["rows5"])
        for sc in range(8):
            S.add("pe", lambda e, sc=sc: e.transpose(out=ps[3][:, sc * 5:(sc + 1) * 5], in_=rows5[0:5, sc * 128:(sc + 1) * 128], identity=identf[0:5, 0:5]),
                  reads=["rows5", "identf"], writes=["ps3"])
        S.add("act", lambda e: e.activation(out=pvs, in_=ps[3][:, 0:40], func=AF.Copy), reads=["ps3"], writes=["pvs"])
        pv = pvs.rearrange("p (s t) -> p s t", t=5)
        S.add("dve", lambda e, pv=pv: e.scalar_tensor_tensor(out=tokf, in0=pv[:, :, 0], scalar=128.0, in1=pv[:, :, 1], op0=ALU.mult, op1=ALU.add), reads=["pvs"], writes=["tokf"])
        S.add("dve", lambda e, ex_=ex_: e.tensor_copy(out=idx_all[:, ex_, :], in_=tokf), reads=["tokf"], writes=["idx_all"])
        S.add("dve", lambda e, pv=pv, ex_=ex_: e.tensor_tensor(out=w_all[:, ex_, :], in0=pv[:, :, 2], in1=pv[:, :, 3], op=ALU.add), reads=["pvs"], writes=["w_all"])
        S.add("dve", lambda e, pv=pv, ex_=ex_: e.tensor_tensor(out=w_all[:, ex_, :], in0=w_all[:, ex_, :], in1=pv[:, :, 4], op=ALU.add), reads=["pvs", "w_all"], writes=["w_all"])
    if "idx" in dbg:
        dbg_out["idx"] = nc.dram_tensor("dbg_idx", [128, 16, 8], I32, kind="ExternalOutput")
        dbg_out["wsl"] = nc.dram_tensor("dbg_w", [128, 16, 8], F32, kind="ExternalOutput")
        dma("sp", dbg_out["idx"].ap(), idx_all, reads=["idx_all"], stream="c0")
        dma("sp", dbg_out["wsl"].ap(), w_all, reads=["w_all"], stream="c0")
    if stop_after == "F3":
        S.emit()
        return nc, dbg_out

    S.barrier()
    keep = A.off
    A.off = A.lo
    idx2 = A.alloc([128, 16, 8], I32)
    w2 = A.alloc([128, 16, 8], F32)
    S.add("dve", lambda e: e.tensor_copy(out=idx2, in_=idx_all), reads=["idx_all"], writes=["idx2"])
    S.add("dve", lambda e: e.tensor_copy(out=w2, in_=w_all), reads=["w_all"], writes=["w2"])
    S.barrier()
    wsl = [A.alloc([128, 8 * 2048], BF16) for _ in range(4)]
    xeT = A.alloc([128, 8, 1024], BF16)
    hidT = A.alloc([128, 16, 1024], BF16)
    xeg = [A.alloc([128, D], BF16) for _ in range(2)]
    ye = [A.alloc([128, D], F32) for _ in range(2)]
    sg = [A.alloc([128, 512], BF16) for _ in range(2)]
    wseq = [0]

    def wload(src_ap):
        k = wseq[0] % 4
        wseq[0] += 1
        flat = src_ap.rearrange("p a b -> p (a b)")
        for hh in range(2):
            dma("pool", wsl[k][:, hh * 8192:(hh + 1) * 8192], flat[:, hh * 8192:(hh + 1) * 8192], writes=[f"wsl{k}"], stream=f"w{k}{hh}")
        return k

    slots = {}
    slots[(0, "g")] = wload(wg_d.ap()[0])
    slots[(0, "u")] = wload(wu_d.ap()[0])
    slots[(0, "d")] = wload(wd_d.ap()[0])
    gcnt = [0]
    ycnt = 0

    def g_issue(ex_, sc):
        xs_ = gcnt[0] % 2
        gcnt[0] += 1
        S.add("pool", lambda e: e.indirect_dma_start(out=xeg[xs_], out_offset=None, in_=h2_s.ap(),
                                                     in_offset=bass.IndirectOffsetOnAxis(ap=idx2[:, ex_, sc:sc + 1], axis=0)),
              reads=["idx2", "h2_s"], writes=[f"xeg{xs_}"], dma=f"gath{xs_}")
        return xs_

    def g_transpose(xs_, sc):
        for kc in range(8):
            S.add("pe", lambda e, kc=kc: e.transpose(out=psb(0)[:, kc * 128:(kc + 1) * 128], in_=xeg[xs_][:, kc * 128:(kc + 1) * 128], identity=I_b),
                  reads=[f"xeg{xs_}", "identb"], writes=["ps0"])
        S.add("act", lambda e: e.activation(out=xeT[:, :, sc * 128:(sc + 1) * 128], in_=psb(0)[:, :].rearrange("p (k t) -> p k t", k=8), func=AF.Copy),
              reads=["ps0"], writes=["xeT"])

    for ex_ in range(16):
        kg_, ku_, kd_ = slots[(ex_, "g")], slots[(ex_, "u")], slots[(ex_, "d")]
        wgv = wsl[kg_].rearrange("p (a b) -> p a b", a=8)
        wuv = wsl[ku_].rearrange("p (a b) -> p a b", a=8)
        wdv = wsl[kd_].rearrange("p (a b) -> p a b", a=16)
        if ex_ == 0:
            for sc in range(8):
                xs_ = g_issue(0, sc)
                g_transpose(xs_, sc)
        if ex_ + 1 < 16:
            slots[(ex_ + 1, "g")] = wload(wg_d.ap()[ex_ + 1])
        for ffc in range(16):
            for h in range(2):
                bg, bu = 1 + (ffc * 2 + h) % 2, 3 + (ffc * 2 + h) % 2
                s_ = (ffc * 2 + h) % 2
                for kc in range(8):
                    S.add("pe", lambda e, bg=bg, kc=kc, ffc=ffc, h=h, wgv=wgv: e.matmul(ps[bg][:, :], lhsT=wgv[:, kc, ffc * 128:(ffc + 1) * 128], rhs=xeT[:, kc, h * 512:(h + 1) * 512],
                                                                                    start=(kc == 0), stop=(kc == 7)),
                          reads=[f"wsl{kg_}", "xeT"], writes=[f"ps{bg}"])
                for kc in range(8):
                    S.add("pe", lambda e, bu=bu, kc=kc, ffc=ffc, h=h, wuv=wuv: e.matmul(ps[bu][:, :], lhsT=wuv[:, kc, ffc * 128:(ffc + 1) * 128], rhs=xeT[:, kc, h * 512:(h + 1) * 512],
                                                                                    start=(kc == 0), stop=(kc == 7)),
                          reads=[f"wsl{ku_}", "xeT"], writes=[f"ps{bu}"])
                S.add("act", lambda e, bg=bg, s_=s_: e.activation(out=sg[s_], in_=ps[bg][:, :], func=AF.Silu), reads=[f"ps{bg}"], writes=[f"sg{s_}"])
                S.add("dve", lambda e, bu=bu, s_=s_, ffc=ffc, h=h: e.tensor_tensor(out=hidT[:, ffc, h * 512:(h + 1) * 512], in0=ps[bu][:, :], in1=sg[s_], op=ALU.mult),
                      reads=[f"ps{bu}", f"sg{s_}"], writes=["hidT"])
        if ex_ + 1 < 16:
            slots[(ex_ + 1, "u")] = wload(wu_d.ap()[ex_ + 1])
            slots[(ex_ + 1, "d")] = wload(wd_d.ap()[ex_ + 1])
        for sc in range(8):
            ys = ycnt % 2
            ycnt += 1
            nxs = g_issue(ex_ + 1, sc) if ex_ + 1 < 16 else None
            for dh in range(2):
                bd = 5 + (sc * 2 + dh) % 3
                for ffc in range(16):
                    S.add("pe", lambda e, bd=bd, ffc=ffc, sc=sc, dh=dh, wdv=wdv: e.matmul(ps[bd][:, :], lhsT=hidT[:, ffc, sc * 128:(sc + 1) * 128], rhs=wdv[:, ffc, dh * 512:(dh + 1) * 512],
                                                                                      start=(ffc == 0), stop=(ffc == 15)),
                          reads=["hidT", f"wsl{kd_}"], writes=[f"ps{bd}"])
                S.add("dve", lambda e, bd=bd, ys=ys, dh=dh, sc=sc, ex_=ex_: e.scalar_tensor_tensor(out=ye[ys][:, dh * 512:(dh + 1) * 512], in0=ps[bd][:, :], scalar=w2[:, ex_, sc:sc + 1],
                                                                                                in1=gt2b[:, dh * 512:(dh + 1) * 512], op0=ALU.mult, op1=ALU.mult),
                      reads=[f"ps{bd}", "w2", "gt2b"], writes=[f"ye{ys}"])
            S.add("pool", lambda e, ys=ys, sc=sc, ex_=ex_: e.indirect_dma_start(out=out_d.ap(), out_offset=bass.IndirectOffsetOnAxis(ap=idx2[:, ex_, sc:sc + 1], axis=0),
                                                                             in_=ye[ys], in_offset=None, compute_op=ALU.add),
                  reads=[f"ye{ys}", "idx2", "out_d"], writes=["out_d"], dma="scat")
            if nxs is not None:
                g_transpose(nxs, sc)

    S.emit()
    return nc, dbg_out


QPERM = np.array([1536 + (hh * 4 + g) * 64 + d for g in range(4) for hh in range(2) for d in range(64)])
COLPERM = np.concatenate([np.arange(0, 512), np.arange(1024, 1536), np.arange(512, 1024), QPERM, np.arange(2048, 4352)])
HPERM = COLPERM[:1536]
ATT_ROWPERM = QPERM - 1536


def consts():
    c = {}
    ident = np.zeros((128, 256), np.float32)
    ident[:, :128] = np.eye(128)
    ident[:, 128:] = np.eye(128)[::-1]
    c["ident"] = ident
    ind = np.ones((4, 5, 128), np.float32)
    ind[0, 1, 0] = 0
    ind[2, 2, 127] = 0
    ind[0, 3, 127] = 0
    ind[2, 4, 0] = 0
    c["ind"] = ind
    t01 = np.linspace(0.0, 1.0, L, dtype=np.float32)
    bands = 16
    f = np.linspace(1e-4, bands - 1, bands, dtype=np.float32)[None, :]
    w = (np.float32(2.0 * math.pi) * np.arange(L, dtype=np.float32)[:, None] / np.float32(L)).astype(np.float32)
    z = np.concatenate([t01[:, None], np.cos(f * w), -np.sin(f * w)], axis=-1).astype(np.float32)
    c["zT"] = np.ascontiguousarray(np.stack([z.T, z[::-1].T], axis=1))
    c["t01"] = np.ascontiguousarray(np.stack([t01, t01[::-1]], axis=0))
    max_decay = math.log(1e-2) / 0.3
    min_decay = math.log(1e-2) / 1.5
    deltas = np.abs(np.linspace(min_decay, max_decay, 512, dtype=np.float32))
    c["deltacol"] = np.ascontiguousarray((-deltas).reshape(4, 128).T)
    rows = L // 64
    row = np.repeat(np.arange(rows), 64).astype(np.float32)
    col = np.tile(np.arange(64), rows).astype(np.float32)
    inv = (10000.0 ** (-np.arange(0, 32, 2, dtype=np.float32) / 32)).astype(np.float32)
    ang = np.concatenate([row[:, None] * inv, col[:, None] * inv], axis=-1)
    rope = np.concatenate([np.cos(ang), np.sin(ang)], axis=-1).astype(np.float32)
    c["rope"] = np.ascontiguousarray(rope.reshape(NT, 128, 64).transpose(1, 0, 2))
    c["iota"] = np.tile(np.arange(1024, dtype=np.float32)[None], (128, 1))
    c["tokid"] = (np.arange(NT)[None, :] * 128 + np.arange(128)[:, None]).astype(np.float32)
    c["tri"] = np.triu(np.ones((128, 128), np.float32))
    return c


def prep_shared(inp):
    l = 0
    m = dict(consts())
    f32 = lambda a: np.ascontiguousarray(a, dtype=np.float32)
    m["wada"] = f32(inp["w_ada"][l].reshape(8, 128, 6144).transpose(1, 0, 2))
    m["bada"] = f32(inp["b_ada"][l][None])
    m["gmixcol"] = f32(inp["g_mix"][l].reshape(8, 128).T)
    m["gffnrow"] = f32(inp["g_ffn"][l][None])
    m["win"] = f32(inp["w_in"][l][:, COLPERM].reshape(8, 128, 4352).transpose(1, 0, 2))
    bin_p = inp["b_in"][l][COLPERM]
    m["binh"] = f32(bin_p[None, :1536])
    m["bino"] = f32(bin_p[None, 1536:])
    m["sw"] = f32(inp["short_w"][l][:, HPERM])
    m["sb"] = f32(inp["short_b"][l][HPERM][None])
    m["hw1"] = f32(inp["hy_w1"][l]); m["hw2"] = f32(inp["hy_w2"][l]); m["hw3"] = f32(inp["hy_w3"][l]); m["hw4"] = f32(inp["hy_w4"][l])
    m["hbcol"] = f32(np.stack([inp["hy_b1"][l], inp["hy_b2"][l], inp["hy_b3"][l]], axis=1))
    m["hfrcol"] = f32(inp["hy_freq"][l].T)
    m["fbcol"] = f32(inp["hy_bias"][l].reshape(2, 4, 128).transpose(2, 0, 1).reshape(128, 8))
    m["qgain"] = f32(inp["q_gain"][l][None]); m["kgain"] = f32(inp["k_gain"][l][None])
    m["why"] = f32(inp["w_hy_out"][l].reshape(4, 128, D).transpose(1, 0, 2))
    m["wat"] = f32(inp["w_att_out"][l][ATT_ROWPERM].reshape(4, 128, D).transpose(1, 0, 2))
    m["wout"] = f32(inp["w_out"][l].reshape(8, 128, D).transpose(1, 0, 2))
    m["wr"] = f32(inp["w_router"][l].reshape(8, 128, 16).transpose(1, 0, 2))
    m["wg"] = f32(inp["w_gate"][l].reshape(16, 8, 128, 2048).transpose(0, 2, 1, 3))
    m["wu"] = f32(inp["w_up"][l].reshape(16, 8, 128, 2048).transpose(0, 2, 1, 3))
    m["wd"] = f32(inp["w_down"][l].reshape(16, 16, 128, D).transpose(0, 2, 1, 3))
    return m


def prep(inp, b, shared=None):
    m = dict(shared if shared is not None else prep_shared(inp))
    m["x"] = np.ascontiguousarray(inp["x"][b], dtype=np.float32)
    m["ccol"] = np.ascontiguousarray(inp["c"][b].reshape(8, 128).T, dtype=np.float32)
    return m


_CACHE = {}


REAL_CORES = (0, 1, 4, 5)


def kernel(**inputs):
    inp = {k: np.asarray(v) for k, v in inputs.items()}
    if "nc" not in _CACHE:
        _CACHE["nc"] = build("F")[0]
    nc = _CACHE["nc"]
    shared = prep_shared(inp)
    real = {core: prep(inp, b, shared) for b, core in enumerate(REAL_CORES)}
    ck = set(consts().keys())
    zero = {k: (v if k in ck else np.zeros_like(v)) for k, v in real[0].items()}
    maps = [real.get(core, zero) for core in range(8)]
    res = run_bass_kernel_spmd(nc, maps, core_ids=list(range(8)))
    return np.stack([res.results[core]["out"] for core in REAL_CORES], axis=0).astype(np.float32)
```
